# Optimizing a Trainium2 kernel written in Bass

```python
import jax, jax.numpy as jnp
from jax import lax
import numpy as np

D_MODEL = 1024
BATCH = 8
SEQ = 2048
DEPTH = 4
DEC_BATCH = 128
DEC_SEQ = 4
PAST_LEN = 16384
PAGE_SIZE = 128

N_MIXERS = 2
N_ML_LAYERS = (DEPTH + 1) // 2
N_CM_LAYERS = DEPTH // 2
ML_PROJ_FACTOR = 2
ML_INNER = ML_PROJ_FACTOR * D_MODEL
ML_HEADS = 4
ML_HEAD_DIM = ML_INNER // ML_HEADS
ML_QKV_BLOCK = 4
ML_N_BLOCKS = ML_INNER // ML_QKV_BLOCK
ML_CONV_W = 4
ML_CHUNK = 128
CM_CHUNK = 128
CM_GROUPS = 4
CM_WIDTH = D_MODEL
CM_GROUP_DIM = CM_WIDTH // CM_GROUPS
D_FF = 2816
EPS = 1e-6

kernel_name = 'hybrid_mlstm_chunkmlp_macaron_step'


def _rmsnorm(x, g):
    xf = x.astype(jnp.float32)
    y = xf * lax.rsqrt(jnp.mean(xf * xf, axis=-1, keepdims=True) + EPS)
    return (y * g.astype(jnp.float32)).astype(x.dtype)


def _swiglu(x, w_gate, w_up, w_down):
    return (jax.nn.silu(x @ w_gate) * (x @ w_up)) @ w_down


def _blockdiag(x, w):
    B, S, _ = x.shape
    xb = x.reshape(B, S, ML_N_BLOCKS, ML_QKV_BLOCK)
    return jnp.einsum('bsnc,ncd->bsnd', xb, w).reshape(B, S, ML_INNER)


def _mlstm_chunk(C0, n0, m0, q, k, v, ig, lf):
    L = q.shape[1]
    bt = jnp.cumsum(lf, axis=1).transpose(0, 2, 1)
    it = ig.transpose(0, 2, 1)
    causal = jnp.tril(jnp.ones((L, L), dtype=bool))
    dmat = jnp.where(causal, bt[..., :, None] - bt[..., None, :] + it[..., None, :], -jnp.inf)
    m_inter = m0[..., None] + bt
    m = jnp.maximum(jnp.max(dmat, axis=-1), m_inter)
    w_intra = jnp.exp(dmat - m[..., None])
    w_inter = jnp.exp(m_inter - m)
    sc = jnp.einsum('bthd,bshd->bhts', q, k) * w_intra
    num = (jnp.einsum('bhts,bshe->bthe', sc, v)
           + jnp.einsum('bthd,bhde->bthe', q, C0) * w_inter.transpose(0, 2, 1)[..., None])
    nq = jnp.sum(sc, axis=-1) + w_inter * jnp.einsum('bthd,bhd->bht', q, n0)
    den = jnp.maximum(jnp.abs(nq), jnp.exp(-m)).transpose(0, 2, 1)[..., None]
    h = num / den
    bl = bt[..., -1]
    ml = m[..., -1]
    g_inter = jnp.exp(m0 + bl - ml)
    g_s = jnp.exp(bl[..., None] - bt + it - ml[..., None])
    C = g_inter[..., None, None] * C0 + jnp.einsum('bhs,bshd,bshe->bhde', g_s, k, v)
    n = g_inter[..., None] * n0 + jnp.einsum('bhs,bshd->bhd', g_s, k)
    return h, C, n, ml


def _mlstm_core(q, k, v, ig, lf, C0, n0, m0, chunk):
    B, S, H, DH = q.shape
    nc = S // chunk

    def to_chunks(a):
        return a.reshape((B, nc, chunk) + a.shape[2:]).swapaxes(0, 1)

    def step(carry, blk):
        C, n, m = carry
        h, C, n, m = _mlstm_chunk(C, n, m, *blk)
        return (C, n, m), h

    (C, n, m), hs = lax.scan(step, (C0, n0, m0),
                             (to_chunks(q), to_chunks(k), to_chunks(v), to_chunks(ig), to_chunks(lf)))
    return hs.swapaxes(0, 1).reshape(B, S, H, DH), C, n, m


def _mlstm_mixer(xn, conv_buf, C0, n0, m0, p, j, chunk):
    B, S, _ = xn.shape
    f32 = jnp.float32
    up = xn @ p['ml_w_up'][j]
    xm, z = up[..., :ML_INNER], up[..., ML_INNER:]
    xfull = jnp.concatenate([conv_buf.astype(xm.dtype), xm], axis=1)
    wc = p['ml_w_conv'][j]
    xc = p['ml_b_conv'][j]
    for w in range(ML_CONV_W):
        xc = xc + xfull[:, w:w + S] * wc[w]
    xc = jax.nn.silu(xc)
    new_buf = xfull[:, xfull.shape[1] - (ML_CONV_W - 1):]
    q = _blockdiag(xc, p['ml_w_q'][j])
    k = _blockdiag(xc, p['ml_w_k'][j])
    v = _blockdiag(xm, p['ml_w_v'][j])
    gate_in = jnp.concatenate([q, k, v], axis=-1)
    ig = (gate_in @ p['ml_w_ig'][j] + p['ml_b_ig'][j]).astype(f32)
    lf = jax.nn.log_sigmoid((gate_in @ p['ml_w_fg'][j] + p['ml_b_fg'][j]).astype(f32))

    def heads(a):
        return a.reshape(B, S, ML_HEADS, ML_HEAD_DIM).astype(f32)

    h, C, n, m = _mlstm_core(heads(q), heads(k) * (ML_HEAD_DIM ** -0.5), heads(v), ig, lf,
                             C0.astype(f32), n0.astype(f32), m0.astype(f32), chunk)
    mu = jnp.mean(h, axis=-1, keepdims=True)
    var = jnp.mean(jnp.square(h - mu), axis=-1, keepdims=True)
    hn = ((h - mu) * lax.rsqrt(var + EPS)).reshape(B, S, ML_INNER).astype(xn.dtype) * p['ml_hn_g'][j]
    out = (hn + p['ml_skip'][j] * xc) * jax.nn.silu(z)
    return out @ p['ml_w_down'][j], new_buf, C, n, m


def _chunk_mlp(xn, p, j):
    B, S, _ = xn.shape
    hp = jax.nn.gelu(xn @ p['cm_w_in'][j] + p['cm_b_in'][j], approximate=False)
    u, v = hp[..., :CM_WIDTH], hp[..., CM_WIDTH:]
    vf = v.astype(jnp.float32)
    mu = jnp.mean(vf, axis=-1, keepdims=True)
    var = jnp.mean(jnp.square(vf - mu), axis=-1, keepdims=True)
    vn = ((vf - mu) * lax.rsqrt(var + EPS)).astype(v.dtype) * p['cm_ln_g'][j]
    L = min(S, CM_CHUNK)
    nc = S // L
    causal = jnp.tril(jnp.ones((L, L), dtype=bool))
    ws = jnp.where(causal, p['cm_w_s'][j][:, :L, :L], 0)
    bs = p['cm_b_s'][j][:, :L]
    vc = vn.reshape(B, nc, L, CM_GROUPS, CM_GROUP_DIM)
    mix = jnp.einsum('gts,bcsgd->bctgd', ws, vc) + bs.T[None, None, :, :, None]
    y = u * mix.reshape(B, S, CM_WIDTH)
    return y @ p['cm_w_out'][j], vn


def _trunk(x, conv_bufs, Cs, ns, ms, p, ml_chunk):
    new_C, new_n, new_m, new_conv, new_v = [], [], [], [], []
    for i in range(DEPTH):
        x = x + 0.5 * _swiglu(_rmsnorm(x, p['norm_ff1'][i]),
                              p['ffn1_w_gate'][i], p['ffn1_w_up'][i], p['ffn1_w_down'][i])
        xn = _rmsnorm(x, p['norm_mix'][i])
        j = i // N_MIXERS
        if i % N_MIXERS == 0:
            out, buf, C, n, m = _mlstm_mixer(xn, conv_bufs[j], Cs[j], ns[j], ms[j], p, j, ml_chunk)
            new_C.append(C)
            new_n.append(n)
            new_m.append(m)
            new_conv.append(buf)
        else:
            out, vrows = _chunk_mlp(xn, p, j)
            new_v.append(vrows)
        x = x + out
        x = x + 0.5 * _swiglu(_rmsnorm(x, p['norm_ff2'][i]),
                              p['ffn2_w_gate'][i], p['ffn2_w_up'][i], p['ffn2_w_down'][i])
    y = _rmsnorm(x, p['norm_final'])
    return y, jnp.stack(new_C), jnp.stack(new_n), jnp.stack(new_m), jnp.stack(new_conv), new_v


def setup_inputs(seed: int = 0) -> dict:
    key = jax.random.key(seed)
    ks = jax.random.split(key, 40)
    f32 = jnp.float32

    def nrm(k, shape, scale):
        return jax.random.normal(k, shape, f32) * scale

    d = {}
    d['x_prompt'] = nrm(ks[0], (BATCH, SEQ, D_MODEL), 1.0)
    d['x_sample'] = nrm(ks[1], (DEC_BATCH, DEC_SEQ, D_MODEL), 1.0)
    d['state_C'] = nrm(ks[2], (N_ML_LAYERS, DEC_BATCH, ML_HEADS, ML_HEAD_DIM, ML_HEAD_DIM), 0.05)
    d['state_n'] = nrm(ks[3], (N_ML_LAYERS, DEC_BATCH, ML_HEADS, ML_HEAD_DIM), 0.1)
    d['state_m'] = nrm(ks[4], (N_ML_LAYERS, DEC_BATCH, ML_HEADS), 1.0)
    d['state_conv'] = nrm(ks[5], (N_ML_LAYERS, DEC_BATCH, ML_CONV_W - 1, ML_INNER), 1.0)
    d['norm_ff1'] = 1.0 + nrm(ks[6], (DEPTH, D_MODEL), 0.02)
    d['norm_mix'] = 1.0 + nrm(ks[7], (DEPTH, D_MODEL), 0.02)
    d['norm_ff2'] = 1.0 + nrm(ks[8], (DEPTH, D_MODEL), 0.02)
    d['norm_final'] = 1.0 + nrm(ks[9], (D_MODEL,), 0.02)
    d['ffn1_w_gate'] = nrm(ks[10], (DEPTH, D_MODEL, D_FF), D_MODEL ** -0.5)
    d['ffn1_w_up'] = nrm(ks[11], (DEPTH, D_MODEL, D_FF), D_MODEL ** -0.5)
    d['ffn1_w_down'] = nrm(ks[12], (DEPTH, D_FF, D_MODEL), D_FF ** -0.5)
    d['ffn2_w_gate'] = nrm(ks[13], (DEPTH, D_MODEL, D_FF), D_MODEL ** -0.5)
    d['ffn2_w_up'] = nrm(ks[14], (DEPTH, D_MODEL, D_FF), D_MODEL ** -0.5)
    d['ffn2_w_down'] = nrm(ks[15], (DEPTH, D_FF, D_MODEL), D_FF ** -0.5)
    d['ml_w_up'] = nrm(ks[16], (N_ML_LAYERS, D_MODEL, 2 * ML_INNER), D_MODEL ** -0.5)
    d['ml_w_conv'] = nrm(ks[17], (N_ML_LAYERS, ML_CONV_W, ML_INNER), ML_CONV_W ** -0.5)
    d['ml_b_conv'] = nrm(ks[18], (N_ML_LAYERS, ML_INNER), 0.02)
    d['ml_w_q'] = nrm(ks[19], (N_ML_LAYERS, ML_N_BLOCKS, ML_QKV_BLOCK, ML_QKV_BLOCK), ML_QKV_BLOCK ** -0.5)
    d['ml_w_k'] = nrm(ks[20], (N_ML_LAYERS, ML_N_BLOCKS, ML_QKV_BLOCK, ML_QKV_BLOCK), ML_QKV_BLOCK ** -0.5)
    d['ml_w_v'] = nrm(ks[21], (N_ML_LAYERS, ML_N_BLOCKS, ML_QKV_BLOCK, ML_QKV_BLOCK), ML_QKV_BLOCK ** -0.5)
    d['ml_w_ig'] = nrm(ks[22], (N_ML_LAYERS, 3 * ML_INNER, ML_HEADS), (3 * ML_INNER) ** -0.5)
    d['ml_b_ig'] = nrm(ks[23], (N_ML_LAYERS, ML_HEADS), 0.1)
    d['ml_w_fg'] = nrm(ks[24], (N_ML_LAYERS, 3 * ML_INNER, ML_HEADS), (3 * ML_INNER) ** -0.5)
    d['ml_b_fg'] = jnp.linspace(3.0, 6.0, ML_HEADS, dtype=f32)[None, :] + nrm(ks[25], (N_ML_LAYERS, ML_HEADS), 0.1)
    d['ml_hn_g'] = 1.0 + nrm(ks[26], (N_ML_LAYERS, ML_INNER), 0.02)
    d['ml_skip'] = 1.0 + nrm(ks[27], (N_ML_LAYERS, ML_INNER), 0.02)
    d['ml_w_down'] = nrm(ks[28], (N_ML_LAYERS, ML_INNER, D_MODEL), ML_INNER ** -0.5)
    d['cm_w_in'] = nrm(ks[29], (N_CM_LAYERS, D_MODEL, 2 * CM_WIDTH), D_MODEL ** -0.5)
    d['cm_b_in'] = nrm(ks[30], (N_CM_LAYERS, 2 * CM_WIDTH), 0.02)
    d['cm_ln_g'] = 1.0 + nrm(ks[31], (N_CM_LAYERS, CM_WIDTH), 0.02)
    d['cm_w_s'] = nrm(ks[32], (N_CM_LAYERS, CM_GROUPS, CM_CHUNK, CM_CHUNK), 0.5 * CM_CHUNK ** -0.5)
    d['cm_b_s'] = 1.0 + nrm(ks[33], (N_CM_LAYERS, CM_GROUPS, CM_CHUNK), 0.02)
    d['cm_w_out'] = nrm(ks[34], (N_CM_LAYERS, CM_WIDTH, D_MODEL), CM_WIDTH ** -0.5)
    return d


def reference(x_prompt, x_sample, state_C, state_n, state_m, state_conv,
              norm_ff1, norm_mix, norm_ff2, norm_final,
              ffn1_w_gate, ffn1_w_up, ffn1_w_down, ffn2_w_gate, ffn2_w_up, ffn2_w_down,
              ml_w_up, ml_w_conv, ml_b_conv, ml_w_q, ml_w_k, ml_w_v,
              ml_w_ig, ml_b_ig, ml_w_fg, ml_b_fg, ml_hn_g, ml_skip, ml_w_down,
              cm_w_in, cm_b_in, cm_ln_g, cm_w_s, cm_b_s, cm_w_out):
    p = {'norm_ff1': norm_ff1, 'norm_mix': norm_mix, 'norm_ff2': norm_ff2, 'norm_final': norm_final,
         'ffn1_w_gate': ffn1_w_gate, 'ffn1_w_up': ffn1_w_up, 'ffn1_w_down': ffn1_w_down,
         'ffn2_w_gate': ffn2_w_gate, 'ffn2_w_up': ffn2_w_up, 'ffn2_w_down': ffn2_w_down,
         'ml_w_up': ml_w_up, 'ml_w_conv': ml_w_conv, 'ml_b_conv': ml_b_conv,
         'ml_w_q': ml_w_q, 'ml_w_k': ml_w_k, 'ml_w_v': ml_w_v,
         'ml_w_ig': ml_w_ig, 'ml_b_ig': ml_b_ig, 'ml_w_fg': ml_w_fg, 'ml_b_fg': ml_b_fg,
         'ml_hn_g': ml_hn_g, 'ml_skip': ml_skip, 'ml_w_down': ml_w_down,
         'cm_w_in': cm_w_in, 'cm_b_in': cm_b_in, 'cm_ln_g': cm_ln_g,
         'cm_w_s': cm_w_s, 'cm_b_s': cm_b_s, 'cm_w_out': cm_w_out}
    f32 = jnp.float32
    bp = x_prompt.shape[0]
    zero_conv = jnp.zeros((N_ML_LAYERS, bp, ML_CONV_W - 1, ML_INNER), x_prompt.dtype)
    zero_C = jnp.zeros((N_ML_LAYERS, bp, ML_HEADS, ML_HEAD_DIM, ML_HEAD_DIM), f32)
    zero_n = jnp.zeros((N_ML_LAYERS, bp, ML_HEADS, ML_HEAD_DIM), f32)
    zero_m = jnp.zeros((N_ML_LAYERS, bp, ML_HEADS), f32)
    y_prompt, C_prompt, n_prompt, m_prompt, conv_prompt, _ = _trunk(
        x_prompt, zero_conv, zero_C, zero_n, zero_m, p, min(ML_CHUNK, x_prompt.shape[1]))
    y_sample, C_sample, n_sample, m_sample, conv_sample, v_rows = _trunk(
        x_sample, state_conv, state_C, state_n, state_m, p, x_sample.shape[1])
    v_sample = jnp.stack(v_rows)
    return (y_prompt, y_sample, C_prompt, n_prompt, m_prompt, conv_prompt,
            C_sample, n_sample, m_sample, conv_sample, v_sample)
```

```python
from contextlib import ExitStack
import math
import types
import numpy as np
import concourse.bass as bass
import concourse.mybir as mybir
from concourse.bass_utils import run_bass_kernel_spmd

F32 = mybir.dt.float32
BF16 = mybir.dt.bfloat16
AF = mybir.ActivationFunctionType
ALU = mybir.AluOpType
AX = mybir.AxisListType

DEPTH = 4
D = 1024
TP = 2048
TS = 64
TT = TP + TS
FF = 2816
NFC = 22
INNER = 2048
H = 4
DH = 512
EPS = 1e-6
NB = 16
N_CORES = 8

ENGS = ("pe", "act", "dve", "pool", "sp")
N_LANES = 8


def _freeze(fn, depth=0):
    if not isinstance(fn, types.FunctionType) or fn.__closure__ is None or depth > 4:
        return fn
    cells = []
    for c in fn.__closure__:
        try:
            v = c.cell_contents
        except ValueError:
            cells.append(c)
            continue
        if isinstance(v, types.FunctionType):
            v = _freeze(v, depth + 1)
        cells.append(types.CellType(v))
    g = types.FunctionType(fn.__code__, fn.__globals__, fn.__name__, fn.__defaults__, tuple(cells))
    g.__kwdefaults__ = fn.__kwdefaults__
    return g


class Res:
    __slots__ = ("lw", "rd")

    def __init__(self, lw=None):
        self.lw = lw
        self.rd = []


class Tile:
    def __init__(self, t, base_lw=None):
        self.t = t
        self._res = {}
        self.base_lw = base_lw

    def __getitem__(self, key):
        r = self._res.get(key)
        if r is None:
            r = self._res[key] = Res(self.base_lw)
        return r

    @property
    def r(self):
        return self[None]


class Op:
    __slots__ = ("eng", "fn", "deps", "dma", "lane", "val", "signal", "idx")


class Prog:
    def __init__(self, nc, arena_words):
        self.nc = nc
        self.ops = []
        self.stack = ExitStack()
        self.n_t = 0
        self.arena = self.stack.enter_context(nc.sbuf_tensor("arena", [128, arena_words], F32))
        self.arena_words = arena_words
        self.arena_ptr = 0
        self.arena_tiles = []
        self.cur_barrier = None
        self.dummy = self.sbuf([128, 8], F32, "dummy_bar")

    def sbuf(self, shape, dtype, name=None):
        self.n_t += 1
        t = self.stack.enter_context(self.nc.sbuf_tensor("sb_" + (name or f"t{self.n_t}"), list(shape), dtype))
        tl = Tile(t)
        self.persist = getattr(self, "persist", [])
        self.persist.append(tl)
        return tl

    def psum(self, shape, dtype, name=None):
        self.n_t += 1
        t = self.stack.enter_context(self.nc.psum_tensor("ps_" + (name or f"t{self.n_t}"), list(shape), dtype))
        tl = Tile(t)
        self.persist = getattr(self, "persist", [])
        self.persist.append(tl)
        return tl

    def phase(self):
        ress = []
        for tl in self.arena_tiles:
            ress.extend(tl._res.values())
        if ress:
            d = self.dummy
            o = self.op("pool", lambda e: e.memset(d.t[:, 0:1], 0.0), [], ress + [d.r])
            self.cur_barrier = o.idx
        self.arena_tiles = []
        self.arena_ptr = 0

    def barrier_all(self):
        ress = []
        for tl in self.arena_tiles + getattr(self, "persist", []):
            ress.extend(tl._res.values())
        d = self.dummy
        if d.r not in ress:
            ress.append(d.r)
        self.op("pool", lambda e: e.memset(d.t[:, 0:1], 0.0), [], ress)

    def atile(self, shape, dtype):
        n = 1
        for s in shape[1:]:
            n *= s
        words = (n * (2 if dtype == BF16 else 4) + 3) // 4
        words = (words + 7) // 8 * 8
        a0 = self.arena_ptr
        self.arena_ptr += words
        assert self.arena_ptr <= self.arena_words, f"arena overflow {self.arena_ptr} > {self.arena_words}"
        ap = self.arena.__getitem__((slice(0, shape[0]), slice(a0, a0 + words)))
        if dtype == BF16:
            ap = ap.bitcast(BF16)
        ap = ap[:, 0:n]
        if len(shape) == 3:
            ap = ap.rearrange("p (a b) -> p a b", a=shape[1])
        elif len(shape) == 4:
            ap = ap.rearrange("p (a b c) -> p a b c", a=shape[1], b=shape[2])
        tl = Tile(ap, self.cur_barrier)
        self.arena_tiles.append(tl)
        return tl

    def op(self, eng, fn, reads=(), writes=(), dma=False):
        o = Op()
        o.eng, o.fn, o.dma = eng, _freeze(fn), dma
        o.idx = len(self.ops)
        o.lane = None
        o.val = None
        o.signal = False
        deps = set()
        for r in reads:
            if r.lw is not None:
                deps.add(r.lw)
        for w in writes:
            if w.lw is not None:
                deps.add(w.lw)
            for x in w.rd:
                deps.add(x)
        deps.discard(o.idx)
        o.deps = deps
        for r in reads:
            r.rd.append(o.idx)
        for w in writes:
            w.lw = o.idx
            w.rd = []
        self.ops.append(o)
        return o

    def dma(self, eng, out_ap, in_ap, reads=(), writes=()):
        return self.op(eng, lambda e: e.dma_start(out=out_ap, in_=in_ap, allow_slow_non_contiguous=True), reads, writes, dma=True)

    def emit(self):
        nc = self.nc
        ops = self.ops
        fin = Op()
        fin.eng, fin.fn, fin.dma = "sp", None, False
        fin.idx = len(ops)
        fin.deps = {o.idx for o in ops if o.dma}
        fin.lane = None
        fin.val = None
        fin.signal = False
        ops.append(fin)
        for o in ops:
            nd = set()
            for d in o.deps:
                p = ops[d]
                if (not p.dma) and (not o.dma) and p.eng == o.eng and o.eng == "pe":
                    continue
                nd.add(d)
            o.deps = nd
        for o in ops:
            for d in o.deps:
                ops[d].signal = True
        st = self.stack
        sems = {e: st.enter_context(nc.semaphore(f"s_{e}")) for e in ENGS}
        lanes = {e: [st.enter_context(nc.semaphore(f"l_{e}{i}")) for i in range(N_LANES)] for e in ("sp", "pool")}
        cnt = {e: 0 for e in ENGS}
        lane_cnt = {e: [0] * N_LANES for e in lanes}
        lane_prev = {e: [None] * N_LANES for e in lanes}
        dma_seq = {e: 0 for e in lanes}
        for o in ops:
            if o.dma:
                i = dma_seq[o.eng] % N_LANES
                dma_seq[o.eng] += 1
                o.lane = (o.eng, i)
                prev = lane_prev[o.eng][i]
                if prev is not None:
                    o.deps.add(prev)
                lane_cnt[o.eng][i] += 16
                o.val = lane_cnt[o.eng][i]
                lane_prev[o.eng][i] = o.idx
            elif o.signal:
                cnt[o.eng] += 1
                o.val = cnt[o.eng]

        def sem_of(p):
            if p.dma:
                return lanes[p.lane[0]][p.lane[1]], ("L",) + p.lane
            return sems[p.eng], ("E", p.eng)

        by_eng = {e: [o for o in ops if o.eng == e] for e in ENGS}
        self.stats = {e: len(by_eng[e]) for e in ENGS}
        blk = st.enter_context(nc.Block())

        def run(eng_name, eng):
            known = {}
            for o in by_eng[eng_name]:
                need = {}
                for d in o.deps:
                    p = ops[d]
                    s, key = sem_of(p)
                    if known.get(key, 0) >= p.val:
                        continue
                    if key not in need or need[key][1] < p.val:
                        need[key] = (s, p.val)
                for key, (s, v) in need.items():
                    eng.wait_ge(s, v)
                    known[key] = v
                if o.fn is None:
                    continue
                ins = o.fn(eng)
                if o.dma:
                    ins.then_inc(lanes[o.lane[0]][o.lane[1]], 16)
                elif o.signal:
                    ins.then_inc(sems[o.eng], 1)

        @blk.tensor
        def _(e):
            run("pe", e)

        @blk.scalar
        def _(e):
            run("act", e)

        @blk.vector
        def _(e):
            run("dve", e)

        @blk.gpsimd
        def _(e):
            run("pool", e)

        @blk.sync
        def _(e):
            run("sp", e)

        st.close()


def vec_layout():
    lay = {}
    c = 0

    def add(name, n):
        nonlocal c
        lay[name] = c
        c += n

    for i in range(DEPTH):
        add(f"norm_ff1_{i}", 8)
        add(f"norm_mix_{i}", 8)
        add(f"norm_ff2_{i}", 8)
    add("norm_final", 8)
    for j in range(2):
        for w in range(4):
            add(f"ml_wconv_{j}_{w}", 16)
        add(f"ml_bconv_{j}", 16)
        add(f"ml_hn_g_{j}", 16)
        add(f"ml_skip_{j}", 16)
        add(f"cm_b_in_{j}", 16)
        add(f"cm_ln_g_{j}", 8)
    add("c_eps", 1)
    add("c_lnscale", 1)
    lay["_n"] = c
    return lay


def const_layout():
    lay = {}
    c = 0
    for name, n in [("ident", 128), ("maskP", 128), ("onesP", 128), ("maskS", 64), ("onesS", 64),
                    ("seqS", 16), ("firstS", 16), ("seqP", 1), ("firstP", 1)]:
        lay[name] = c
        c += n
    lay["_n"] = c
    return lay


VL = vec_layout()
CL = const_layout()

FFN_TILES = [(0, 512), (512, 512), (1024, 512), (1536, 512), (2048, 64)]
FFN_PARTS = [(0, 4), (4, 8), (8, 11)]
ML_T = 256


def build_program(n_layers=DEPTH):
    nc = bass.Bass("TRN2", target_bir_lowering=False)
    import os
    ARENA_WORDS = 28712
    P = Prog(nc, ARENA_WORDS)

    def din(name, shape):
        return nc.dram_tensor(name, list(shape), F32, kind="ExternalInput").ap()

    def dout(name, shape):
        return nc.dram_tensor(name, list(shape), F32, kind="ExternalOutput").ap()

    xT_d = din("xT", [D, TT])
    vecs_d = din("vecs", [128, VL["_n"]])
    consts_d = din("consts", [128, CL["_n"]])
    colmask_d = din("colmask", [128, NB, TS])
    wg_d, wu_d, wd_d = {}, {}, {}
    for i in range(DEPTH):
        for a in (1, 2):
            wg_d[i, a] = din(f"wg{i}{a}", [11, 128, 8, 256])
            wu_d[i, a] = din(f"wu{i}{a}", [11, 128, 8, 256])
            wd_d[i, a] = din(f"wd{i}{a}", [4, 128, NFC, 256])
    wup_d = [din(f"wup{j}", [16, 128, 8, 256]) for j in range(2)]
    wdn_d = [din(f"wdn{j}", [8, 128, 16, 128]) for j in range(2)]
    bd_d = [din(f"bd{j}", [128, 3, 16, 128]) for j in range(2)]
    wgate_d = [din(f"wgate{j}", [128, 48, 8]) for j in range(2)]
    bgate_d = [din(f"bgate{j}", [128, 8]) for j in range(2)]
    win_d = [din(f"win{j}", [8, 128, 8, 256]) for j in range(2)]
    wout_d = [din(f"wout{j}", [8, 128, 8, 128]) for j in range(2)]
    wsP_d = [din(f"wsP{j}", [128, 4, 128]) for j in range(2)]
    wsS_d = [din(f"wsS{j}", [64, 4, 64]) for j in range(2)]
    bsP_d = [din(f"bsP{j}", [128, 4, 128]) for j in range(2)]
    bsS_d = [din(f"bsS{j}", [128, 4, 64]) for j in range(2)]
    sC_d = din("sC", [2, NB, H, DH, DH])
    snT_d = din("snT", [2, 128, 16, NB])
    sm_d = din("sm", [2, TS, 4])
    sconv_d = din("sconvT", [2, 128, 16, NB, 3])

    yT_o = dout("yT", [D, TT])
    Cp_o = dout("Cp", [2, H, DH, DH])
    npT_o = dout("npT", [2, 128, 16, 1])
    mp_o = dout("mp", [2, 128, 4])
    convp_o = dout("convpT", [2, 128, 16, 3])
    Cs_o = dout("Cs", [2, NB, H, DH, DH])
    nsT_o = dout("nsT", [2, 128, 16, NB])
    ms_o = dout("ms", [2, TS, 4])
    convs_o = dout("convsT", [2, 128, 16, NB, 3])
    vsT_o = dout("vsT", [2, 128, 8, TS])

    x = P.sbuf([128, 8, TT], F32, "x")
    vecs = P.sbuf([128, VL["_n"]], F32, "vecs")
    consts = P.sbuf([128, CL["_n"]], F32, "consts")
    cbf = P.sbuf([128, 512], BF16, "cbf")
    wA = [P.sbuf([128, 8, 256], BF16, f"wA{i}") for i in range(4)]
    wB = [P.sbuf([128, 2048], BF16, f"wB{i}") for i in range(2)]
    banks = [P.psum([128, 512], F32, f"bank{i}") for i in range(8)]

    def V(name, k=0, p=128):
        c = VL[name] + k
        return vecs.t[0:p, c:c + 1]

    def C(name, p, n):
        c = CL[name]
        return consts.t[0:p, c:c + n]

    P.dma("sp", vecs.t[:], vecs_d, writes=[vecs.r])
    P.dma("sp", consts.t[:], consts_d, writes=[consts.r])
    xv = xT_d.rearrange("(k p) t -> p k t", p=128)
    for ti, (t0, n) in enumerate(FFN_TILES):
        P.dma("sp", x.t[:, :, t0:t0 + n], xv[:, :, t0:t0 + n], writes=[x[ti]])
    P.op("dve", lambda e: e.tensor_copy(out=cbf.t[:, 0:128], in_=consts.t[:, CL["ident"]:CL["ident"] + 128]), [consts.r], [cbf.r])
    P.op("dve", lambda e: e.tensor_copy(out=cbf.t[:, 128:256], in_=consts.t[:, CL["onesP"]:CL["onesP"] + 128]), [consts.r], [cbf.r])
    P.op("dve", lambda e: e.tensor_copy(out=cbf.t[:, 256:272], in_=consts.t[:, CL["seqS"]:CL["seqS"] + 16]), [consts.r], [cbf.r])
    P.op("dve", lambda e: e.tensor_copy(out=cbf.t[:, 272:273], in_=consts.t[:, CL["seqP"]:CL["seqP"] + 1]), [consts.r], [cbf.r])
    identb = cbf.t[:, 0:128]
    onesb = cbf.t[:, 128:256]

    def xres(t0, n):
        out = []
        for ti, (a, m) in enumerate(FFN_TILES):
            if a < t0 + n and t0 < a + m:
                out.append(x[ti])
        return out

    import os
    dbg = os.environ.get("KDBG", "")
    dbg_stage = None
    wA_i = [0]
    wB_i = [0]

    def next_wA():
        t = wA[wA_i[0] % len(wA)]
        wA_i[0] += 1
        return t

    def next_wB():
        t = wB[wB_i[0] % len(wB)]
        wB_i[0] += 1
        return t

    def rmsnorm(t0, n, gname, dst, dst_res, sqb, rst, bank, f32_out=False):
        xr = xres(t0, n)
        P.op("act", lambda e: e.activation(out=sqb.t[:, :, 0:n], in_=x.t[:, :, t0:t0 + n], func=AF.Square), xr, [sqb.r])

        def mm(e):
            for k in range(8):
                ins = e.matmul(bank.t[:, 0:n], onesb, sqb.t[:, k, 0:n], start=(k == 0), stop=(k == 7))
            return ins
        P.op("pe", mm, [sqb.r, cbf.r], [bank.r])
        P.op("act", lambda e: e.activation(out=rst.t[:, 0:n], in_=bank.t[:, 0:n], func=AF.Sqrt, scale=1.0 / D, bias=V("c_eps")), [bank.r, vecs.r], [rst.r])
        P.op("dve", lambda e: e.reciprocal(out=rst.t[:, 0:n], in_=rst.t[:, 0:n]), [rst.r], [rst.r])
        for k in range(8):
            P.op("dve", lambda e, k=k: e.scalar_tensor_tensor(out=dst.t[:, k, 0:n], in0=x.t[:, k, t0:t0 + n], scalar=V(gname, k),
                                                                in1=rst.t[:, 0:n], op0=ALU.mult, op1=ALU.mult),
                 xr + [rst.r, vecs.r], [dst_res])

    def ffn(i, a):
        P.phase()
        gname = f"norm_ff{a}_{i}"
        xn = P.atile([128, 8, TT], BF16)
        hbuf = P.atile([128, 8, TT], BF16)
        sqb = P.atile([128, 8, 512], BF16)
        rst = [P.atile([128, 512], F32) for _ in range(2)]
        sg = [P.atile([128, 512], F32) for _ in range(2)]
        for ti, (t0, n) in enumerate(FFN_TILES):
            xn_v = Tile(xn.t[:, :, t0:t0 + n])
            rmsnorm(t0, n, gname, xn_v, xn[ti], sqb, rst[ti % 2], banks[6])
        cnt = 0
        for (b0, b1) in FFN_PARTS:
            for blk in range(b0, b1):
                wgt, wut = next_wA(), next_wA()
                P.dma("pool", wgt.t[:], wg_d[i, a][blk], writes=[wgt.r])
                P.dma("pool", wut.t[:], wu_d[i, a][blk], writes=[wut.r])
                for fc in range(2):
                    fl = (blk - b0) * 2 + fc
                    for ti, (t0, n) in enumerate(FFN_TILES):
                        bg, bu = banks[cnt % 2], banks[2 + cnt % 2]
                        sgt = sg[cnt % 2]
                        cnt += 1

                        def mm(e, wt=wgt, bk=bg, fc=fc, t0=t0, n=n):
                            for k in range(8):
                                ins = e.matmul(bk.t[:, 0:n], wt.t[:, k, fc * 128:(fc + 1) * 128], xn.t[:, k, t0:t0 + n], start=(k == 0), stop=(k == 7))
                            return ins
                        P.op("pe", mm, [wgt.r, xn[ti]], [bg.r])

                        def mm2(e, wt=wut, bk=bu, fc=fc, t0=t0, n=n):
                            for k in range(8):
                                ins = e.matmul(bk.t[:, 0:n], wt.t[:, k, fc * 128:(fc + 1) * 128], xn.t[:, k, t0:t0 + n], start=(k == 0), stop=(k == 7))
                            return ins
                        P.op("pe", mm2, [wut.r, xn[ti]], [bu.r])
                        P.op("act", lambda e, bk=bg, s=sgt, n=n: e.activation(out=s.t[:, 0:n], in_=bk.t[:, 0:n], func=AF.Silu), [bg.r], [sgt.r])
                        P.op("dve", lambda e, bk=bu, s=sgt, fl=fl, t0=t0, n=n: e.tensor_tensor(out=hbuf.t[:, fl, t0:t0 + n], in0=s.t[:, 0:n], in1=bk.t[:, 0:n], op=ALU.mult),
                             [bu.r, sgt.r], [hbuf[ti]])
            nfc = (b1 - b0) * 2
            for dp in range(4):
                wdt = next_wB()
                wv = wdt.t[:, 0:nfc * 256].rearrange("p (f c) -> p f c", f=nfc)
                P.dma("pool", wv, wd_d[i, a][dp, :, b0 * 2:b1 * 2, :], writes=[wdt.r])
                for dc in range(2):
                    d = dp * 2 + dc
                    for ti, (t0, n) in enumerate(FFN_TILES):
                        bk = banks[4 + cnt % 2]
                        cnt += 1

                        def mm(e, wv=wv, bk=bk, dc=dc, t0=t0, n=n, nfc=nfc):
                            for f in range(nfc):
                                ins = e.matmul(bk.t[:, 0:n], wv[:, f, dc * 128:(dc + 1) * 128], hbuf.t[:, f, t0:t0 + n], start=(f == 0), stop=(f == nfc - 1))
                            return ins
                        P.op("pe", mm, [wdt.r, hbuf[ti]], [bk.r])
                        P.op("dve", lambda e, bk=bk, d=d, t0=t0, n=n: e.scalar_tensor_tensor(out=x.t[:, d, t0:t0 + n], in0=bk.t[:, 0:n], scalar=0.5,
                                                                                         in1=x.t[:, d, t0:t0 + n], op0=ALU.mult, op1=ALU.add),
                             [bk.r, x[ti]], [x[ti]])

    def mlstm(i, j, samp):
        P.phase()
        gname = f"norm_mix_{i}"
        T = TS if samp else ML_T
        nseq, L, Pn, nch = (NB, 4, TS, 1) if samp else (1, 128, 128, 2)
        csn, csL = (NB, 4) if samp else (1, T)
        t0s = [TP] if samp else list(range(0, TP, T))
        NS4 = nch * nseq * 4
        bd = P.atile([128, 3, 16, 128], BF16)
        wgate = P.atile([128, 48, 8], BF16)
        bgate = P.atile([128, 8], F32)
        xn = P.atile([128, 8, T], BF16)
        xmh = P.atile([128, 16, csn, 3 + csL], BF16)
        xc = P.atile([128, 16, T], BF16)
        acc = [P.atile([128, T], F32) for _ in range(4)]
        qT = P.atile([128, 16, T], BF16)
        kT = P.atile([128, 16, T], BF16)
        sqb = Tile(kT.t[:, 0:8, :], kT.base_lw)
        sqb._res = kT._res
        vTt = [P.atile([128, T], BF16) for _ in range(2)]
        v_tk = [P.atile([128, 512], BF16) for _ in range(2)]
        kp_tk = [P.atile([128, 512], BF16) for _ in range(2)]
        Cbf = [P.atile([128, 4, 512], BF16) for _ in range(2)]
        nst = P.atile([128, 16, nseq], F32)
        nbf = P.atile([128, 16, nseq], BF16)
        ST = [P.atile([128, 128], BF16) for _ in range(4)]
        hn_tok = P.atile([128, nch, 2048], BF16)
        ea = [P.atile([128, T], F32) for _ in range(2)]
        rst = ea[0]
        sz = acc
        gT_sb = Tile(ea[1].t[0:8, :], ea[1].base_lw)
        gT_sb._res = ea[1]._res
        gg = P.atile([128, nch, 8], F32)
        e1 = P.atile([128, nch, 4], F32)
        lf = P.atile([128, nch, 4], F32)
        bt = P.atile([128, nch, 4], F32)
        bl = P.atile([128, nch, 4], F32)
        aa = P.atile([128, nch, 16], F32)
        RR = P.atile([128, nch, 4], F32)
        mstk = [P.atile([128, nch + 1, 4], F32) for _ in range(2)]
        d1 = P.atile([128, nch, 4], F32)
        es = P.atile([128, nch, 4], F32)
        d2 = P.atile([128, nch, 4], F32)
        c0 = P.atile([128, nch, 4], F32)
        d3 = P.atile([128, nch, 4], F32)
        dfl = P.atile([128, nch, 4], F32)
        amaxT = P.atile([4, nch * nseq], F32)
        amax_exp = P.atile([4, nch * Pn], F32)
        rhsx = P.atile([128, NS4], F32)
        c0bc = P.atile([128, nch, nseq, 4], F32)
        tmpn = P.atile([128, 4, nseq], F32)
        tmpn2 = P.atile([128, 4, nseq], F32)
        nqb = P.atile([128, 4], F32)
        nq = P.atile([128, 3, 4], F32)
        den = P.atile([128, 4], F32)
        t1 = P.atile([128, 4], F32)
        rs = P.atile([128, 4], F32)
        nb = P.atile([128, 4], F32)
        mv = P.atile([128, 4, 2], F32)
        st6 = [P.atile([128, 6], F32) for _ in range(4)]
        oT = qT
        if samp:
            xmc = P.atile([128, 16, TS], BF16)
            tails = P.atile([128, 16, NB, 3], F32)
            colmask = P.atile([128, NB, TS], BF16)
            qm = [P.atile([128, 4, TS], BF16) for _ in range(2)]
            kpm = [P.atile([64, 512], BF16) for _ in range(2)]
            nslot = min(12, (P.arena_words - P.arena_ptr) // 2048)
            assert nslot >= 4, nslot
            Cslots = [P.atile([128, 4, 512], F32) for _ in range(nslot)]
            P.dma("pool", colmask.t[:], colmask_d, writes=[colmask.r])
        else:
            tailp = P.atile([128, 16, 3], F32)
            Cst = P.atile([128, 16, 512], F32)
            P.op("pool", lambda e: e.memset(Cst.t[:], 0.0), [], [Cst[h_] for h_ in range(4)])

        P.dma("pool", bd.t[:], bd_d[j], writes=[bd.r])
        P.dma("pool", wgate.t[:], wgate_d[j], writes=[wgate.r])
        P.dma("sp", bgate.t[:], bgate_d[j], writes=[bgate.r])
        if samp:
            P.dma("sp", nst.t[:], snT_d[j], writes=[nst[h_] for h_ in range(4)])
            P.dma("sp", mstk[1].t[0:TS, nch, :], sm_d[j], writes=[mstk[1].r])
            P.dma("sp", tails.t[:], sconv_d[j], writes=[tails.r])
            P.op("dve", lambda e: e.tensor_copy(out=xmh.t[:, :, :, 0:3], in_=tails.t[:]), [tails.r], [xmh.r] + [xmh[g_] for g_ in range(4)])
        else:
            P.op("pool", lambda e: e.memset(nst.t[:], 0.0), [], [nst[h_] for h_ in range(4)])
            P.op("pool", lambda e: e.memset(mstk[1].t[:], 0.0), [], [mstk[1].r])
            P.op("pool", lambda e: e.memset(xmh.t[:, :, :, 0:3], 0.0), [], [xmh.r] + [xmh[g_] for g_ in range(4)])
        mask = C("maskS" if samp else "maskP", Pn, Pn)
        onesblk = C("onesS" if samp else "onesP", Pn, Pn)
        seqm = C("seqS" if samp else "seqP", Pn, nseq)
        firstm = C("firstS" if samp else "firstP", Pn, nseq)
        seqmb = cbf.t[0:Pn, 256:256 + nseq] if samp else cbf.t[0:Pn, 272:273]
        cnt = [0]
        slot_i = [0]

        def xm_op(fo, a=0, b=None):
            b = T if b is None else b
            if samp:
                return xmc.t[:, fo, a:b]
            return xmh.t[:, fo, 0, 3 + a:3 + b]

        for tidx, t0 in enumerate(t0s):
            last_prompt = (not samp) and (t0 + T == TP)
            mcur, mprev = mstk[tidx % 2], mstk[1 - tidx % 2]
            rmsnorm(t0, T, gname, xn, xn.r, sqb, rst, banks[7])
            P.op("dve", lambda e: e.tensor_copy(out=mcur.t[0:Pn, 0, :], in_=mprev.t[0:Pn, nch, :]), [mprev.r], [mcur.r])

            wts = {}

            def B1(g):
                for fo in range(4 * g, 4 * g + 4):
                    if fo % 2 == 0:
                        wt = next_wA()
                        P.dma("pool", wt.t[:], wup_d[j][fo // 2], writes=[wt.r])
                        wts[fo // 2] = wt
                    wt = wts[fo // 2]
                    fc = fo % 2
                    bx = banks[fo % 2]

                    def mm(e, wt=wt, bx=bx, fc=fc):
                        for k in range(8):
                            ins = e.matmul(bx.t[:, 0:T], wt.t[:, k, fc * 128:(fc + 1) * 128], xn.t[:, k, 0:T], start=(k == 0), stop=(k == 7))
                        return ins
                    P.op("pe", mm, [wt.r, xn.r], [bx.r])
                    bx3 = bx.t[:, 0:T].rearrange("p (s l) -> p s l", s=csn)
                    P.op("act", lambda e, bx3=bx3, fo=fo: e.activation(out=xmh.t[:, fo, :, 3:3 + csL], in_=bx3, func=AF.Copy), [bx.r], [xmh[fo // 4]])
                    if samp:
                        P.op("act", lambda e, bx=bx, fo=fo: e.activation(out=xmc.t[:, fo, :], in_=bx.t[:, 0:T], func=AF.Copy), [bx.r], [xmc.r])
                        P.op("dve", lambda e, bx3=bx3, fo=fo: e.tensor_copy(out=tails.t[:, fo, :, :], in_=bx3[:, :, 1:4]), [bx.r], [tails.r])
                    elif last_prompt:
                        P.op("dve", lambda e, bx=bx, fo=fo: e.tensor_copy(out=tailp.t[:, fo, :], in_=bx.t[:, T - 3:T]), [bx.r], [tailp.r])

            def B2(g):
                fos = range(4 * g, 4 * g + 4)
                for w in range(4):
                    for fo in fos:
                        at = acc[fo % 4]
                        av = at.t[:, 0:T].rearrange("p (s l) -> p s l", s=csn)
                        win = xmh.t[:, fo, :, w:w + csL]
                        if w == 0:
                            P.op("dve", lambda e, av=av, win=win, fo=fo: e.tensor_scalar(out=av, in0=win, scalar1=V(f"ml_wconv_{j}_0", fo), scalar2=V(f"ml_bconv_{j}", fo),
                                                                                         op0=ALU.mult, op1=ALU.add), [xmh[fo // 4], xmh.r, vecs.r], [at.r])
                        else:
                            P.op("dve", lambda e, av=av, win=win, fo=fo, w=w: e.scalar_tensor_tensor(out=av, in0=win, scalar=V(f"ml_wconv_{j}_{w}", fo), in1=av,
                                                                                                   op0=ALU.mult, op1=ALU.add), [xmh[fo // 4], xmh.r, at.r, vecs.r], [at.r])
                for fo in fos:
                    at = acc[fo % 4]
                    P.op("act", lambda e, at=at, fo=fo: e.activation(out=xc.t[:, fo, 0:T], in_=at.t[:, 0:T], func=AF.Silu), [at.r], [xc[fo // 4]])

            def B3(g):
                for fo in range(4 * g, 4 * g + 4):
                    bA, bK, bB = (banks[2], banks[3], banks[4]) if fo % 2 == 0 else (banks[5], banks[7], banks[1])
                    P.op("pe", lambda e, fo=fo, bA=bA: e.matmul(bA.t[:, 0:T], bd.t[:, 0, fo, :], xc.t[:, fo, 0:T], start=True, stop=True), [bd.r, xc[fo // 4]], [bA.r])
                    P.op("pe", lambda e, fo=fo, bK=bK: e.matmul(bK.t[:, 0:T], bd.t[:, 1, fo, :], xc.t[:, fo, 0:T], start=True, stop=True), [bd.r, xc[fo // 4]], [bK.r])
                    P.op("pe", lambda e, fo=fo, bB=bB: e.matmul(bB.t[:, 0:T], bd.t[:, 2, fo, :], xm_op(fo), start=True, stop=True),
                         [bd.r, xmh[fo // 4]] + ([xmc.r] if samp else []), [bB.r])
                    vt = vTt[fo % 2]
                    P.op("act", lambda e, fo=fo, bA=bA: e.activation(out=qT.t[:, fo, 0:T], in_=bA.t[:, 0:T], func=AF.Copy), [bA.r], [qT.r])
                    P.op("dve", lambda e, fo=fo, bK=bK: e.tensor_copy(out=kT.t[:, fo, 0:T], in_=bK.t[:, 0:T]), [bK.r], [kT.r])
                    P.op("act", lambda e, vt=vt, bB=bB: e.activation(out=vt.t[:, 0:T], in_=bB.t[:, 0:T], func=AF.Copy), [bB.r], [vt.r])
                    bg = banks[6]
                    for which, (src, sres) in enumerate([(qT.t[:, fo, 0:T], qT.r), (kT.t[:, fo, 0:T], kT.r), (vt.t[:, 0:T], vt.r)]):
                        first = (fo == 0 and which == 0)
                        lastm = (fo == 15 and which == 2)
                        P.op("pe", lambda e, src=src, which=which, fo=fo, first=first, lastm=lastm: e.matmul(bg.t[0:8, 0:T], wgate.t[:, which * 16 + fo, :], src, start=first, stop=lastm),
                             [wgate.r, sres], [bg.r])

            kstop = int(os.environ.get("KSTOP", "0"))
            if kstop == 11:
                return
            if samp:
                P.barrier_all()
            B1(0)
            B1(1)
            if kstop == 12:
                return
            B2(0)
            if kstop == 13:
                return
            B3(0)
            if kstop == 14:
                return
            B1(2)
            B2(1)
            B3(1)
            B1(3)
            B2(2)
            B3(2)
            B2(3)
            B3(3)
            P.op("act", lambda e: e.activation(out=gT_sb.t[:, 0:T], in_=banks[6].t[0:8, 0:T], func=AF.Copy), [banks[6].r], [gT_sb.r])
            kstop = int(os.environ.get("KSTOP", "0"))
            if kstop == 1:
                return
            if samp:
                P.barrier_all()

            bS = banks[4]

            def A2(tl, a=0, b=4):
                return tl.t[0:Pn, :, a:b]

            def trg(e):
                for c in range(nch):
                    ins = e.transpose(out=bS.t[0:Pn, c * 16:c * 16 + 8], in_=gT_sb.t[0:8, c * Pn:(c + 1) * Pn], identity=C("ident", 8, 8))
                return ins
            P.op("pe", trg, [gT_sb.r, consts.r], [bS.r])
            P.op("dve", lambda e: e.tensor_tensor(out=A2(gg, 0, 8), in0=bS.t[0:Pn, 0:nch * 16].rearrange("p (c g) -> p c g", c=nch)[:, :, 0:8],
                                                   in1=bgate.t[0:Pn, 0:8].unsqueeze(1).to_broadcast([Pn, nch, 8]), op=ALU.add), [bS.r, bgate.r], [gg.r])
            P.op("act", lambda e: e.activation(out=A2(e1), in_=A2(gg, 4, 8), func=AF.Exp, scale=-1.0), [gg.r], [e1.r])
            P.op("act", lambda e: e.activation(out=A2(lf), in_=A2(e1), func=AF.Ln, bias=1.0), [e1.r], [lf.r])
            P.op("dve", lambda e: e.tensor_scalar(out=A2(lf), in0=A2(lf), scalar1=-1.0, scalar2=None, op0=ALU.mult), [lf.r], [lf.r])

            def mmcs(e):
                lfv = lf.t[0:Pn, :, :].rearrange("p c h -> p (c h)")
                e.matmul(bS.t[0:Pn, 32:32 + nch * 4], mask, lfv, start=True, stop=True)
                return e.matmul(bS.t[0:Pn, 48:48 + nch * 4], onesblk, lfv, start=True, stop=True)
            P.op("pe", mmcs, [consts.r, lf.r], [bS.r])
            P.op("dve", lambda e: e.tensor_copy(out=A2(bt), in_=bS.t[0:Pn, 32:32 + nch * 4].rearrange("p (c h) -> p c h", c=nch)), [bS.r], [bt.r])
            P.op("dve", lambda e: e.tensor_copy(out=A2(bl), in_=bS.t[0:Pn, 48:48 + nch * 4].rearrange("p (c h) -> p c h", c=nch)), [bS.r], [bl.r])
            P.op("dve", lambda e: e.tensor_tensor(out=A2(aa), in0=A2(gg, 0, 4), in1=A2(bt), op=ALU.subtract), [gg.r, bt.r], [aa.r])

            def tra(e):
                for c in range(nch):
                    ins = e.transpose(out=bS.t[0:4, 64 + c * Pn:64 + (c + 1) * Pn], in_=aa.t[0:Pn, c, 0:4], identity=C("ident", Pn, Pn))
                return ins
            P.op("pe", tra, [aa.r, consts.r], [bS.r])
            P.op("dve", lambda e: e.tensor_reduce(out=amaxT.t[0:4, :], in_=bS.t[0:4, 64:64 + nch * Pn].rearrange("p (s l) -> p s l", l=L), axis=AX.X, op=ALU.max),
                 [bS.r], [amaxT.r])
            P.op("dve", lambda e: e.tensor_copy(out=amax_exp.t[0:4, :].rearrange("p (s l) -> p s l", l=L),
                                                 in_=amaxT.t[0:4, :].unsqueeze(2).to_broadcast([4, nch * nseq, L])), [amaxT.r], [amax_exp.r])

            def mmbc(e):
                for c in range(nch):
                    ins = e.matmul(bS.t[0:Pn, 384 + c * 16:388 + c * 16], amax_exp.t[0:4, c * Pn:(c + 1) * Pn], C("ident", 4, 4), start=True, stop=True)
                return ins
            P.op("pe", mmbc, [amax_exp.r, consts.r], [bS.r])
            for c in range(nch):
                P.op("dve", lambda e, c=c: e.tensor_tensor(out=RR.t[0:Pn, c, :], in0=bS.t[0:Pn, 384 + c * 16:388 + c * 16], in1=mcur.t[0:Pn, c, :], op=ALU.max), [bS.r, mcur.r], [RR.r])
                P.op("dve", lambda e, c=c: e.tensor_tensor(out=mcur.t[0:Pn, c + 1, :], in0=bl.t[0:Pn, c, :], in1=RR.t[0:Pn, c, :], op=ALU.add), [bl.r, RR.r], [mcur.r])
            P.op("dve", lambda e: e.tensor_tensor(out=A2(d1), in0=A2(aa), in1=A2(RR), op=ALU.subtract), [aa.r, RR.r], [d1.r])
            P.op("act", lambda e: e.activation(out=A2(es), in_=A2(d1), func=AF.Exp, bias=V("c_lnscale", 0, Pn)), [d1.r, vecs.r], [es.r])
            P.op("dve", lambda e: e.tensor_tensor(out=A2(d2), in0=mcur.t[0:Pn, 0:nch, :], in1=A2(RR), op=ALU.subtract), [mcur.r, RR.r], [d2.r])
            P.op("act", lambda e: e.activation(out=A2(c0), in_=A2(d2), func=AF.Exp), [d2.r], [c0.r])
            P.op("dve", lambda e: e.tensor_tensor(out=A2(d3), in0=A2(bt), in1=A2(RR), op=ALU.add), [bt.r, RR.r], [d3.r])
            P.op("act", lambda e: e.activation(out=A2(dfl), in_=A2(d3), func=AF.Exp, scale=-1.0), [d3.r], [dfl.r])
            for c in range(nch):
                P.op("dve", lambda e, c=c: e.tensor_tensor(out=rhsx.t[0:Pn, c * nseq * 4:(c + 1) * nseq * 4].rearrange("p (s h) -> p s h", s=nseq),
                                                           in0=c0.t[0:Pn, c, :].unsqueeze(1).to_broadcast([Pn, nseq, 4]),
                                                           in1=firstm.unsqueeze(2).to_broadcast([Pn, nseq, 4]), op=ALU.mult), [c0.r, consts.r], [rhsx.r])
            P.op("pe", lambda e: e.matmul(bS.t[:, 448:448 + NS4], C("onesP", Pn, 128), rhsx.t[0:Pn, 0:NS4], start=True, stop=True), [rhsx.r, consts.r], [bS.r])
            P.op("dve", lambda e: e.tensor_copy(out=c0bc.t[:].rearrange("p c s h -> p (c s h)"), in_=bS.t[:, 448:448 + NS4]), [bS.r], [c0bc.r])

            if kstop == 2:
                return
            if samp:
                P.barrier_all()
            bV2 = banks[4]
            bSc = banks[2]
            bNs = [banks[3], banks[6], banks[5], banks[1]]
            bcus = [banks[0], banks[7]]
            for c in range(nch):
                cs = slice(c * Pn, (c + 1) * Pn)

                def ES(hh, c=c):
                    return es.t[0:Pn, c, hh:hh + 1]

                def F(hh, c=c, cs=cs):
                    v_tok, kp_tok, stt = v_tk[hh % 2], kp_tk[hh % 2], ST[hh]

                    def mmv(e):
                        for fl in range(4):
                            fo = hh * 4 + fl
                            ins = e.matmul(bV2.t[0:Pn, fl * 128:(fl + 1) * 128], xm_op(fo, cs.start, cs.stop), bd.t[:, 2, fo, :], start=True, stop=True)
                        return ins
                    P.op("pe", mmv, [xmh[hh], bd.r] + ([xmc.r] if samp else []), [bV2.r])
                    P.op("act", lambda e: e.activation(out=v_tok.t[0:Pn, :], in_=bV2.t[0:Pn, :], func=AF.Copy), [bV2.r], [v_tok.r])

                    def mmk(e):
                        for fl in range(4):
                            fo = hh * 4 + fl
                            ins = e.matmul(bV2.t[0:Pn, fl * 128:(fl + 1) * 128], xc.t[:, fo, cs], bd.t[:, 1, fo, :], start=True, stop=True)
                        return ins
                    P.op("pe", mmk, [xc[hh], bd.r], [bV2.r])
                    P.op("act", lambda e: e.activation(out=kp_tok.t[0:Pn, :], in_=bV2.t[0:Pn, :], func=AF.Copy, scale=ES(hh)), [bV2.r, es.r], [kp_tok.r])

                    def mms(e):
                        for dc in range(4):
                            ins = e.matmul(bSc.t[0:Pn, 0:Pn], kT.t[:, hh * 4 + dc, cs], qT.t[:, hh * 4 + dc, cs], start=(dc == 0), stop=(dc == 3))
                        return ins
                    P.op("pe", mms, [kT.r, qT.r], [bSc.r])
                    P.op("dve", lambda e: e.scalar_tensor_tensor(out=stt.t[0:Pn, 0:Pn], in0=bSc.t[0:Pn, 0:Pn], scalar=ES(hh), in1=mask, op0=ALU.mult, op1=ALU.mult),
                         [bSc.r, es.r, consts.r], [stt.r])
                    P.op("dve", lambda e: e.tensor_tensor(out=nbf.t[:, hh * 4:(hh + 1) * 4, :], in0=nst.t[:, hh * 4:(hh + 1) * 4, :],
                                                          in1=c0bc.t[:, c, :, hh].unsqueeze(1).to_broadcast([128, 4, nseq]), op=ALU.mult), [nst[hh], c0bc.r], [nbf[hh]])
                    if not samp:
                        cb = Cbf[hh % 2]
                        P.op("act", lambda e: e.activation(out=cb.t[:], in_=Cst.t[:, hh * 4:(hh + 1) * 4, :], func=AF.Copy, scale=c0bc.t[:, c, 0, hh:hh + 1]),
                             [Cst[hh], c0bc.r], [cb.r])

                def NQ(hh, cs=cs):
                    stt = ST[hh]

                    def mmq(e):
                        e.matmul(bSc.t[0:Pn, 128 + hh:129 + hh], stt.t[0:Pn, 0:Pn], onesb[0:Pn, 0:1], start=True, stop=True)
                        for dc in range(4):
                            ins = e.matmul(bSc.t[0:Pn, 160 + hh * 16:160 + hh * 16 + nseq], qT.t[:, hh * 4 + dc, cs], nbf.t[:, hh * 4 + dc, :], start=(dc == 0), stop=(dc == 3))
                        return ins
                    P.op("pe", mmq, [stt.r, cbf.r, qT.r, nbf[hh]], [bSc.r])

                def NU_prompt(hh, c=c, cs=cs):
                    v_tok, kp_tok, stt, cb, bN = v_tk[hh % 2], kp_tk[hh % 2], ST[hh], Cbf[hh % 2], bNs[hh]

                    def mmn(e):
                        e.matmul(bN.t[0:Pn, :], stt.t[0:Pn, 0:Pn], v_tok.t[0:Pn, :], start=True, stop=False)
                        for dc in range(4):
                            ins = e.matmul(bN.t[0:Pn, :], qT.t[:, hh * 4 + dc, cs], cb.t[:, dc, :], start=False, stop=(dc == 3))
                        return ins
                    P.op("pe", mmn, [stt.r, v_tok.r, qT.r, cb.r], [bN.r])
                    NQ(hh)
                    for dc in range(4):
                        bcu = bcus[cnt[0] % 2]
                        cnt[0] += 1
                        P.op("pe", lambda e, bcu=bcu, dc=dc: e.matmul(bcu.t[:, :], kp_tok.t[0:Pn, dc * 128:(dc + 1) * 128], v_tok.t[0:Pn, :], start=True, stop=True),
                             [kp_tok.r, v_tok.r], [bcu.r])
                        P.op("dve", lambda e, bcu=bcu, dc=dc: e.scalar_tensor_tensor(out=Cst.t[:, hh * 4 + dc, :], in0=Cst.t[:, hh * 4 + dc, :], scalar=c0bc.t[:, c, 0, hh:hh + 1],
                                                                                   in1=bcu.t[:, :], op0=ALU.mult, op1=ALU.add), [bcu.r, Cst[hh], c0bc.r], [Cst[hh]])

                def NU_samp(hh, c=c, cs=cs):
                    v_tok, kp_tok, stt, bN = v_tk[hh % 2], kp_tk[hh % 2], ST[hh], bNs[hh]
                    P.op("pe", lambda e: e.matmul(bN.t[0:Pn, :], stt.t[0:Pn, 0:Pn], v_tok.t[0:Pn, :], start=True, stop=False), [stt.r, v_tok.r], [bN.r])
                    for b in range(NB):
                        idx = hh * NB + b
                        ensure_loaded(idx + len(Cslots) - 2)
                        sl = Cslots[idx % len(Cslots)]
                        cb = Cbf[b % 2]
                        P.op("act", lambda e, cb=cb, sl=sl, b=b: e.activation(out=cb.t[:], in_=sl.t[:], func=AF.Copy, scale=c0bc.t[:, c, b, hh:hh + 1]), [sl.r, c0bc.r], [cb.r])
                        qmt = qm[b % 2]
                        P.op("dve", lambda e, qmt=qmt, b=b: e.tensor_tensor(out=qmt.t[:], in0=qT.t[:, hh * 4:(hh + 1) * 4, 0:TS],
                                                                            in1=colmask.t[:, b, :].unsqueeze(1).to_broadcast([128, 4, TS]), op=ALU.mult), [qT.r, colmask.r], [qmt.r])

                        def mmc(e, qmt=qmt, cb=cb, b=b):
                            for dc in range(4):
                                ins = e.matmul(bN.t[0:Pn, :], qmt.t[:, dc, :], cb.t[:, dc, :], start=False, stop=(b == NB - 1 and dc == 3))
                            return ins
                        P.op("pe", mmc, [qmt.r, cb.r], [bN.r])
                        kpt = kpm[b % 2]
                        P.op("dve", lambda e, kpt=kpt, b=b: e.tensor_scalar(out=kpt.t[0:Pn, :], in0=kp_tok.t[0:Pn, :], scalar1=consts.t[0:Pn, CL["seqS"] + b:CL["seqS"] + b + 1],
                                                                            scalar2=None, op0=ALU.mult), [kp_tok.r, consts.r], [kpt.r])
                        for dc in range(4):
                            bcu = bcus[cnt[0] % 2]
                            cnt[0] += 1
                            P.op("pe", lambda e, bcu=bcu, kpt=kpt, dc=dc: e.matmul(bcu.t[:, :], kpt.t[0:Pn, dc * 128:(dc + 1) * 128], v_tok.t[0:Pn, :], start=True, stop=True),
                                 [kpt.r, v_tok.r], [bcu.r])
                            P.op("dve", lambda e, bcu=bcu, dc=dc, b=b, sl=sl: e.scalar_tensor_tensor(out=sl.t[:, dc, :], in0=sl.t[:, dc, :], scalar=c0bc.t[:, c, b, hh:hh + 1],
                                                                                                   in1=bcu.t[:, :], op0=ALU.mult, op1=ALU.add), [bcu.r, sl.r, c0bc.r], [sl.r])
                        P.dma("sp", Cs_o[j, b, hh].rearrange("(dc p) e -> p dc e", p=128), sl.t[:], reads=[sl.r])
                    NQ(hh)

                def NUPD(hh, c=c):
                    kp_tok = kp_tk[hh % 2]

                    def mmnn(e):
                        for dc in range(4):
                            ins = e.matmul(bSc.t[:, 256 + hh * 64 + dc * 16:256 + hh * 64 + dc * 16 + nseq], kp_tok.t[0:Pn, dc * 128:(dc + 1) * 128], seqmb, start=True, stop=True)
                        return ins
                    P.op("pe", mmnn, [kp_tok.r, cbf.r], [bSc.r])
                    P.op("dve", lambda e: e.tensor_tensor(out=tmpn2.t[:], in0=nst.t[:, hh * 4:(hh + 1) * 4, :],
                                                          in1=c0bc.t[:, c, :, hh].unsqueeze(1).to_broadcast([128, 4, nseq]), op=ALU.mult), [nst[hh], c0bc.r], [tmpn2.r])
                    P.op("dve", lambda e: e.tensor_tensor(out=nst.t[:, hh * 4:(hh + 1) * 4, :], in0=tmpn2.t[:],
                                                          in1=bSc.t[:, 256 + hh * 64:256 + hh * 64 + 64].rearrange("p (d s) -> p d s", d=4)[:, :, 0:nseq], op=ALU.add),
                         [tmpn2.r, bSc.r], [nst[hh]])

                ld_issued = [0]

                def ensure_loaded(upto):
                    while ld_issued[0] <= min(upto, 4 * NB - 1):
                        k = ld_issued[0]
                        P.dma("sp", Cslots[k % len(Cslots)].t[:], sC_d[j, k % NB, k // NB].rearrange("(dc p) e -> p dc e", p=128), writes=[Cslots[k % len(Cslots)].r])
                        ld_issued[0] += 1

                NU = NU_samp if samp else NU_prompt
                F(0)
                F(1)
                NU(0)
                NUPD(0)
                F(2)
                NU(1)
                NUPD(1)
                F(3)
                NU(2)
                NUPD(2)
                NU(3)
                NUPD(3)
                P.op("dve", lambda e: e.tensor_tensor(out=tmpn.t[0:Pn], in0=bSc.t[0:Pn, 160:224].rearrange("p (h s) -> p h s", h=4)[:, :, 0:nseq],
                                                      in1=seqm.unsqueeze(1).to_broadcast([Pn, 4, nseq]), op=ALU.mult), [bSc.r, consts.r], [tmpn.r])
                P.op("dve", lambda e: e.tensor_reduce(out=nqb.t[0:Pn, :], in_=tmpn.t[0:Pn], axis=AX.X, op=ALU.add), [tmpn.r], [nqb.r])
                P.op("dve", lambda e: e.tensor_tensor(out=nq.t[0:Pn, 0, :], in0=nqb.t[0:Pn, :], in1=bSc.t[0:Pn, 128:132], op=ALU.add), [nqb.r, bSc.r], [nq.r])
                P.op("dve", lambda e: e.tensor_scalar(out=nq.t[0:Pn, 1, :], in0=nq.t[0:Pn, 0, :], scalar1=-1.0, scalar2=None, op0=ALU.mult), [nq.r], [nq.r])
                P.op("dve", lambda e: e.tensor_tensor(out=nq.t[0:Pn, 2, :], in0=nq.t[0:Pn, 0, :], in1=nq.t[0:Pn, 1, :], op=ALU.max), [nq.r], [nq.r])
                P.op("dve", lambda e, c=c: e.tensor_tensor(out=den.t[0:Pn, :], in0=nq.t[0:Pn, 2, :], in1=dfl.t[0:Pn, c, :], op=ALU.max), [nq.r, dfl.r], [den.r])
                for hh in range(4):
                    P.op("dve", lambda e, hh=hh: e.bn_stats(out=st6[hh].t[0:Pn, :], in_=bNs[hh].t[0:Pn, :]), [bNs[hh].r], [st6[hh].r])
                for hh in range(4):
                    P.op("dve", lambda e, hh=hh: e.bn_aggr(out=mv.t[0:Pn, hh, :], in_=st6[hh].t[0:Pn, :]), [st6[hh].r], [mv.r])
                P.op("dve", lambda e: e.tensor_tensor(out=t1.t[0:Pn, :], in0=den.t[0:Pn, :], in1=den.t[0:Pn, :], op=ALU.mult), [den.r], [t1.r])
                P.op("dve", lambda e: e.scalar_tensor_tensor(out=t1.t[0:Pn, :], in0=t1.t[0:Pn, :], scalar=EPS, in1=mv.t[0:Pn, :, 1], op0=ALU.mult, op1=ALU.add), [t1.r, mv.r], [t1.r])
                P.op("act", lambda e: e.activation(out=rs.t[0:Pn, :], in_=t1.t[0:Pn, :], func=AF.Sqrt), [t1.r], [rs.r])
                P.op("dve", lambda e: e.reciprocal(out=rs.t[0:Pn, :], in_=rs.t[0:Pn, :]), [rs.r], [rs.r])
                P.op("dve", lambda e: e.scalar_tensor_tensor(out=nb.t[0:Pn, :], in0=mv.t[0:Pn, :, 0], scalar=-1.0, in1=rs.t[0:Pn, :], op0=ALU.mult, op1=ALU.mult), [mv.r, rs.r], [nb.r])
                for hh in range(4):
                    P.op("act", lambda e, hh=hh, c=c: e.activation(out=hn_tok.t[0:Pn, c, hh * 512:(hh + 1) * 512], in_=bNs[hh].t[0:Pn, :], func=AF.Identity,
                                                                   scale=rs.t[0:Pn, hh:hh + 1], bias=nb.t[0:Pn, hh:hh + 1]), [bNs[hh].r, rs.r, nb.r], [hn_tok.r])

            if kstop == 3:
                return
            pend = []
            for fo in range(16):
                if fo % 2 == 0:
                    wt = next_wA()
                    P.dma("pool", wt.t[:], wup_d[j][8 + fo // 2], writes=[wt.r])
                fc = fo % 2
                bx = banks[fo % 2]
                bT = banks[2 + 3 * (fo % 2)]
                bTb = bT.t[:].bitcast(BF16)

                def mm(e, wt=wt, bx=bx, fc=fc):
                    for k in range(8):
                        ins = e.matmul(bx.t[:, 0:T], wt.t[:, k, fc * 128:(fc + 1) * 128], xn.t[:, k, 0:T], start=(k == 0), stop=(k == 7))
                    return ins
                P.op("pe", mm, [wt.r, xn.r], [bx.r])
                szt = sz[fo % 2]
                eat = ea[fo % 2]
                P.op("act", lambda e, bx=bx, szt=szt: e.activation(out=szt.t[:, 0:T], in_=bx.t[:, 0:T], func=AF.Silu), [bx.r], [szt.r])

                def mmt(e, fo=fo, bTb=bTb):
                    for c in range(nch):
                        ins = e.transpose(out=bTb[:, c * Pn:(c + 1) * Pn], in_=hn_tok.t[0:Pn, c, fo * 128:(fo + 1) * 128], identity=identb[0:Pn, 0:Pn])
                    return ins
                P.op("pe", mmt, [hn_tok.r, cbf.r], [bT.r])
                P.op("act", lambda e, eat=eat, fo=fo, bTb=bTb: e.activation(out=eat.t[:, 0:T], in_=bTb[:, 0:T], func=AF.Copy, scale=V(f"ml_hn_g_{j}", fo)), [bT.r, vecs.r], [eat.r])
                P.op("dve", lambda e, eat=eat, fo=fo: e.scalar_tensor_tensor(out=eat.t[:, 0:T], in0=xc.t[:, fo, 0:T], scalar=V(f"ml_skip_{j}", fo), in1=eat.t[:, 0:T],
                                                                           op0=ALU.mult, op1=ALU.add), [xc[fo // 4], eat.r, vecs.r], [eat.r])
                pend.append((eat, szt, fo))
                if len(pend) == 2:
                    eat_, szt_, fo_ = pend.pop(0)
                    P.op("dve", lambda e, eat_=eat_, szt_=szt_, fo_=fo_: e.tensor_tensor(out=oT.t[:, fo_, 0:T], in0=eat_.t[:, 0:T], in1=szt_.t[:, 0:T], op=ALU.mult), [eat_.r, szt_.r], [oT.r])
            eat_, szt_, fo_ = pend.pop(0)
            P.op("dve", lambda e, eat_=eat_, szt_=szt_, fo_=fo_: e.tensor_tensor(out=oT.t[:, fo_, 0:T], in0=eat_.t[:, 0:T], in1=szt_.t[:, 0:T], op=ALU.mult), [eat_.r, szt_.r], [oT.r])
            xr = xres(t0, T)
            for d in range(8):
                wdt = next_wB()
                wv = wdt.t[:, :].rearrange("p (f c) -> p f c", f=16)
                P.dma("pool", wv, wdn_d[j][d], writes=[wdt.r])
                bk = banks[3 + 3 * (d % 2)]

                def mm(e, wv=wv, bk=bk):
                    for f in range(16):
                        ins = e.matmul(bk.t[:, 0:T], wv[:, f, :], oT.t[:, f, 0:T], start=(f == 0), stop=(f == 15))
                    return ins
                P.op("pe", mm, [wdt.r, oT.r], [bk.r])
                P.op("dve", lambda e, bk=bk, d=d: e.tensor_tensor(out=x.t[:, d, t0:t0 + T], in0=bk.t[:, 0:T], in1=x.t[:, d, t0:t0 + T], op=ALU.add), [bk.r] + xr, xr)
            if not samp:
                P.op("pool", lambda e: e.tensor_copy(out=xmh.t[:, :, 0, 0:3], in_=xmh.t[:, :, 0, T:T + 3]), [xmh.r] + [xmh[g] for g in range(4)], [xmh.r] + [xmh[g] for g in range(4)])
            if last_prompt:
                P.dma("sp", Cp_o[j].rearrange("h (dc p) e -> p (h dc) e", p=128), Cst.t[:], reads=[Cst[h_] for h_ in range(4)])
                P.dma("sp", npT_o[j], nst.t[:], reads=[nst[h_] for h_ in range(4)])
                P.dma("sp", mp_o[j], mcur.t[:, nch, :], reads=[mcur.r])
                P.dma("sp", convp_o[j], tailp.t[:], reads=[tailp.r])
            if samp:
                P.dma("sp", nsT_o[j], nst.t[:], reads=[nst[h_] for h_ in range(4)])
                P.dma("sp", ms_o[j], mcur.t[0:TS, nch, :], reads=[mcur.r])
                P.dma("sp", convs_o[j], tails.t[:], reads=[tails.r])

    def mlstm_v1_sample(i, j):
        P.phase()
        gname = f"norm_mix_{i}"
        T = 128
        NSL = 5
        bd = P.atile([128, 3, 16, 128], BF16)
        colmask = P.atile([128, NB, TS], BF16)
        P.dma("pool", colmask.t[:], colmask_d, writes=[colmask.r])
        wgate = P.atile([128, 48, 8], BF16)
        bgate = P.atile([128, 8], F32)
        xn = P.atile([128, 8, 256], BF16)
        xmh = P.atile([128, 16, 3 + T], BF16)
        tailp = P.atile([128, 16, 3], F32)
        xc = P.atile([128, 16, T], BF16)
        acc = [P.atile([128, T], F32) for _ in range(2)]
        qT = P.atile([128, 16, T], BF16)
        kT = P.atile([128, 16, T], BF16)
        sqb = Tile(kT.t[:, 0:8, :], kT.base_lw)
        sqb._res = kT._res
        vTt = [P.atile([128, T], BF16) for _ in range(2)]
        v_tk = [P.atile([128, 512], BF16) for _ in range(2)]
        kp_tk = [P.atile([128, 512], BF16) for _ in range(2)]
        Cst = P.atile([128, 4 * NSL, 512], F32)
        Cbf = [P.atile([128, 4, 512], BF16) for _ in range(2)]
        nst = P.atile([128, 16, NB], F32)
        nbf = P.atile([128, 16, NB], BF16)
        ST = [P.atile([128, 128], BF16) for _ in range(2)]
        hn_tok = P.atile([128, 2, 2048], BF16)
        tails = Tile(hn_tok.t[:, 1, 512:2048].bitcast(F32).rearrange("p (f s w) -> p f s w", f=16, s=NB), hn_tok.base_lw)
        tails._res = hn_tok._res
        sz = acc
        ea = [P.atile([128, T], F32)]
        rst = ea[0]
        gT_sb = P.atile([8, T], F32)
        sm = {nm: P.atile([128, 8], F32) for nm in ["g", "e1", "lf", "btbl", "a", "R", "d1", "es", "d2", "c0", "d3", "dfl", "m0a", "m0b",
                                                    "nq", "den", "mv", "t1", "rs", "nb", "nqb"]}
        st6 = P.atile([128, 6], F32)
        amaxT = P.atile([4, NB], F32)
        amax_exp = P.atile([4, 128], F32)
        rhsx = P.atile([128, NB, 4], F32)
        c0bc = P.atile([128, NB, 4], F32)
        tmpn = P.atile([128, NB], F32)
        tmpn2 = P.atile([128, 4, NB], F32)
        qm = [P.atile([128, 4, TS], BF16) for _ in range(2)]
        kpm = [P.atile([64, 512], BF16) for _ in range(2)]
        oT = qT
        xmh_s = Tile(xmh.t[:, :, 0:NB * 7].rearrange("p f (s l) -> p f s l", s=NB), xmh.base_lw)
        xmh_s._res = xmh._res

        P.dma("pool", bd.t[:], bd_d[j], writes=[bd.r])
        P.dma("pool", wgate.t[:], wgate_d[j], writes=[wgate.r])
        P.dma("sp", bgate.t[:], bgate_d[j], writes=[bgate.r])
        P.op("pool", lambda e: e.memset(Cst.t[:], 0.0), [], [Cst[s_] for s_ in range(NSL)])
        P.op("pool", lambda e: e.memset(nst.t[:], 0.0), [], [nst.r])
        P.op("pool", lambda e: e.memset(sm["m0a"].t[:], 0.0), [], [sm["m0a"].r])
        P.op("pool", lambda e: e.memset(xmh.t[:, :, 0:3], 0.0), [], [xmh.r])

        def xmc_ap(fo, cs=slice(0, TS)):
            base = 64 + (fo % 2) * 64
            return xn.t[:, fo // 2, base + cs.start:base + cs.stop]
        m0 = [sm["m0a"], sm["m0b"]]
        m0_i = [0]
        lnscale = math.log(DH ** -0.5)

        tiles = [(TP, TS, NB, 4, TS, 1)]
        ld_issued = [0]
        cnt = [0]
        for tidx, (t0, Tn, nseq, L, Pn, nch) in enumerate(tiles):
            samp = nseq > 1
            last_prompt = (t0 + Tn == TP)
            if samp:
                P.dma("sp", nst.t[:], snT_d[j], writes=[nst.r])
                P.dma("sp", m0[m0_i[0]].t[0:TS, 0:4], sm_d[j], writes=[m0[m0_i[0]].r])
                P.dma("sp", tails.t[:], sconv_d[j], writes=[tails.r])
                P.op("dve", lambda e: e.tensor_copy(out=xmh_s.t[:, :, :, 0:3], in_=tails.t[:]), [tails.r], [xmh.r])
            rmsnorm(t0, Tn, gname, xn, xn.r, sqb, rst, banks[6])
            for blk in range(8):
                wt = next_wA()
                P.dma("pool", wt.t[:], wup_d[j][blk], writes=[wt.r])
                for fc in range(2):
                    fo = blk * 2 + fc
                    bx = banks[cnt[0] % 2]
                    cnt[0] += 1

                    def mm(e, wt=wt, bx=bx, fc=fc):
                        for k in range(8):
                            ins = e.matmul(bx.t[:, 0:Tn], wt.t[:, k, fc * 128:(fc + 1) * 128], xn.t[:, k, 0:Tn], start=(k == 0), stop=(k == 7))
                        return ins
                    P.op("pe", mm, [wt.r, xn.r], [bx.r])
                    if not samp:
                        P.op("act", lambda e, bx=bx, fo=fo: e.activation(out=xmh.t[:, fo, 3:3 + Tn], in_=bx.t[:, 0:Tn], func=AF.Copy), [bx.r], [xmh.r])
                        if last_prompt:
                            P.op("dve", lambda e, bx=bx, fo=fo: e.tensor_copy(out=tailp.t[:, fo, :], in_=bx.t[:, Tn - 3:Tn]), [bx.r], [tailp.r])
                        xm_op = xmh.t[:, fo, 3:3 + Tn]
                        wins = [xmh.t[:, fo, w:w + Tn] for w in range(4)]
                        accv = [a_.t[:, 0:Tn] for a_ in acc]
                    else:
                        bx3 = bx.t[:, 0:Tn].rearrange("p (s l) -> p s l", s=NB)
                        P.op("act", lambda e, bx3=bx3, fo=fo: e.activation(out=xmh_s.t[:, fo, :, 3:7], in_=bx3, func=AF.Copy), [bx.r], [xmh.r])
                        P.op("act", lambda e, bx=bx, fo=fo: e.activation(out=xmc_ap(fo), in_=bx.t[:, 0:Tn], func=AF.Copy), [bx.r], [xn.r])
                        P.op("dve", lambda e, bx3=bx3, fo=fo: e.tensor_copy(out=tails.t[:, fo, :, :], in_=bx3[:, :, 1:4]), [bx.r], [tails.r])
                        xm_op = xmc_ap(fo)
                        wins = [xmh_s.t[:, fo, :, w:w + 4] for w in range(4)]
                        accv = [a_.t[:, 0:Tn].rearrange("p (s l) -> p s l", s=NB) for a_ in acc]
                    at = acc[fo % 2]
                    av = accv[fo % 2]
                    P.op("dve", lambda e, av=av, wins=wins, fo=fo: e.tensor_scalar(out=av, in0=wins[0], scalar1=V(f"ml_wconv_{j}_0", fo), scalar2=V(f"ml_bconv_{j}", fo),
                                                                                   op0=ALU.mult, op1=ALU.add), [xmh.r, vecs.r], [at.r])
                    for w in range(1, 4):
                        P.op("dve", lambda e, av=av, wins=wins, fo=fo, w=w: e.scalar_tensor_tensor(out=av, in0=wins[w], scalar=V(f"ml_wconv_{j}_{w}", fo), in1=av,
                                                                                                 op0=ALU.mult, op1=ALU.add), [xmh.r, at.r, vecs.r], [at.r])
                    P.op("act", lambda e, at=at, fo=fo: e.activation(out=xc.t[:, fo, 0:Tn], in_=at.t[:, 0:Tn], func=AF.Silu), [at.r], [xc.r])
                    bq, bk_, bv = banks[2], banks[3], banks[4]
                    P.op("pe", lambda e, fo=fo: e.matmul(bq.t[:, 0:Tn], bd.t[:, 0, fo, :], xc.t[:, fo, 0:Tn], start=True, stop=True), [bd.r, xc.r], [bq.r])
                    P.op("pe", lambda e, fo=fo: e.matmul(bk_.t[:, 0:Tn], bd.t[:, 1, fo, :], xc.t[:, fo, 0:Tn], start=True, stop=True), [bd.r, xc.r], [bk_.r])
                    P.op("pe", lambda e, fo=fo, xm_op=xm_op: e.matmul(bv.t[:, 0:Tn], bd.t[:, 2, fo, :], xm_op, start=True, stop=True), [bd.r, xmh.r, xn.r], [bv.r])
                    vt = vTt[fo % 2]
                    P.op("act", lambda e, fo=fo: e.activation(out=qT.t[:, fo, 0:Tn], in_=bq.t[:, 0:Tn], func=AF.Copy), [bq.r], [qT.r])
                    P.op("dve", lambda e, fo=fo: e.tensor_copy(out=kT.t[:, fo, 0:Tn], in_=bk_.t[:, 0:Tn]), [bk_.r], [kT.r])
                    P.op("act", lambda e, vt=vt: e.activation(out=vt.t[:, 0:Tn], in_=bv.t[:, 0:Tn], func=AF.Copy), [bv.r], [vt.r])
                    bg = banks[5]
                    for which, (src, sres) in enumerate([(qT.t[:, fo, 0:Tn], qT.r), (kT.t[:, fo, 0:Tn], kT.r), (vt.t[:, 0:Tn], vt.r)]):
                        first = (fo == 0 and which == 0)
                        lastm = (fo == 15 and which == 2)
                        P.op("pe", lambda e, src=src, which=which, fo=fo, first=first, lastm=lastm: e.matmul(bg.t[0:8, 0:Tn], wgate.t[:, which * 16 + fo, :], src, start=first, stop=lastm),
                             [wgate.r, sres], [bg.r])
            P.op("act", lambda e: e.activation(out=gT_sb.t[:, 0:Tn], in_=banks[5].t[0:8, 0:Tn], func=AF.Copy), [banks[5].r], [gT_sb.r])
            if dbg == "ml0" and tidx == int(os.environ.get("KDBG_T", "0")) and j == 0:
                do_ = dout("dbg_x", [128, 8, T])
                P.op("dve", lambda e: e.tensor_copy(out=dbg_stage.t[:, 0:8, :], in_=x.t[:, :, t0:t0 + T]), xres(t0, T), [dbg_stage.r])
                P.dma("sp", do_, dbg_stage.t[:], reads=[dbg_stage.r])
                for nm_, src_, n3 in [("dbg_xn", xn, 8), ("dbg_xm", None, 16), ("dbg_xc", xc, 16), ("dbg_q", qT, 16)]:
                    do_ = dout(nm_, [128, n3, T])
                    stg = dbg_stage
                    for h0 in range(0, n3, 8):
                        if src_ is None:
                            P.op("dve", lambda e, stg=stg, h0=h0: e.tensor_copy(out=stg.t[:, 0:8, :], in_=xmh.t[:, h0:h0 + 8, 3:3 + T]), [xmh.r], [stg.r])
                        else:
                            P.op("dve", lambda e, stg=stg, src_=src_, h0=h0: e.tensor_copy(out=stg.t[:, 0:8, :], in_=src_.t[:, h0:h0 + 8, 0:T]), [src_.r], [stg.r])
                        P.dma("sp", do_[:, h0:h0 + 8, :], stg.t[:, 0:8, :], reads=[stg.r])

            for c in range(nch):
                cs = slice(c * Pn, (c + 1) * Pn)
                bS = banks[7]
                mask = C("maskS" if samp else "maskP", Pn, Pn)
                onesblk = C("onesS" if samp else "onesP", Pn, Pn)
                seqm = C("seqS" if samp else "seqP", Pn, nseq)
                firstm = C("firstS" if samp else "firstP", Pn, nseq)
                seqmb = cbf.t[0:Pn, 256:256 + nseq] if samp else cbf.t[0:Pn, 272:273]
                m0c = m0[m0_i[0]]
                m0n = m0[1 - m0_i[0]]
                m0_i[0] = 1 - m0_i[0]
                s = sm

                def S(nm, a=0, b=4):
                    return s[nm].t[0:Pn, a:b]
                P.op("pe", lambda e, cs=cs: e.transpose(out=bS.t[0:Pn, 0:8], in_=gT_sb.t[0:8, cs], identity=C("ident", 8, 8)), [gT_sb.r, consts.r], [bS.r])
                P.op("dve", lambda e: e.tensor_tensor(out=S("g", 0, 8), in0=bS.t[0:Pn, 0:8], in1=bgate.t[0:Pn, 0:8], op=ALU.add), [bS.r, bgate.r], [s["g"].r])
                P.op("act", lambda e: e.activation(out=S("e1"), in_=S("g", 4, 8), func=AF.Exp, scale=-1.0), [s["g"].r], [s["e1"].r])
                P.op("act", lambda e: e.activation(out=S("lf"), in_=S("e1"), func=AF.Ln, bias=1.0), [s["e1"].r], [s["lf"].r])
                P.op("dve", lambda e: e.tensor_scalar(out=S("lf"), in0=S("lf"), scalar1=-1.0, scalar2=None, op0=ALU.mult), [s["lf"].r], [s["lf"].r])

                def mm(e, mask=mask, onesblk=onesblk):
                    e.matmul(bS.t[0:Pn, 8:12], mask, S("lf"), start=True, stop=True)
                    return e.matmul(bS.t[0:Pn, 12:16], onesblk, S("lf"), start=True, stop=True)
                P.op("pe", mm, [consts.r, s["lf"].r], [bS.r])
                P.op("dve", lambda e: e.tensor_copy(out=S("btbl", 0, 8), in_=bS.t[0:Pn, 8:16]), [bS.r], [s["btbl"].r])
                P.op("dve", lambda e: e.tensor_tensor(out=S("a"), in0=S("g", 0, 4), in1=S("btbl", 0, 4), op=ALU.subtract), [s["g"].r, s["btbl"].r], [s["a"].r])
                P.op("pe", lambda e: e.transpose(out=bS.t[0:4, 16:16 + Pn], in_=S("a"), identity=C("ident", Pn, Pn)), [s["a"].r, consts.r], [bS.r])
                P.op("dve", lambda e: e.tensor_reduce(out=amaxT.t[0:4, 0:nseq], in_=bS.t[0:4, 16:16 + Pn].rearrange("p (s l) -> p s l", s=nseq), axis=AX.X, op=ALU.max),
                     [bS.r], [amaxT.r])
                P.op("dve", lambda e: e.tensor_copy(out=amax_exp.t[0:4, 0:Pn].rearrange("p (s l) -> p s l", s=nseq),
                                                     in_=amaxT.t[0:4, 0:nseq].unsqueeze(2).to_broadcast([4, nseq, L])), [amaxT.r], [amax_exp.r])
                P.op("pe", lambda e: e.matmul(bS.t[0:Pn, 160:164], amax_exp.t[0:4, 0:Pn], C("ident", 4, 4), start=True, stop=True), [amax_exp.r, consts.r], [bS.r])
                P.op("dve", lambda e, m0c=m0c: e.tensor_tensor(out=S("R"), in0=bS.t[0:Pn, 160:164], in1=m0c.t[0:Pn, 0:4], op=ALU.max), [bS.r, m0c.r], [s["R"].r])
                P.op("dve", lambda e: e.tensor_tensor(out=S("d1"), in0=S("a"), in1=S("R"), op=ALU.subtract), [s["a"].r, s["R"].r], [s["d1"].r])
                P.op("act", lambda e: e.activation(out=S("es"), in_=S("d1"), func=AF.Exp, bias=V("c_lnscale", 0, Pn)), [s["d1"].r, vecs.r], [s["es"].r])
                P.op("dve", lambda e, m0c=m0c: e.tensor_tensor(out=S("d2"), in0=m0c.t[0:Pn, 0:4], in1=S("R"), op=ALU.subtract), [m0c.r, s["R"].r], [s["d2"].r])
                P.op("act", lambda e: e.activation(out=S("c0"), in_=S("d2"), func=AF.Exp), [s["d2"].r], [s["c0"].r])
                P.op("dve", lambda e: e.tensor_tensor(out=S("d3"), in0=S("btbl", 0, 4), in1=S("R"), op=ALU.add), [s["btbl"].r, s["R"].r], [s["d3"].r])
                P.op("act", lambda e: e.activation(out=S("dfl"), in_=S("d3"), func=AF.Exp, scale=-1.0), [s["d3"].r], [s["dfl"].r])
                P.op("dve", lambda e, m0n=m0n: e.tensor_tensor(out=m0n.t[0:Pn, 0:4], in0=S("btbl", 4, 8), in1=S("R"), op=ALU.add), [s["btbl"].r, s["R"].r], [m0n.r])
                P.op("dve", lambda e, firstm=firstm: e.tensor_tensor(out=rhsx.t[0:Pn, 0:nseq, :], in0=S("c0").unsqueeze(1).to_broadcast([Pn, nseq, 4]),
                                                                        in1=firstm.unsqueeze(2).to_broadcast([Pn, nseq, 4]), op=ALU.mult), [s["c0"].r, consts.r], [rhsx.r])
                P.op("pe", lambda e: e.matmul(bS.t[:, 192:192 + nseq * 4], C("onesP", Pn, 128), rhsx.t[0:Pn, 0:nseq, :].rearrange("p s h -> p (s h)"), start=True, stop=True),
                     [rhsx.r, consts.r], [bS.r])
                P.op("dve", lambda e: e.tensor_copy(out=c0bc.t[:, 0:nseq, :].rearrange("p s h -> p (s h)"), in_=bS.t[:, 192:192 + nseq * 4]), [bS.r], [c0bc.r])

                for hh in range(4):
                    bSc, bN, bQ2 = banks[2], banks[3], banks[5]
                    stt = ST[hh % 2]
                    bV2 = banks[4]
                    v_tok = v_tk[hh % 2]
                    kp_tok = kp_tk[hh % 2]

                    def mmv(e, hh=hh):
                        for fl in range(4):
                            fo = hh * 4 + fl
                            lhs = (xmc_ap(fo, cs) if samp else xmh.t[:, fo, 3 + c * Pn:3 + (c + 1) * Pn])
                            ins = e.matmul(bV2.t[0:Pn, fl * 128:(fl + 1) * 128], lhs, bd.t[:, 2, fo, :], start=True, stop=True)
                        return ins
                    P.op("pe", mmv, [xmh.r, xn.r, bd.r], [bV2.r])
                    P.op("act", lambda e, v_tok=v_tok: e.activation(out=v_tok.t[0:Pn, :], in_=bV2.t[0:Pn, :], func=AF.Copy), [bV2.r], [v_tok.r])

                    def mmk(e, hh=hh):
                        for fl in range(4):
                            fo = hh * 4 + fl
                            ins = e.matmul(bV2.t[0:Pn, fl * 128:(fl + 1) * 128], xc.t[:, fo, cs], bd.t[:, 1, fo, :], start=True, stop=True)
                        return ins
                    P.op("pe", mmk, [xc.r, bd.r], [bV2.r])
                    P.op("act", lambda e, hh=hh, kp_tok=kp_tok: e.activation(out=kp_tok.t[0:Pn, :], in_=bV2.t[0:Pn, :], func=AF.Copy, scale=S("es", hh, hh + 1)),
                         [bV2.r, s["es"].r], [kp_tok.r])

                    def mms(e, hh=hh):
                        for dc in range(4):
                            ins = e.matmul(bSc.t[0:Pn, 0:Pn], kT.t[:, hh * 4 + dc, cs], qT.t[:, hh * 4 + dc, cs], start=(dc == 0), stop=(dc == 3))
                        return ins
                    P.op("pe", mms, [kT.r, qT.r], [bSc.r])
                    P.op("dve", lambda e, hh=hh, stt=stt, mask=mask: e.scalar_tensor_tensor(out=stt.t[0:Pn, 0:Pn], in0=bSc.t[0:Pn, 0:Pn], scalar=S("es", hh, hh + 1), in1=mask,
                                                                                          op0=ALU.mult, op1=ALU.mult), [bSc.r, s["es"].r, consts.r], [stt.r])
                    P.op("dve", lambda e, hh=hh: e.tensor_tensor(out=nbf.t[:, hh * 4:(hh + 1) * 4, 0:nseq], in0=nst.t[:, hh * 4:(hh + 1) * 4, 0:nseq],
                                                                 in1=c0bc.t[:, 0:nseq, hh].unsqueeze(1).to_broadcast([128, 4, nseq]), op=ALU.mult), [nst.r, c0bc.r], [nbf.r])
                    if not samp:
                        cb = Cbf[0]
                        P.op("act", lambda e, hh=hh, cb=cb: e.activation(out=cb.t[:], in_=Cst.t[:, hh * 4:(hh + 1) * 4, :], func=AF.Copy, scale=c0bc.t[:, 0, hh:hh + 1]),
                             [Cst[hh], c0bc.r], [cb.r])

                        def mmn(e, hh=hh, stt=stt, cb=cb, v_tok=v_tok):
                            e.matmul(bN.t[0:Pn, :], stt.t[0:Pn, 0:Pn], v_tok.t[0:Pn, :], start=True, stop=False)
                            for dc in range(4):
                                ins = e.matmul(bN.t[0:Pn, :], qT.t[:, hh * 4 + dc, cs], cb.t[:, dc, :], start=False, stop=(dc == 3))
                            return ins
                        P.op("pe", mmn, [stt.r, v_tok.r, qT.r, cb.r], [bN.r])
                    else:
                        P.op("pe", lambda e, hh=hh, stt=stt, v_tok=v_tok: e.matmul(bN.t[0:Pn, :], stt.t[0:Pn, 0:Pn], v_tok.t[0:Pn, :], start=True, stop=False),
                             [stt.r, v_tok.r], [bN.r])
                        for b in range(NB):
                            slot = (hh * NB + b) % NSL
                            cslot = Cst.t[:, slot * 4:(slot + 1) * 4, :]
                            cres = Cst[slot]
                            while ld_issued[0] <= min(hh * NB + b + NSL - 2, 4 * NB - 1):
                                k_ = ld_issued[0]
                                P.dma("sp", Cst.t[:, (k_ % NSL) * 4:(k_ % NSL + 1) * 4, :], sC_d[j, k_ % NB, k_ // NB].rearrange("(dc p) e -> p dc e", p=128), writes=[Cst[k_ % NSL]])
                                ld_issued[0] += 1
                            cb = Cbf[b % 2]
                            P.op("act", lambda e, cb=cb, cslot=cslot, b=b, hh=hh: e.activation(out=cb.t[:], in_=cslot, func=AF.Copy, scale=c0bc.t[:, b, hh:hh + 1]),
                                 [cres, c0bc.r], [cb.r])
                            qmt = qm[b % 2]
                            P.op("dve", lambda e, qmt=qmt, b=b, hh=hh: e.tensor_tensor(out=qmt.t[:], in0=qT.t[:, hh * 4:(hh + 1) * 4, 0:TS],
                                                                                       in1=colmask.t[:, b, :].unsqueeze(1).to_broadcast([128, 4, TS]), op=ALU.mult),
                                 [qT.r, colmask.r], [qmt.r])

                            def mmc(e, qmt=qmt, cb=cb, b=b):
                                for dc in range(4):
                                    ins = e.matmul(bN.t[0:Pn, :], qmt.t[:, dc, :], cb.t[:, dc, :], start=False, stop=(b == NB - 1 and dc == 3))
                                return ins
                            P.op("pe", mmc, [qmt.r, cb.r], [bN.r])
                            kpt = kpm[b % 2]
                            P.op("dve", lambda e, kpt=kpt, b=b, hh=hh, kp_tok=kp_tok: e.tensor_scalar(out=kpt.t[0:Pn, :], in0=kp_tok.t[0:Pn, :],
                                                                                       scalar1=consts.t[0:Pn, CL["seqS"] + b:CL["seqS"] + b + 1], scalar2=None, op0=ALU.mult),
                                 [kp_tok.r, consts.r], [kpt.r])
                            for dc in range(4):
                                bcu = banks[cnt[0] % 2]
                                cnt[0] += 1
                                P.op("pe", lambda e, bcu=bcu, kpt=kpt, dc=dc, hh=hh, v_tok=v_tok: e.matmul(bcu.t[:, :], kpt.t[0:Pn, dc * 128:(dc + 1) * 128], v_tok.t[0:Pn, :],
                                                                                              start=True, stop=True), [kpt.r, v_tok.r], [bcu.r])
                                P.op("dve", lambda e, bcu=bcu, dc=dc, b=b, hh=hh, slot=slot: e.scalar_tensor_tensor(out=Cst.t[:, slot * 4 + dc, :], in0=Cst.t[:, slot * 4 + dc, :],
                                                                                                                   scalar=c0bc.t[:, b, hh:hh + 1], in1=bcu.t[:, :], op0=ALU.mult, op1=ALU.add),
                                     [bcu.r, cres, c0bc.r], [cres])
                            P.dma("sp", Cs_o[j, b, hh].rearrange("(dc p) e -> p dc e", p=128), cslot, reads=[cres])

                    def mmq(e, hh=hh, stt=stt, seqmb=seqmb):
                        e.matmul(bQ2.t[0:Pn, 0:1], stt.t[0:Pn, 0:Pn], onesb[0:Pn, 0:1], start=True, stop=True)
                        for dc in range(4):
                            ins = e.matmul(bQ2.t[0:Pn, 8:8 + nseq], qT.t[:, hh * 4 + dc, cs], nbf.t[:, hh * 4 + dc, 0:nseq], start=(dc == 0), stop=(dc == 3))
                        return ins
                    P.op("pe", mmq, [stt.r, cbf.r, qT.r, nbf.r], [bQ2.r])
                    P.op("dve", lambda e, seqm=seqm: e.tensor_tensor(out=tmpn.t[0:Pn, 0:nseq], in0=bQ2.t[0:Pn, 8:8 + nseq], in1=seqm, op=ALU.mult), [bQ2.r, consts.r], [tmpn.r])
                    P.op("dve", lambda e: e.tensor_reduce(out=S("nqb", 0, 1), in_=tmpn.t[0:Pn, 0:nseq], axis=AX.X, op=ALU.add), [tmpn.r], [s["nqb"].r])
                    P.op("dve", lambda e: e.tensor_tensor(out=S("nq", 0, 1), in0=S("nqb", 0, 1), in1=bQ2.t[0:Pn, 0:1], op=ALU.add), [s["nqb"].r, bQ2.r], [s["nq"].r])
                    P.op("dve", lambda e: e.tensor_scalar(out=S("nq", 1, 2), in0=S("nq", 0, 1), scalar1=-1.0, scalar2=None, op0=ALU.mult), [s["nq"].r], [s["nq"].r])
                    P.op("dve", lambda e: e.tensor_tensor(out=S("nq", 2, 3), in0=S("nq", 0, 1), in1=S("nq", 1, 2), op=ALU.max), [s["nq"].r], [s["nq"].r])
                    P.op("dve", lambda e, hh=hh: e.tensor_tensor(out=S("den", 0, 1), in0=S("nq", 2, 3), in1=S("dfl", hh, hh + 1), op=ALU.max),
                         [s["nq"].r, s["dfl"].r], [s["den"].r])
                    P.op("dve", lambda e: e.bn_stats(out=st6.t[0:Pn, :], in_=bN.t[0:Pn, :]), [bN.r], [st6.r])
                    P.op("dve", lambda e: e.bn_aggr(out=S("mv", 0, 2), in_=st6.t[0:Pn, :]), [st6.r], [s["mv"].r])
                    P.op("dve", lambda e: e.tensor_tensor(out=S("t1", 0, 1), in0=S("den", 0, 1), in1=S("den", 0, 1), op=ALU.mult), [s["den"].r], [s["t1"].r])
                    P.op("dve", lambda e: e.scalar_tensor_tensor(out=S("t1", 0, 1), in0=S("t1", 0, 1), scalar=EPS, in1=S("mv", 1, 2), op0=ALU.mult, op1=ALU.add),
                         [s["t1"].r, s["mv"].r], [s["t1"].r])
                    P.op("act", lambda e: e.activation(out=S("rs", 0, 1), in_=S("t1", 0, 1), func=AF.Sqrt), [s["t1"].r], [s["rs"].r])
                    P.op("dve", lambda e: e.reciprocal(out=S("rs", 0, 1), in_=S("rs", 0, 1)), [s["rs"].r], [s["rs"].r])
                    P.op("dve", lambda e: e.scalar_tensor_tensor(out=S("nb", 0, 1), in0=S("mv", 0, 1), scalar=-1.0, in1=S("rs", 0, 1), op0=ALU.mult, op1=ALU.mult),
                         [s["mv"].r, s["rs"].r], [s["nb"].r])
                    P.op("act", lambda e, hh=hh: e.activation(out=hn_tok.t[0:Pn, c, hh * 512:(hh + 1) * 512], in_=bN.t[0:Pn, :], func=AF.Identity,
                                                              scale=S("rs", 0, 1), bias=S("nb", 0, 1)), [bN.r, s["rs"].r, s["nb"].r], [hn_tok.r])
                    if not samp:
                        for dc in range(4):
                            bcu = banks[cnt[0] % 2]
                            cnt[0] += 1
                            P.op("pe", lambda e, bcu=bcu, dc=dc, hh=hh, kp_tok=kp_tok, v_tok=v_tok: e.matmul(bcu.t[:, :], kp_tok.t[0:Pn, dc * 128:(dc + 1) * 128],
                                                                               v_tok.t[0:Pn, :], start=True, stop=True), [kp_tok.r, v_tok.r], [bcu.r])
                            P.op("dve", lambda e, bcu=bcu, dc=dc, hh=hh: e.scalar_tensor_tensor(out=Cst.t[:, hh * 4 + dc, :], in0=Cst.t[:, hh * 4 + dc, :], scalar=c0bc.t[:, 0, hh:hh + 1],
                                                                                              in1=bcu.t[:, :], op0=ALU.mult, op1=ALU.add), [bcu.r, Cst[hh], c0bc.r], [Cst[hh]])

                    def mmnn(e, hh=hh, seqmb=seqmb, kp_tok=kp_tok):
                        for dc in range(4):
                            ins = e.matmul(bQ2.t[:, 64 + dc * NB:64 + dc * NB + nseq], kp_tok.t[0:Pn, dc * 128:(dc + 1) * 128], seqmb, start=True, stop=True)
                        return ins
                    P.op("pe", mmnn, [kp_tok.r, cbf.r], [bQ2.r])
                    P.op("dve", lambda e, hh=hh: e.tensor_tensor(out=tmpn2.t[:, :, 0:nseq], in0=nst.t[:, hh * 4:(hh + 1) * 4, 0:nseq],
                                                                 in1=c0bc.t[:, 0:nseq, hh].unsqueeze(1).to_broadcast([128, 4, nseq]), op=ALU.mult), [nst.r, c0bc.r], [tmpn2.r])
                    P.op("dve", lambda e, hh=hh: e.tensor_tensor(out=nst.t[:, hh * 4:(hh + 1) * 4, 0:nseq], in0=tmpn2.t[:, :, 0:nseq],
                                                                 in1=bQ2.t[:, 64:64 + 4 * NB].rearrange("p (d s) -> p d s", d=4)[:, :, 0:nseq], op=ALU.add), [tmpn2.r, bQ2.r], [nst.r])

            bT = banks[2]
            bTb = bT.t[:].bitcast(BF16)
            for blk in range(8, 16):
                wt = next_wA()
                P.dma("pool", wt.t[:], wup_d[j][blk], writes=[wt.r])
                for fc in range(2):
                    fo = (blk - 8) * 2 + fc
                    bx = banks[cnt[0] % 2]
                    cnt[0] += 1

                    def mm(e, wt=wt, bx=bx, fc=fc):
                        for k in range(8):
                            ins = e.matmul(bx.t[:, 0:Tn], wt.t[:, k, fc * 128:(fc + 1) * 128], xn.t[:, k, 0:Tn], start=(k == 0), stop=(k == 7))
                        return ins
                    P.op("pe", mm, [wt.r, xn.r], [bx.r])
                    szt = sz[fo % 2]
                    eat = ea[0]
                    P.op("act", lambda e, bx=bx, szt=szt: e.activation(out=szt.t[:, 0:Tn], in_=bx.t[:, 0:Tn], func=AF.Silu), [bx.r], [szt.r])

                    def mmt(e, fo=fo):
                        for c in range(nch):
                            ins = e.transpose(out=bTb[:, c * Pn:(c + 1) * Pn], in_=hn_tok.t[0:Pn, c, fo * 128:(fo + 1) * 128], identity=identb[0:Pn, 0:Pn])
                        return ins
                    P.op("pe", mmt, [hn_tok.r, cbf.r], [bT.r])
                    P.op("act", lambda e, eat=eat, fo=fo: e.activation(out=eat.t[:, 0:Tn], in_=bTb[:, 0:Tn], func=AF.Copy, scale=V(f"ml_hn_g_{j}", fo)), [bT.r, vecs.r], [eat.r])
                    P.op("dve", lambda e, eat=eat, fo=fo: e.scalar_tensor_tensor(out=eat.t[:, 0:Tn], in0=xc.t[:, fo, 0:Tn], scalar=V(f"ml_skip_{j}", fo), in1=eat.t[:, 0:Tn],
                                                                               op0=ALU.mult, op1=ALU.add), [xc.r, eat.r, vecs.r], [eat.r])
                    P.op("dve", lambda e, eat=eat, szt=szt, fo=fo: e.tensor_tensor(out=oT.t[:, fo, 0:Tn], in0=eat.t[:, 0:Tn], in1=szt.t[:, 0:Tn], op=ALU.mult), [eat.r, szt.r], [oT.r])
            xr = xres(t0, Tn)
            for d in range(8):
                wdt = next_wB()
                wv = wdt.t[:, :].rearrange("p (f c) -> p f c", f=16)
                P.dma("pool", wv, wdn_d[j][d], writes=[wdt.r])
                bk = banks[cnt[0] % 2]
                cnt[0] += 1

                def mm(e, wv=wv, bk=bk):
                    for f in range(16):
                        ins = e.matmul(bk.t[:, 0:Tn], wv[:, f, :], oT.t[:, f, 0:Tn], start=(f == 0), stop=(f == 15))
                    return ins
                P.op("pe", mm, [wdt.r, oT.r], [bk.r])
                P.op("dve", lambda e, bk=bk, d=d: e.tensor_tensor(out=x.t[:, d, t0:t0 + Tn], in0=bk.t[:, 0:Tn], in1=x.t[:, d, t0:t0 + Tn], op=ALU.add), [bk.r] + xr, xr)
            if not samp:
                P.op("pool", lambda e: e.tensor_copy(out=xmh.t[:, :, 0:3], in_=xmh.t[:, :, Tn:Tn + 3]), [xmh.r], [xmh.r])
            if last_prompt:
                P.dma("sp", Cp_o[j].rearrange("h (dc p) e -> p (h dc) e", p=128), Cst.t[:], reads=[Cst[h_] for h_ in range(4)])
                P.dma("sp", npT_o[j], nst.t[:, :, 0:1], reads=[nst.r])
                P.dma("sp", mp_o[j], m0[m0_i[0]].t[:, 0:4], reads=[m0[m0_i[0]].r])
                P.dma("sp", convp_o[j], tailp.t[:], reads=[tailp.r])
            if samp:
                P.dma("sp", nsT_o[j], nst.t[:], reads=[nst.r])
                P.dma("sp", ms_o[j], m0[m0_i[0]].t[0:TS, 0:4], reads=[m0[m0_i[0]].r])
                P.dma("sp", convs_o[j], tails.t[:], reads=[tails.r])

    def chunk_mlp(i, j):
        P.phase()
        gname = f"norm_mix_{i}"
        T = 512
        win = P.atile([128, 8, 8, 256], BF16)
        wsP = P.atile([128, 4, 128], F32)
        wsS = P.atile([64, 4, 64], F32)
        wsPb = P.atile([128, 4, 128], BF16)
        wsSb = P.atile([64, 4, 64], BF16)
        bsP = P.atile([128, 4, 128], F32)
        bsS = P.atile([128, 4, 64], F32)
        xn = P.atile([128, 8, T], BF16)
        sqb = P.atile([128, 8, T], BF16)
        uT = P.atile([128, 8, T], BF16)
        vT = P.atile([128, 8, T], F32)
        vsq = sqb
        mu = P.atile([128, T], F32)
        rst = mu
        var = P.atile([128, T], F32)
        tmp = [P.atile([128, T], F32) for _ in range(2)]
        vn = P.atile([128, 8, T], BF16)
        vnf = P.atile([128, 8, TS], F32)
        vn_tok = P.atile([128, 1024], BF16)
        yT = xn
        for blk in range(8):
            P.dma("pool", win.t[:, blk], win_d[j][blk], writes=[win.r])
        P.dma("sp", wsP.t[:], wsP_d[j], writes=[wsP.r])
        P.dma("sp", wsS.t[:], wsS_d[j], writes=[wsS.r])
        P.dma("sp", bsP.t[:], bsP_d[j], writes=[bsP.r])
        P.dma("sp", bsS.t[:], bsS_d[j], writes=[bsS.r])
        P.op("dve", lambda e: e.tensor_tensor(out=wsPb.t[:], in0=wsP.t[:], in1=C("maskP", 128, 128).unsqueeze(1).to_broadcast([128, 4, 128]), op=ALU.mult),
             [wsP.r, consts.r], [wsPb.r])
        P.op("dve", lambda e: e.tensor_tensor(out=wsSb.t[:], in0=wsS.t[:], in1=C("maskS", 64, 64).unsqueeze(1).to_broadcast([64, 4, 64]), op=ALU.mult),
             [wsS.r, consts.r], [wsSb.r])
        cnt = 0
        for ti, (t0, Tn) in enumerate(FFN_TILES):
            samp = ti == 4
            Pn = TS if samp else 128
            nch = Tn // Pn
            rmsnorm(t0, Tn, gname, xn, xn.r, sqb, rst, banks[6])
            for fo in range(16):
                bx = banks[cnt % 2]
                cnt += 1

                def mm(e, bx=bx, fo=fo):
                    for k in range(8):
                        ins = e.matmul(bx.t[:, 0:Tn], win.t[:, fo // 2, k, (fo % 2) * 128:(fo % 2 + 1) * 128], xn.t[:, k, 0:Tn], start=(k == 0), stop=(k == 7))
                    return ins
                P.op("pe", mm, [win.r, xn.r], [bx.r])
                if fo < 8:
                    P.op("act", lambda e, bx=bx, fo=fo: e.activation(out=uT.t[:, fo, 0:Tn], in_=bx.t[:, 0:Tn], func=AF.Gelu, bias=V(f"cm_b_in_{j}", fo)), [bx.r, vecs.r], [uT.r])
                else:
                    P.op("act", lambda e, bx=bx, fo=fo: e.activation(out=vT.t[:, fo - 8, 0:Tn], in_=bx.t[:, 0:Tn], func=AF.Gelu, bias=V(f"cm_b_in_{j}", fo)), [bx.r, vecs.r], [vT.r])
            P.op("act", lambda e: e.activation(out=vsq.t[:, :, 0:Tn], in_=vT.t[:, :, 0:Tn], func=AF.Square), [vT.r], [vsq.r])
            bM1, bM2 = banks[2], banks[3]

            def mm1(e):
                for k in range(8):
                    ins = e.matmul(bM1.t[:, 0:Tn], C("onesP", 128, 128), vT.t[:, k, 0:Tn], start=(k == 0), stop=(k == 7))
                return ins
            P.op("pe", mm1, [vT.r, consts.r], [bM1.r])

            def mm2(e):
                for k in range(8):
                    ins = e.matmul(bM2.t[:, 0:Tn], onesb, vsq.t[:, k, 0:Tn], start=(k == 0), stop=(k == 7))
                return ins
            P.op("pe", mm2, [vsq.r, cbf.r], [bM2.r])
            P.op("dve", lambda e: e.tensor_scalar(out=mu.t[:, 0:Tn], in0=bM1.t[:, 0:Tn], scalar1=1.0 / D, scalar2=None, op0=ALU.mult), [bM1.r], [mu.r])
            P.op("dve", lambda e: e.tensor_tensor(out=var.t[:, 0:Tn], in0=mu.t[:, 0:Tn], in1=mu.t[:, 0:Tn], op=ALU.mult), [mu.r], [var.r])
            P.op("dve", lambda e: e.scalar_tensor_tensor(out=var.t[:, 0:Tn], in0=bM2.t[:, 0:Tn], scalar=1.0 / D, in1=var.t[:, 0:Tn], op0=ALU.mult, op1=ALU.subtract),
                 [bM2.r, var.r], [var.r])
            P.op("act", lambda e: e.activation(out=var.t[:, 0:Tn], in_=var.t[:, 0:Tn], func=AF.Sqrt, bias=V("c_eps")), [var.r, vecs.r], [var.r])
            P.op("dve", lambda e: e.reciprocal(out=var.t[:, 0:Tn], in_=var.t[:, 0:Tn]), [var.r], [var.r])
            for k in range(8):
                tt = tmp[k % 2]
                P.op("dve", lambda e, k=k, tt=tt: e.tensor_tensor(out=tt.t[:, 0:Tn], in0=vT.t[:, k, 0:Tn], in1=mu.t[:, 0:Tn], op=ALU.subtract), [vT.r, mu.r], [tt.r])
                P.op("dve", lambda e, k=k, tt=tt: e.scalar_tensor_tensor(out=vn.t[:, k, 0:Tn], in0=tt.t[:, 0:Tn], scalar=V(f"cm_ln_g_{j}", k), in1=var.t[:, 0:Tn],
                                                                       op0=ALU.mult, op1=ALU.mult), [tt.r, var.r, vecs.r], [vn.r])
                if samp:
                    P.op("dve", lambda e, k=k, tt=tt: e.scalar_tensor_tensor(out=vnf.t[:, k, 0:Tn], in0=tt.t[:, 0:Tn], scalar=V(f"cm_ln_g_{j}", k), in1=var.t[:, 0:Tn],
                                                                           op0=ALU.mult, op1=ALU.mult), [tt.r, var.r, vecs.r], [vnf.r])
            if samp:
                P.dma("sp", vsT_o[j], vnf.t[:], reads=[vnf.r])
            bT = banks[4]
            bTb = bT.t[:].bitcast(BF16)
            wsb = wsSb if samp else wsPb
            bsb = bsS if samp else bsP
            for c in range(nch):
                cs = slice(c * Pn, (c + 1) * Pn)

                def mmt(e, cs=cs):
                    for k in range(8):
                        ins = e.transpose(out=bTb[0:Pn, k * 128:(k + 1) * 128], in_=vn.t[:, k, cs], identity=identb)
                    return ins
                P.op("pe", mmt, [vn.r, cbf.r], [bT.r])
                P.op("act", lambda e: e.activation(out=vn_tok.t[0:Pn, :], in_=bTb[0:Pn, :], func=AF.Copy), [bT.r], [vn_tok.r])
                for k in range(8):
                    g = k // 2
                    bmx = banks[cnt % 2]
                    cnt += 1
                    tt = tmp[k % 2]
                    P.op("pe", lambda e, bmx=bmx, k=k, g=g: e.matmul(bmx.t[:, 0:Pn], vn_tok.t[0:Pn, k * 128:(k + 1) * 128], wsb.t[0:Pn, g, 0:Pn], start=True, stop=True),
                         [vn_tok.r, wsb.r], [bmx.r])
                    P.op("dve", lambda e, bmx=bmx, tt=tt, g=g: e.tensor_tensor(out=tt.t[:, 0:Pn], in0=bmx.t[:, 0:Pn], in1=bsb.t[:, g, 0:Pn], op=ALU.add), [bmx.r, bsb.r], [tt.r])
                    P.op("dve", lambda e, tt=tt, k=k, cs=cs: e.tensor_tensor(out=yT.t[:, k, cs], in0=tt.t[:, 0:Pn], in1=uT.t[:, k, cs], op=ALU.mult), [tt.r, uT.r], [yT.r])
            for d in range(8):
                bk = banks[cnt % 2]
                cnt += 1
                wdt = next_wB()
                wv = wdt.t[:, 0:1024].rearrange("p (f c) -> p f c", f=8)
                P.dma("pool", wv, wout_d[j][d], writes=[wdt.r])

                def mm(e, bk=bk, wv=wv):
                    for f in range(8):
                        ins = e.matmul(bk.t[:, 0:Tn], wv[:, f, :], yT.t[:, f, 0:Tn], start=(f == 0), stop=(f == 7))
                    return ins
                P.op("pe", mm, [wdt.r, yT.r], [bk.r])
                P.op("dve", lambda e, bk=bk, d=d: e.tensor_tensor(out=x.t[:, d, t0:t0 + Tn], in0=bk.t[:, 0:Tn], in1=x.t[:, d, t0:t0 + Tn], op=ALU.add), [bk.r, x[ti]], [x[ti]])

    if dbg == "ffn":
        ffn(0, 1)
    for i in range(n_layers):
        ffn(i, 1)
        if i % 2 == 0:
            if dbg != "onlysamp":
                mlstm(i, i // 2, False)
            if dbg == "newsamp":
                mlstm(i, i // 2, True)
            elif dbg != "nosamp":
                mlstm_v1_sample(i, i // 2)
        else:
            chunk_mlp(i, i // 2)
        ffn(i, 2)

    P.phase()
    sqb = P.atile([128, 8, 512], BF16)
    rst = P.atile([128, 512], F32)
    yst = [P.atile([128, 8, 512], F32) for _ in range(2)]
    yv = yT_o.rearrange("(k p) t -> p k t", p=128)
    for ti, (t0, n) in enumerate(FFN_TILES):
        ys = yst[ti % 2]
        rmsnorm(t0, n, "norm_final", ys, ys.r, sqb, rst, banks[6])
        P.dma("sp", yv[:, :, t0:t0 + n], ys.t[:, :, 0:n], reads=[ys.r])
    P.emit()
    return nc, P


def _cols(v):
    v = np.asarray(v, np.float32)
    return np.ascontiguousarray(v.reshape(-1, 128).T)


def _blk_k(W, ncol):
    K, F = W.shape
    return np.ascontiguousarray(W.reshape(K // 128, 128, F // ncol, ncol).transpose(2, 1, 0, 3))


def _blk_f(W, ncol):
    Fin, Dn = W.shape
    return np.ascontiguousarray(W.reshape(Fin // 128, 128, Dn // ncol, ncol).transpose(2, 1, 0, 3))


def prep_inputs(inp):
    f32 = np.float32
    g = {k: np.asarray(v) for k, v in inp.items()}
    shared = {}
    vecs = np.zeros((128, VL["_n"]), f32)

    def put(name, v):
        c = _cols(v)
        vecs[:, VL[name]:VL[name] + c.shape[1]] = c
    for i in range(DEPTH):
        put(f"norm_ff1_{i}", g["norm_ff1"][i])
        put(f"norm_mix_{i}", g["norm_mix"][i])
        put(f"norm_ff2_{i}", g["norm_ff2"][i])
    put("norm_final", g["norm_final"])
    for j in range(2):
        for w in range(4):
            put(f"ml_wconv_{j}_{w}", g["ml_w_conv"][j, w])
        put(f"ml_bconv_{j}", g["ml_b_conv"][j])
        put(f"ml_hn_g_{j}", g["ml_hn_g"][j])
        put(f"ml_skip_{j}", g["ml_skip"][j])
        put(f"cm_b_in_{j}", g["cm_b_in"][j])
        put(f"cm_ln_g_{j}", g["cm_ln_g"][j])
    vecs[:, VL["c_eps"]] = EPS
    vecs[:, VL["c_lnscale"]] = math.log(DH ** -0.5)
    shared["vecs"] = vecs
    cst = np.zeros((128, CL["_n"]), f32)
    cst[:, CL["ident"]:CL["ident"] + 128] = np.eye(128, dtype=f32)
    cst[:, CL["maskP"]:CL["maskP"] + 128] = np.triu(np.ones((128, 128), f32))
    cst[:, CL["onesP"]:CL["onesP"] + 128] = 1.0
    cst[:64, CL["maskS"]:CL["maskS"] + 64] = np.kron(np.eye(NB, dtype=f32), np.triu(np.ones((4, 4), f32)))
    cst[:64, CL["onesS"]:CL["onesS"] + 64] = np.kron(np.eye(NB, dtype=f32), np.ones((4, 4), f32))
    cst[:64, CL["seqS"]:CL["seqS"] + 16] = np.kron(np.eye(NB, dtype=f32), np.ones((4, 1), f32))
    fs = np.zeros((64, 16), f32)
    fs[np.arange(16) * 4, np.arange(16)] = 1.0
    cst[:64, CL["firstS"]:CL["firstS"] + 16] = fs
    cst[:, CL["seqP"]] = 1.0
    cst[0, CL["firstP"]] = 1.0
    shared["consts"] = cst
    cm = np.zeros((128, NB, TS), f32)
    for b in range(NB):
        cm[:, b, 4 * b:4 * b + 4] = 1.0
    shared["colmask"] = cm
    for i in range(DEPTH):
        for a in (1, 2):
            shared[f"wg{i}{a}"] = _blk_k(g[f"ffn{a}_w_gate"][i], 256)
            shared[f"wu{i}{a}"] = _blk_k(g[f"ffn{a}_w_up"][i], 256)
            shared[f"wd{i}{a}"] = _blk_f(g[f"ffn{a}_w_down"][i], 256)
    for j in range(2):
        shared[f"wup{j}"] = _blk_k(g["ml_w_up"][j], 256)
        shared[f"wdn{j}"] = _blk_f(g["ml_w_down"][j], 128)
        bd = np.zeros((128, 3, 16, 128), f32)
        for wi, nm in enumerate(["ml_w_q", "ml_w_k", "ml_w_v"]):
            w = g[nm][j].reshape(16, 32, 4, 4)
            for n in range(32):
                bd[4 * n:4 * n + 4, wi, :, 4 * n:4 * n + 4] = w[:, n].transpose(1, 0, 2)
        shared[f"bd{j}"] = bd
        wcat = np.concatenate([g["ml_w_ig"][j], g["ml_w_fg"][j]], axis=1)
        shared[f"wgate{j}"] = np.ascontiguousarray(wcat.reshape(48, 128, 8).transpose(1, 0, 2))
        shared[f"bgate{j}"] = np.ascontiguousarray(np.broadcast_to(np.concatenate([g["ml_b_ig"][j], g["ml_b_fg"][j]])[None, :], (128, 8))).astype(f32)
        shared[f"win{j}"] = _blk_k(g["cm_w_in"][j], 256)
        shared[f"wout{j}"] = _blk_f(g["cm_w_out"][j], 128)
        ws = g["cm_w_s"][j]
        shared[f"wsP{j}"] = np.ascontiguousarray(ws.transpose(2, 0, 1))
        wss = np.zeros((64, 4, 64), f32)
        for b in range(NB):
            wss[4 * b:4 * b + 4, :, 4 * b:4 * b + 4] = ws[:, :4, :4].transpose(2, 0, 1)
        shared[f"wsS{j}"] = wss
        bs = g["cm_b_s"][j]
        shared[f"bsP{j}"] = np.ascontiguousarray(np.broadcast_to(bs[None, :, :], (128, 4, 128))).astype(f32)
        shared[f"bsS{j}"] = np.ascontiguousarray(np.broadcast_to(np.tile(bs[:, :4], (1, NB))[None, :, :], (128, 4, 64))).astype(f32)
    in_maps = []
    for c in range(N_CORES):
        m = dict(shared)
        sl = slice(c * NB, (c + 1) * NB)
        xp = g["x_prompt"][c]
        xs = g["x_sample"][sl].reshape(TS, D)
        m["xT"] = np.ascontiguousarray(np.concatenate([xp, xs], axis=0).T)
        m["sC"] = np.ascontiguousarray(g["state_C"][:, sl])
        sn = g["state_n"][:, sl]
        m["snT"] = np.ascontiguousarray(sn.reshape(2, NB, 16, 128).transpose(0, 3, 2, 1))
        m["sm"] = np.ascontiguousarray(np.repeat(g["state_m"][:, sl], 4, axis=1))
        sc = g["state_conv"][:, sl]
        m["sconvT"] = np.ascontiguousarray(sc.reshape(2, NB, 3, 16, 128).transpose(0, 4, 3, 1, 2))
        in_maps.append(m)
    return in_maps


def assemble(results, n_cores=N_CORES):
    f32 = np.float32
    y_p = np.zeros((8, TP, D), f32)
    y_s = np.zeros((128, 4, D), f32)
    C_p = np.zeros((2, 8, H, DH, DH), f32)
    n_p = np.zeros((2, 8, H, DH), f32)
    m_p = np.zeros((2, 8, H), f32)
    cv_p = np.zeros((2, 8, 3, INNER), f32)
    C_s = np.zeros((2, 128, H, DH, DH), f32)
    n_s = np.zeros((2, 128, H, DH), f32)
    m_s = np.zeros((2, 128, H), f32)
    cv_s = np.zeros((2, 128, 3, INNER), f32)
    v_s = np.zeros((2, 128, 4, D), f32)
    for c in range(n_cores):
        r = results[c]
        sl = slice(c * NB, (c + 1) * NB)
        yT = r["yT"]
        y_p[c] = yT[:, :TP].T
        y_s[sl] = yT[:, TP:].T.reshape(NB, 4, D)
        C_p[:, c] = r["Cp"]
        n_p[:, c] = r["npT"][:, :, :, 0].transpose(0, 2, 1).reshape(2, H, DH)
        m_p[:, c] = r["mp"][:, 0, :]
        cv_p[:, c] = r["convpT"].transpose(0, 3, 2, 1).reshape(2, 3, INNER)
        C_s[:, sl] = r["Cs"]
        n_s[:, sl] = r["nsT"].transpose(0, 3, 2, 1).reshape(2, NB, H, DH)
        m_s[:, sl] = r["ms"][:, ::4, :]
        cv_s[:, sl] = r["convsT"].transpose(0, 3, 4, 2, 1).reshape(2, NB, 3, INNER)
        v_s[:, sl] = r["vsT"].transpose(0, 3, 2, 1).reshape(2, NB, 4, D)
    return (y_p, y_s, C_p, n_p, m_p, cv_p, C_s, n_s, m_s, cv_s, v_s)


_NC_CACHE = {}


def kernel(**inputs):
    in_maps = prep_inputs(inputs)
    if "nc" not in _NC_CACHE:
        _NC_CACHE["nc"] = build_program()[0]
    nc = _NC_CACHE["nc"]
    res = run_bass_kernel_spmd(nc, in_maps, core_ids=list(range(N_CORES)))
    return assemble(res.results)
```

```python
from contextlib import ExitStack
import math
import types
import numpy as np
import concourse.bass as bass
import concourse.mybir as mybir
from concourse.bass_utils import run_bass_kernel_spmd

F32 = mybir.dt.float32
BF16 = mybir.dt.bfloat16
AF = mybir.ActivationFunctionType
ALU = mybir.AluOpType
AX = mybir.AxisListType

DEPTH = 4
D = 1024
TP = 2048
TS = 64
TT = TP + TS
FF = 2816
NFC = 22
INNER = 2048
H = 4
DH = 512
EPS = 1e-6
NB = 16
N_CORES = 8

ENGS = ("pe", "act", "dve", "pool", "sp")
N_LANES = 8


def _freeze(fn, depth=0):
    if not isinstance(fn, types.FunctionType) or fn.__closure__ is None or depth > 4:
        return fn
    cells = []
    for c in fn.__closure__:
        try:
            v = c.cell_contents
        except ValueError:
            cells.append(c)
            continue
        if isinstance(v, types.FunctionType):
            v = _freeze(v, depth + 1)
        cells.append(types.CellType(v))
    g = types.FunctionType(fn.__code__, fn.__globals__, fn.__name__, fn.__defaults__, tuple(cells))
    g.__kwdefaults__ = fn.__kwdefaults__
    return g


class Res:
    __slots__ = ("lw", "rd")

    def __init__(self, lw=None):
        self.lw = lw
        self.rd = []


class Tile:
    def __init__(self, t, base_lw=None):
        self.t = t
        self._res = {}
        self.base_lw = base_lw

    def __getitem__(self, key):
        r = self._res.get(key)
        if r is None:
            r = self._res[key] = Res(self.base_lw)
        return r

    @property
    def r(self):
        return self[None]


class Op:
    __slots__ = ("eng", "fn", "deps", "dma", "lane", "val", "signal", "idx")


class Prog:
    def __init__(self, nc, arena_words):
        self.nc = nc
        self.ops = []
        self.stack = ExitStack()
        self.n_t = 0
        self.arena = self.stack.enter_context(nc.sbuf_tensor("arena", [128, arena_words], F32))
        self.arena_words = arena_words
        self.arena_ptr = 0
        self.arena_tiles = []
        self.cur_barrier = None
        self.dummy = self.sbuf([128, 8], F32, "dummy_bar")

    def sbuf(self, shape, dtype, name=None):
        self.n_t += 1
        t = self.stack.enter_context(self.nc.sbuf_tensor("sb_" + (name or f"t{self.n_t}"), list(shape), dtype))
        tl = Tile(t)
        self.persist = getattr(self, "persist", [])
        self.persist.append(tl)
        return tl

    def psum(self, shape, dtype, name=None):
        self.n_t += 1
        t = self.stack.enter_context(self.nc.psum_tensor("ps_" + (name or f"t{self.n_t}"), list(shape), dtype))
        tl = Tile(t)
        self.persist = getattr(self, "persist", [])
        self.persist.append(tl)
        return tl

    def phase(self):
        ress = []
        for tl in self.arena_tiles:
            ress.extend(tl._res.values())
        if ress:
            d = self.dummy
            o = self.op("pool", lambda e: e.memset(d.t[:, 0:1], 0.0), [], ress + [d.r])
            self.cur_barrier = o.idx
        self.arena_tiles = []
        self.arena_ptr = 0

    def barrier_all(self):
        ress = []
        for tl in self.arena_tiles + getattr(self, "persist", []):
            ress.extend(tl._res.values())
        d = self.dummy
        if d.r not in ress:
            ress.append(d.r)
        self.op("pool", lambda e: e.memset(d.t[:, 0:1], 0.0), [], ress)

    def atile(self, shape, dtype):
        n = 1
        for s in shape[1:]:
            n *= s
        words = (n * (2 if dtype == BF16 else 4) + 3) // 4
        words = (words + 7) // 8 * 8
        a0 = self.arena_ptr
        self.arena_ptr += words
        assert self.arena_ptr <= self.arena_words, f"arena overflow {self.arena_ptr} > {self.arena_words}"
        ap = self.arena.__getitem__((slice(0, shape[0]), slice(a0, a0 + words)))
        if dtype == BF16:
            ap = ap.bitcast(BF16)
        ap = ap[:, 0:n]
        if len(shape) == 3:
            ap = ap.rearrange("p (a b) -> p a b", a=shape[1])
        elif len(shape) == 4:
            ap = ap.rearrange("p (a b c) -> p a b c", a=shape[1], b=shape[2])
        tl = Tile(ap, self.cur_barrier)
        self.arena_tiles.append(tl)
        return tl

    def op(self, eng, fn, reads=(), writes=(), dma=False):
        o = Op()
        o.eng, o.fn, o.dma = eng, _freeze(fn), dma
        o.idx = len(self.ops)
        o.lane = None
        o.val = None
        o.signal = False
        deps = set()
        for r in reads:
            if r.lw is not None:
                deps.add(r.lw)
        for w in writes:
            if w.lw is not None:
                deps.add(w.lw)
            for x in w.rd:
                deps.add(x)
        deps.discard(o.idx)
        o.deps = deps
        for r in reads:
            r.rd.append(o.idx)
        for w in writes:
            w.lw = o.idx
            w.rd = []
        self.ops.append(o)
        return o

    def dma(self, eng, out_ap, in_ap, reads=(), writes=()):
        return self.op(eng, lambda e: e.dma_start(out=out_ap, in_=in_ap, allow_slow_non_contiguous=True), reads, writes, dma=True)

    def emit(self):
        nc = self.nc
        ops = self.ops
        fin = Op()
        fin.eng, fin.fn, fin.dma = "sp", None, False
        fin.idx = len(ops)
        fin.deps = {o.idx for o in ops if o.dma}
        fin.lane = None
        fin.val = None
        fin.signal = False
        ops.append(fin)
        for o in ops:
            nd = set()
            for d in o.deps:
                p = ops[d]
                if (not p.dma) and (not o.dma) and p.eng == o.eng and o.eng == "pe":
                    continue
                nd.add(d)
            o.deps = nd
        for o in ops:
            for d in o.deps:
                ops[d].signal = True
        st = self.stack
        sems = {e: st.enter_context(nc.semaphore(f"s_{e}")) for e in ENGS}
        lanes = {e: [st.enter_context(nc.semaphore(f"l_{e}{i}")) for i in range(N_LANES)] for e in ("sp", "pool")}
        cnt = {e: 0 for e in ENGS}
        lane_cnt = {e: [0] * N_LANES for e in lanes}
        lane_prev = {e: [None] * N_LANES for e in lanes}
        dma_seq = {e: 0 for e in lanes}
        for o in ops:
            if o.dma:
                i = dma_seq[o.eng] % N_LANES
                dma_seq[o.eng] += 1
                o.lane = (o.eng, i)
                prev = lane_prev[o.eng][i]
                if prev is not None:
                    o.deps.add(prev)
                lane_cnt[o.eng][i] += 16
                o.val = lane_cnt[o.eng][i]
                lane_prev[o.eng][i] = o.idx
            elif o.signal:
                cnt[o.eng] += 1
                o.val = cnt[o.eng]

        def sem_of(p):
            if p.dma:
                return lanes[p.lane[0]][p.lane[1]], ("L",) + p.lane
            return sems[p.eng], ("E", p.eng)

        by_eng = {e: [o for o in ops if o.eng == e] for e in ENGS}
        self.stats = {e: len(by_eng[e]) for e in ENGS}
        blk = st.enter_context(nc.Block())

        def run(eng_name, eng):
            known = {}
            for o in by_eng[eng_name]:
                need = {}
                for d in o.deps:
                    p = ops[d]
                    s, key = sem_of(p)
                    if known.get(key, 0) >= p.val:
                        continue
                    if key not in need or need[key][1] < p.val:
                        need[key] = (s, p.val)
                for key, (s, v) in need.items():
                    eng.wait_ge(s, v)
                    known[key] = v
                if o.fn is None:
                    continue
                ins = o.fn(eng)
                if o.dma:
                    ins.then_inc(lanes[o.lane[0]][o.lane[1]], 16)
                elif o.signal:
                    ins.then_inc(sems[o.eng], 1)

        @blk.tensor
        def _(e):
            run("pe", e)

        @blk.scalar
        def _(e):
            run("act", e)

        @blk.vector
        def _(e):
            run("dve", e)

        @blk.gpsimd
        def _(e):
            run("pool", e)

        @blk.sync
        def _(e):
            run("sp", e)

        st.close()


def vec_layout():
    lay = {}
    c = 0

    def add(name, n):
        nonlocal c
        lay[name] = c
        c += n

    for i in range(DEPTH):
        add(f"norm_ff1_{i}", 8)
        add(f"norm_mix_{i}", 8)
        add(f"norm_ff2_{i}", 8)
    add("norm_final", 8)
    for j in range(2):
        for w in range(4):
            add(f"ml_wconv_{j}_{w}", 16)
        add(f"ml_bconv_{j}", 16)
        add(f"ml_hn_g_{j}", 16)
        add(f"ml_skip_{j}", 16)
        add(f"cm_b_in_{j}", 16)
        add(f"cm_ln_g_{j}", 8)
    add("c_eps", 1)
    add("c_lnscale", 1)
    lay["_n"] = c
    return lay


def const_layout():
    lay = {}
    c = 0
    for name, n in [("ident", 128), ("maskP", 128), ("onesP", 128), ("maskS", 64), ("onesS", 64),
                    ("seqS", 16), ("firstS", 16), ("seqP", 1), ("firstP", 1)]:
        lay[name] = c
        c += n
    lay["_n"] = c
    return lay


VL = vec_layout()
CL = const_layout()

FFN_TILES = [(0, 512), (512, 512), (1024, 512), (1536, 512), (2048, 64)]
FFN_PARTS = [(0, 4), (4, 8), (8, 11)]
ML_T = 256


def build_program(n_layers=DEPTH):
    nc = bass.Bass("TRN2", target_bir_lowering=False)
    import os
    ARENA_WORDS = 28712
    P = Prog(nc, ARENA_WORDS)

    def din(name, shape):
        return nc.dram_tensor(name, list(shape), F32, kind="ExternalInput").ap()

    def dout(name, shape):
        return nc.dram_tensor(name, list(shape), F32, kind="ExternalOutput").ap()

    xT_d = din("xT", [D, TT])
    vecs_d = din("vecs", [128, VL["_n"]])
    consts_d = din("consts", [128, CL["_n"]])
    colmask_d = din("colmask", [128, NB, TS])
    wg_d, wu_d, wd_d = {}, {}, {}
    for i in range(DEPTH):
        for a in (1, 2):
            wg_d[i, a] = din(f"wg{i}{a}", [11, 128, 8, 256])
            wu_d[i, a] = din(f"wu{i}{a}", [11, 128, 8, 256])
            wd_d[i, a] = din(f"wd{i}{a}", [4, 128, NFC, 256])
    wup_d = [din(f"wup{j}", [16, 128, 8, 256]) for j in range(2)]
    wdn_d = [din(f"wdn{j}", [8, 128, 16, 128]) for j in range(2)]
    bd_d = [din(f"bd{j}", [128, 3, 16, 128]) for j in range(2)]
    wgate_d = [din(f"wgate{j}", [128, 48, 8]) for j in range(2)]
    bgate_d = [din(f"bgate{j}", [128, 8]) for j in range(2)]
    win_d = [din(f"win{j}", [8, 128, 8, 256]) for j in range(2)]
    wout_d = [din(f"wout{j}", [8, 128, 8, 128]) for j in range(2)]
    wsP_d = [din(f"wsP{j}", [128, 4, 128]) for j in range(2)]
    wsS_d = [din(f"wsS{j}", [64, 4, 64]) for j in range(2)]
    bsP_d = [din(f"bsP{j}", [128, 4, 128]) for j in range(2)]
    bsS_d = [din(f"bsS{j}", [128, 4, 64]) for j in range(2)]
    sC_d = din("sC", [2, NB, H, DH, DH])
    snT_d = din("snT", [2, 128, 16, NB])
    sm_d = din("sm", [2, TS, 4])
    sconv_d = din("sconvT", [2, 128, 16, NB, 3])

    yT_o = dout("yT", [D, TT])
    Cp_o = dout("Cp", [2, H, DH, DH])
    npT_o = dout("npT", [2, 128, 16, 1])
    mp_o = dout("mp", [2, 128, 4])
    convp_o = dout("convpT", [2, 128, 16, 3])
    Cs_o = dout("Cs", [2, NB, H, DH, DH])
    nsT_o = dout("nsT", [2, 128, 16, NB])
    ms_o = dout("ms", [2, TS, 4])
    convs_o = dout("convsT", [2, 128, 16, NB, 3])
    vsT_o = dout("vsT", [2, 128, 8, TS])

    x = P.sbuf([128, 8, TT], F32, "x")
    vecs = P.sbuf([128, VL["_n"]], F32, "vecs")
    consts = P.sbuf([128, CL["_n"]], F32, "consts")
    cbf = P.sbuf([128, 512], BF16, "cbf")
    wA = [P.sbuf([128, 8, 256], BF16, f"wA{i}") for i in range(4)]
    wB = [P.sbuf([128, 2048], BF16, f"wB{i}") for i in range(2)]
    banks = [P.psum([128, 512], F32, f"bank{i}") for i in range(8)]

    def V(name, k=0, p=128):
        c = VL[name] + k
        return vecs.t[0:p, c:c + 1]

    def C(name, p, n):
        c = CL[name]
        return consts.t[0:p, c:c + n]

    P.dma("sp", vecs.t[:], vecs_d, writes=[vecs.r])
    P.dma("sp", consts.t[:], consts_d, writes=[consts.r])
    xv = xT_d.rearrange("(k p) t -> p k t", p=128)
    for ti, (t0, n) in enumerate(FFN_TILES):
        P.dma("sp", x.t[:, :, t0:t0 + n], xv[:, :, t0:t0 + n], writes=[x[ti]])
    P.op("dve", lambda e: e.tensor_copy(out=cbf.t[:, 0:128], in_=consts.t[:, CL["ident"]:CL["ident"] + 128]), [consts.r], [cbf.r])
    P.op("dve", lambda e: e.tensor_copy(out=cbf.t[:, 128:256], in_=consts.t[:, CL["onesP"]:CL["onesP"] + 128]), [consts.r], [cbf.r])
    P.op("dve", lambda e: e.tensor_copy(out=cbf.t[:, 256:272], in_=consts.t[:, CL["seqS"]:CL["seqS"] + 16]), [consts.r], [cbf.r])
    P.op("dve", lambda e: e.tensor_copy(out=cbf.t[:, 272:273], in_=consts.t[:, CL["seqP"]:CL["seqP"] + 1]), [consts.r], [cbf.r])
    identb = cbf.t[:, 0:128]
    onesb = cbf.t[:, 128:256]

    def xres(t0, n):
        out = []
        for ti, (a, m) in enumerate(FFN_TILES):
            if a < t0 + n and t0 < a + m:
                out.append(x[ti])
        return out

    import os
    dbg = os.environ.get("KDBG", "")
    dbg_stage = None
    wA_i = [0]
    wB_i = [0]

    def next_wA():
        t = wA[wA_i[0] % len(wA)]
        wA_i[0] += 1
        return t

    def next_wB():
        t = wB[wB_i[0] % len(wB)]
        wB_i[0] += 1
        return t

    def rmsnorm(t0, n, gname, dst, dst_res, sqb, rst, bank, f32_out=False):
        xr = xres(t0, n)
        P.op("act", lambda e: e.activation(out=sqb.t[:, :, 0:n], in_=x.t[:, :, t0:t0 + n], func=AF.Square), xr, [sqb.r])

        def mm(e):
            for k in range(8):
                ins = e.matmul(bank.t[:, 0:n], onesb, sqb.t[:, k, 0:n], start=(k == 0), stop=(k == 7))
            return ins
        P.op("pe", mm, [sqb.r, cbf.r], [bank.r])
        P.op("act", lambda e: e.activation(out=rst.t[:, 0:n], in_=bank.t[:, 0:n], func=AF.Sqrt, scale=1.0 / D, bias=V("c_eps")), [bank.r, vecs.r], [rst.r])
        P.op("dve", lambda e: e.reciprocal(out=rst.t[:, 0:n], in_=rst.t[:, 0:n]), [rst.r], [rst.r])
        for k in range(8):
            P.op("dve", lambda e, k=k: e.scalar_tensor_tensor(out=dst.t[:, k, 0:n], in0=x.t[:, k, t0:t0 + n], scalar=V(gname, k),
                                                                in1=rst.t[:, 0:n], op0=ALU.mult, op1=ALU.mult),
                 xr + [rst.r, vecs.r], [dst_res])

    def ffn(i, a):
        P.phase()
        gname = f"norm_ff{a}_{i}"
        xn = P.atile([128, 8, TT], BF16)
        hbuf = P.atile([128, 8, TT], BF16)
        sqb = P.atile([128, 8, 512], BF16)
        rst = [P.atile([128, 512], F32) for _ in range(2)]
        sg = [P.atile([128, 512], F32) for _ in range(2)]
        for ti, (t0, n) in enumerate(FFN_TILES):
            xn_v = Tile(xn.t[:, :, t0:t0 + n])
            rmsnorm(t0, n, gname, xn_v, xn[ti], sqb, rst[ti % 2], banks[6])
        cnt = 0
        for (b0, b1) in FFN_PARTS:
            for blk in range(b0, b1):
                wgt, wut = next_wA(), next_wA()
                P.dma("pool", wgt.t[:], wg_d[i, a][blk], writes=[wgt.r])
                P.dma("pool", wut.t[:], wu_d[i, a][blk], writes=[wut.r])
                for fc in range(2):
                    fl = (blk - b0) * 2 + fc
                    for ti, (t0, n) in enumerate(FFN_TILES):
                        bg, bu = banks[cnt % 2], banks[2 + cnt % 2]
                        sgt = sg[cnt % 2]
                        cnt += 1

                        def mm(e, wt=wgt, bk=bg, fc=fc, t0=t0, n=n):
                            for k in range(8):
                                ins = e.matmul(bk.t[:, 0:n], wt.t[:, k, fc * 128:(fc + 1) * 128], xn.t[:, k, t0:t0 + n], start=(k == 0), stop=(k == 7))
                            return ins
                        P.op("pe", mm, [wgt.r, xn[ti]], [bg.r])

                        def mm2(e, wt=wut, bk=bu, fc=fc, t0=t0, n=n):
                            for k in range(8):
                                ins = e.matmul(bk.t[:, 0:n], wt.t[:, k, fc * 128:(fc + 1) * 128], xn.t[:, k, t0:t0 + n], start=(k == 0), stop=(k == 7))
                            return ins
                        P.op("pe", mm2, [wut.r, xn[ti]], [bu.r])
                        P.op("act", lambda e, bk=bg, s=sgt, n=n: e.activation(out=s.t[:, 0:n], in_=bk.t[:, 0:n], func=AF.Silu), [bg.r], [sgt.r])
                        P.op("dve", lambda e, bk=bu, s=sgt, fl=fl, t0=t0, n=n: e.tensor_tensor(out=hbuf.t[:, fl, t0:t0 + n], in0=s.t[:, 0:n], in1=bk.t[:, 0:n], op=ALU.mult),
                             [bu.r, sgt.r], [hbuf[ti]])
            nfc = (b1 - b0) * 2
            for dp in range(4):
                wdt = next_wB()
                wv = wdt.t[:, 0:nfc * 256].rearrange("p (f c) -> p f c", f=nfc)
                P.dma("pool", wv, wd_d[i, a][dp, :, b0 * 2:b1 * 2, :], writes=[wdt.r])
                for dc in range(2):
                    d = dp * 2 + dc
                    for ti, (t0, n) in enumerate(FFN_TILES):
                        bk = banks[4 + cnt % 2]
                        cnt += 1

                        def mm(e, wv=wv, bk=bk, dc=dc, t0=t0, n=n, nfc=nfc):
                            for f in range(nfc):
                                ins = e.matmul(bk.t[:, 0:n], wv[:, f, dc * 128:(dc + 1) * 128], hbuf.t[:, f, t0:t0 + n], start=(f == 0), stop=(f == nfc - 1))
                            return ins
                        P.op("pe", mm, [wdt.r, hbuf[ti]], [bk.r])
                        P.op("dve", lambda e, bk=bk, d=d, t0=t0, n=n: e.scalar_tensor_tensor(out=x.t[:, d, t0:t0 + n], in0=bk.t[:, 0:n], scalar=0.5,
                                                                                         in1=x.t[:, d, t0:t0 + n], op0=ALU.mult, op1=ALU.add),
                             [bk.r, x[ti]], [x[ti]])

    def mlstm(i, j, samp):
        P.phase()
        gname = f"norm_mix_{i}"
        T = TS if samp else ML_T
        nseq, L, Pn, nch = (NB, 4, TS, 1) if samp else (1, 128, 128, 2)
        csn, csL = (NB, 4) if samp else (1, T)
        t0s = [TP] if samp else list(range(0, TP, T))
        NS4 = nch * nseq * 4
        bd = P.atile([128, 3, 16, 128], BF16)
        wgate = P.atile([128, 48, 8], BF16)
        bgate = P.atile([128, 8], F32)
        xn = P.atile([128, 8, T], BF16)
        xmh = P.atile([128, 16, csn, 3 + csL], BF16)
        xc = P.atile([128, 16, T], BF16)
        acc = [P.atile([128, T], F32) for _ in range(4)]
        qT = P.atile([128, 16, T], BF16)
        kT = P.atile([128, 16, T], BF16)
        sqb = Tile(kT.t[:, 0:8, :], kT.base_lw)
        sqb._res = kT._res
        vTt = [P.atile([128, T], BF16) for _ in range(2)]
        v_tk = [P.atile([128, 512], BF16) for _ in range(2)]
        kp_tk = [P.atile([128, 512], BF16) for _ in range(2)]
        Cbf = [P.atile([128, 4, 512], BF16) for _ in range(2)]
        nst = P.atile([128, 16, nseq], F32)
        nbf = P.atile([128, 16, nseq], BF16)
        ST = [P.atile([128, 128], BF16) for _ in range(4)]
        hn_tok = P.atile([128, nch, 2048], BF16)
        ea = [P.atile([128, T], F32) for _ in range(2)]
        rst = ea[0]
        sz = acc
        gT_sb = Tile(ea[1].t[0:8, :], ea[1].base_lw)
        gT_sb._res = ea[1]._res
        gg = P.atile([128, nch, 8], F32)
        e1 = P.atile([128, nch, 4], F32)
        lf = P.atile([128, nch, 4], F32)
        bt = P.atile([128, nch, 4], F32)
        bl = P.atile([128, nch, 4], F32)
        aa = P.atile([128, nch, 16], F32)
        RR = P.atile([128, nch, 4], F32)
        mstk = [P.atile([128, nch + 1, 4], F32) for _ in range(2)]
        d1 = P.atile([128, nch, 4], F32)
        es = P.atile([128, nch, 4], F32)
        d2 = P.atile([128, nch, 4], F32)
        c0 = P.atile([128, nch, 4], F32)
        d3 = P.atile([128, nch, 4], F32)
        dfl = P.atile([128, nch, 4], F32)
        amaxT = P.atile([4, nch * nseq], F32)
        amax_exp = P.atile([4, nch * Pn], F32)
        rhsx = P.atile([128, NS4], F32)
        c0bc = P.atile([128, nch, nseq, 4], F32)
        tmpn = P.atile([128, 4, nseq], F32)
        tmpn2 = P.atile([128, 4, nseq], F32)
        nqb = P.atile([128, 4], F32)
        nq = P.atile([128, 3, 4], F32)
        den = P.atile([128, 4], F32)
        t1 = P.atile([128, 4], F32)
        rs = P.atile([128, 4], F32)
        nb = P.atile([128, 4], F32)
        mv = P.atile([128, 4, 2], F32)
        st6 = [P.atile([128, 6], F32) for _ in range(4)]
        oT = qT
        if samp:
            xmc = P.atile([128, 16, TS], BF16)
            tails = P.atile([128, 16, NB, 3], F32)
            colmask = P.atile([128, NB, TS], BF16)
            qm = [P.atile([128, 4, TS], BF16) for _ in range(2)]
            kpm = [P.atile([64, 512], BF16) for _ in range(2)]
            nslot = min(12, (P.arena_words - P.arena_ptr) // 2048)
            assert nslot >= 4, nslot
            Cslots = [P.atile([128, 4, 512], F32) for _ in range(nslot)]
            P.dma("pool", colmask.t[:], colmask_d, writes=[colmask.r])
        else:
            tailp = P.atile([128, 16, 3], F32)
            Cst = P.atile([128, 16, 512], F32)
            P.op("pool", lambda e: e.memset(Cst.t[:], 0.0), [], [Cst[h_] for h_ in range(4)])

        P.dma("pool", bd.t[:], bd_d[j], writes=[bd.r])
        P.dma("pool", wgate.t[:], wgate_d[j], writes=[wgate.r])
        P.dma("sp", bgate.t[:], bgate_d[j], writes=[bgate.r])
        if samp:
            P.dma("sp", nst.t[:], snT_d[j], writes=[nst[h_] for h_ in range(4)])
            P.dma("sp", mstk[1].t[0:TS, nch, :], sm_d[j], writes=[mstk[1].r])
            P.dma("sp", tails.t[:], sconv_d[j], writes=[tails.r])
            P.op("dve", lambda e: e.tensor_copy(out=xmh.t[:, :, :, 0:3], in_=tails.t[:]), [tails.r], [xmh.r] + [xmh[g_] for g_ in range(4)])
        else:
            P.op("pool", lambda e: e.memset(nst.t[:], 0.0), [], [nst[h_] for h_ in range(4)])
            P.op("pool", lambda e: e.memset(mstk[1].t[:], 0.0), [], [mstk[1].r])
            P.op("pool", lambda e: e.memset(xmh.t[:, :, :, 0:3], 0.0), [], [xmh.r] + [xmh[g_] for g_ in range(4)])
        mask = C("maskS" if samp else "maskP", Pn, Pn)
        onesblk = C("onesS" if samp else "onesP", Pn, Pn)
        seqm = C("seqS" if samp else "seqP", Pn, nseq)
        firstm = C("firstS" if samp else "firstP", Pn, nseq)
        seqmb = cbf.t[0:Pn, 256:256 + nseq] if samp else cbf.t[0:Pn, 272:273]
        cnt = [0]
        slot_i = [0]

        def xm_op(fo, a=0, b=None):
            b = T if b is None else b
            if samp:
                return xmc.t[:, fo, a:b]
            return xmh.t[:, fo, 0, 3 + a:3 + b]

        for tidx, t0 in enumerate(t0s):
            last_prompt = (not samp) and (t0 + T == TP)
            mcur, mprev = mstk[tidx % 2], mstk[1 - tidx % 2]
            rmsnorm(t0, T, gname, xn, xn.r, sqb, rst, banks[7])
            P.op("dve", lambda e: e.tensor_copy(out=mcur.t[0:Pn, 0, :], in_=mprev.t[0:Pn, nch, :]), [mprev.r], [mcur.r])

            wts = {}

            def B1(g):
                for fo in range(4 * g, 4 * g + 4):
                    if fo % 2 == 0:
                        wt = next_wA()
                        P.dma("pool", wt.t[:], wup_d[j][fo // 2], writes=[wt.r])
                        wts[fo // 2] = wt
                    wt = wts[fo // 2]
                    fc = fo % 2
                    bx = banks[fo % 2]

                    def mm(e, wt=wt, bx=bx, fc=fc):
                        for k in range(8):
                            ins = e.matmul(bx.t[:, 0:T], wt.t[:, k, fc * 128:(fc + 1) * 128], xn.t[:, k, 0:T], start=(k == 0), stop=(k == 7))
                        return ins
                    P.op("pe", mm, [wt.r, xn.r], [bx.r])
                    bx3 = bx.t[:, 0:T].rearrange("p (s l) -> p s l", s=csn)
                    P.op("act", lambda e, bx3=bx3, fo=fo: e.activation(out=xmh.t[:, fo, :, 3:3 + csL], in_=bx3, func=AF.Copy), [bx.r], [xmh[fo // 4]])
                    if samp:
                        P.op("act", lambda e, bx=bx, fo=fo: e.activation(out=xmc.t[:, fo, :], in_=bx.t[:, 0:T], func=AF.Copy), [bx.r], [xmc.r])
                        P.op("dve", lambda e, bx3=bx3, fo=fo: e.tensor_copy(out=tails.t[:, fo, :, :], in_=bx3[:, :, 1:4]), [bx.r], [tails.r])
                    elif last_prompt:
                        P.op("dve", lambda e, bx=bx, fo=fo: e.tensor_copy(out=tailp.t[:, fo, :], in_=bx.t[:, T - 3:T]), [bx.r], [tailp.r])

            def B2(g):
                fos = range(4 * g, 4 * g + 4)
                for w in range(4):
                    for fo in fos:
                        at = acc[fo % 4]
                        av = at.t[:, 0:T].rearrange("p (s l) -> p s l", s=csn)
                        win = xmh.t[:, fo, :, w:w + csL]
                        if w == 0:
                            P.op("dve", lambda e, av=av, win=win, fo=fo: e.tensor_scalar(out=av, in0=win, scalar1=V(f"ml_wconv_{j}_0", fo), scalar2=V(f"ml_bconv_{j}", fo),
                                                                                         op0=ALU.mult, op1=ALU.add), [xmh[fo // 4], xmh.r, vecs.r], [at.r])
                        else:
                            P.op("dve", lambda e, av=av, win=win, fo=fo, w=w: e.scalar_tensor_tensor(out=av, in0=win, scalar=V(f"ml_wconv_{j}_{w}", fo), in1=av,
                                                                                                   op0=ALU.mult, op1=ALU.add), [xmh[fo // 4], xmh.r, at.r, vecs.r], [at.r])
                for fo in fos:
                    at = acc[fo % 4]
                    P.op("act", lambda e, at=at, fo=fo: e.activation(out=xc.t[:, fo, 0:T], in_=at.t[:, 0:T], func=AF.Silu), [at.r], [xc[fo // 4]])

            def B3(g):
                for fo in range(4 * g, 4 * g + 4):
                    bA, bK, bB = (banks[2], banks[3], banks[4]) if fo % 2 == 0 else (banks[5], banks[7], banks[1])
                    P.op("pe", lambda e, fo=fo, bA=bA: e.matmul(bA.t[:, 0:T], bd.t[:, 0, fo, :], xc.t[:, fo, 0:T], start=True, stop=True), [bd.r, xc[fo // 4]], [bA.r])
                    P.op("pe", lambda e, fo=fo, bK=bK: e.matmul(bK.t[:, 0:T], bd.t[:, 1, fo, :], xc.t[:, fo, 0:T], start=True, stop=True), [bd.r, xc[fo // 4]], [bK.r])
                    P.op("pe", lambda e, fo=fo, bB=bB: e.matmul(bB.t[:, 0:T], bd.t[:, 2, fo, :], xm_op(fo), start=True, stop=True),
                         [bd.r, xmh[fo // 4]] + ([xmc.r] if samp else []), [bB.r])
                    vt = vTt[fo % 2]
                    P.op("act", lambda e, fo=fo, bA=bA: e.activation(out=qT.t[:, fo, 0:T], in_=bA.t[:, 0:T], func=AF.Copy), [bA.r], [qT.r])
                    P.op("dve", lambda e, fo=fo, bK=bK: e.tensor_copy(out=kT.t[:, fo, 0:T], in_=bK.t[:, 0:T]), [bK.r], [kT.r])
                    P.op("act", lambda e, vt=vt, bB=bB: e.activation(out=vt.t[:, 0:T], in_=bB.t[:, 0:T], func=AF.Copy), [bB.r], [vt.r])
                    bg = banks[6]
                    for which, (src, sres) in enumerate([(qT.t[:, fo, 0:T], qT.r), (kT.t[:, fo, 0:T], kT.r), (vt.t[:, 0:T], vt.r)]):
                        first = (fo == 0 and which == 0)
                        lastm = (fo == 15 and which == 2)
                        P.op("pe", lambda e, src=src, which=which, fo=fo, first=first, lastm=lastm: e.matmul(bg.t[0:8, 0:T], wgate.t[:, which * 16 + fo, :], src, start=first, stop=lastm),
                             [wgate.r, sres], [bg.r])

            kstop = int(os.environ.get("KSTOP", "0"))
            if kstop == 11:
                return
            if samp:
                P.barrier_all()
            B1(0)
            B1(1)
            if kstop == 12:
                return
            B2(0)
            if kstop == 13:
                return
            B3(0)
            if kstop == 14:
                return
            B1(2)
            B2(1)
            B3(1)
            B1(3)
            B2(2)
            B3(2)
            B2(3)
            B3(3)
            P.op("act", lambda e: e.activation(out=gT_sb.t[:, 0:T], in_=banks[6].t[0:8, 0:T], func=AF.Copy), [banks[6].r], [gT_sb.r])
            kstop = int(os.environ.get("KSTOP", "0"))
            if kstop == 1:
                return
            if samp:
                P.barrier_all()

            bS = banks[4]

            def A2(tl, a=0, b=4):
                return tl.t[0:Pn, :, a:b]

            def trg(e):
                for c in range(nch):
                    ins = e.transpose(out=bS.t[0:Pn, c * 16:c * 16 + 8], in_=gT_sb.t[0:8, c * Pn:(c + 1) * Pn], identity=C("ident", 8, 8))
                return ins
            P.op("pe", trg, [gT_sb.r, consts.r], [bS.r])
            P.op("dve", lambda e: e.tensor_tensor(out=A2(gg, 0, 8), in0=bS.t[0:Pn, 0:nch * 16].rearrange("p (c g) -> p c g", c=nch)[:, :, 0:8],
                                                   in1=bgate.t[0:Pn, 0:8].unsqueeze(1).to_broadcast([Pn, nch, 8]), op=ALU.add), [bS.r, bgate.r], [gg.r])
            P.op("act", lambda e: e.activation(out=A2(e1), in_=A2(gg, 4, 8), func=AF.Exp, scale=-1.0), [gg.r], [e1.r])
            P.op("act", lambda e: e.activation(out=A2(lf), in_=A2(e1), func=AF.Ln, bias=1.0), [e1.r], [lf.r])
            P.op("dve", lambda e: e.tensor_scalar(out=A2(lf), in0=A2(lf), scalar1=-1.0, scalar2=None, op0=ALU.mult), [lf.r], [lf.r])

            def mmcs(e):
                lfv = lf.t[0:Pn, :, :].rearrange("p c h -> p (c h)")
                e.matmul(bS.t[0:Pn, 32:32 + nch * 4], mask, lfv, start=True, stop=True)
                return e.matmul(bS.t[0:Pn, 48:48 + nch * 4], onesblk, lfv, start=True, stop=True)
            P.op("pe", mmcs, [consts.r, lf.r], [bS.r])
            P.op("dve", lambda e: e.tensor_copy(out=A2(bt), in_=bS.t[0:Pn, 32:32 + nch * 4].rearrange("p (c h) -> p c h", c=nch)), [bS.r], [bt.r])
            P.op("dve", lambda e: e.tensor_copy(out=A2(bl), in_=bS.t[0:Pn, 48:48 + nch * 4].rearrange("p (c h) -> p c h", c=nch)), [bS.r], [bl.r])
            P.op("dve", lambda e: e.tensor_tensor(out=A2(aa), in0=A2(gg, 0, 4), in1=A2(bt), op=ALU.subtract), [gg.r, bt.r], [aa.r])

            def tra(e):
                for c in range(nch):
                    ins = e.transpose(out=bS.t[0:4, 64 + c * Pn:64 + (c + 1) * Pn], in_=aa.t[0:Pn, c, 0:4], identity=C("ident", Pn, Pn))
                return ins
            P.op("pe", tra, [aa.r, consts.r], [bS.r])
            P.op("dve", lambda e: e.tensor_reduce(out=amaxT.t[0:4, :], in_=bS.t[0:4, 64:64 + nch * Pn].rearrange("p (s l) -> p s l", l=L), axis=AX.X, op=ALU.max),
                 [bS.r], [amaxT.r])
            P.op("dve", lambda e: e.tensor_copy(out=amax_exp.t[0:4, :].rearrange("p (s l) -> p s l", l=L),
                                                 in_=amaxT.t[0:4, :].unsqueeze(2).to_broadcast([4, nch * nseq, L])), [amaxT.r], [amax_exp.r])

            def mmbc(e):
                for c in range(nch):
                    ins = e.matmul(bS.t[0:Pn, 384 + c * 16:388 + c * 16], amax_exp.t[0:4, c * Pn:(c + 1) * Pn], C("ident", 4, 4), start=True, stop=True)
                return ins
            P.op("pe", mmbc, [amax_exp.r, consts.r], [bS.r])
            for c in range(nch):
                P.op("dve", lambda e, c=c: e.tensor_tensor(out=RR.t[0:Pn, c, :], in0=bS.t[0:Pn, 384 + c * 16:388 + c * 16], in1=mcur.t[0:Pn, c, :], op=ALU.max), [bS.r, mcur.r], [RR.r])
                P.op("dve", lambda e, c=c: e.tensor_tensor(out=mcur.t[0:Pn, c + 1, :], in0=bl.t[0:Pn, c, :], in1=RR.t[0:Pn, c, :], op=ALU.add), [bl.r, RR.r], [mcur.r])
            P.op("dve", lambda e: e.tensor_tensor(out=A2(d1), in0=A2(aa), in1=A2(RR), op=ALU.subtract), [aa.r, RR.r], [d1.r])
            P.op("act", lambda e: e.activation(out=A2(es), in_=A2(d1), func=AF.Exp, bias=V("c_lnscale", 0, Pn)), [d1.r, vecs.r], [es.r])
            P.op("dve", lambda e: e.tensor_tensor(out=A2(d2), in0=mcur.t[0:Pn, 0:nch, :], in1=A2(RR), op=ALU.subtract), [mcur.r, RR.r], [d2.r])
            P.op("act", lambda e: e.activation(out=A2(c0), in_=A2(d2), func=AF.Exp), [d2.r], [c0.r])
            P.op("dve", lambda e: e.tensor_tensor(out=A2(d3), in0=A2(bt), in1=A2(RR), op=ALU.add), [bt.r, RR.r], [d3.r])
            P.op("act", lambda e: e.activation(out=A2(dfl), in_=A2(d3), func=AF.Exp, scale=-1.0), [d3.r], [dfl.r])
            for c in range(nch):
                P.op("dve", lambda e, c=c: e.tensor_tensor(out=rhsx.t[0:Pn, c * nseq * 4:(c + 1) * nseq * 4].rearrange("p (s h) -> p s h", s=nseq),
                                                           in0=c0.t[0:Pn, c, :].unsqueeze(1).to_broadcast([Pn, nseq, 4]),
                                                           in1=firstm.unsqueeze(2).to_broadcast([Pn, nseq, 4]), op=ALU.mult), [c0.r, consts.r], [rhsx.r])
            P.op("pe", lambda e: e.matmul(bS.t[:, 448:448 + NS4], C("onesP", Pn, 128), rhsx.t[0:Pn, 0:NS4], start=True, stop=True), [rhsx.r, consts.r], [bS.r])
            P.op("dve", lambda e: e.tensor_copy(out=c0bc.t[:].rearrange("p c s h -> p (c s h)"), in_=bS.t[:, 448:448 + NS4]), [bS.r], [c0bc.r])

            if kstop == 2:
                return
            if samp:
                P.barrier_all()
            bV2 = banks[4]
            bSc = banks[2]
            bNs = [banks[3], banks[6], banks[5], banks[1]]
            bcus = [banks[0], banks[7]]
            for c in range(nch):
                cs = slice(c * Pn, (c + 1) * Pn)

                def ES(hh, c=c):
                    return es.t[0:Pn, c, hh:hh + 1]

                def F(hh, c=c, cs=cs):
                    v_tok, kp_tok, stt = v_tk[hh % 2], kp_tk[hh % 2], ST[hh]

                    def mmv(e):
                        for fl in range(4):
                            fo = hh * 4 + fl
                            ins = e.matmul(bV2.t[0:Pn, fl * 128:(fl + 1) * 128], xm_op(fo, cs.start, cs.stop), bd.t[:, 2, fo, :], start=True, stop=True)
                        return ins
                    P.op("pe", mmv, [xmh[hh], bd.r] + ([xmc.r] if samp else []), [bV2.r])
                    P.op("act", lambda e: e.activation(out=v_tok.t[0:Pn, :], in_=bV2.t[0:Pn, :], func=AF.Copy), [bV2.r], [v_tok.r])

                    def mmk(e):
                        for fl in range(4):
                            fo = hh * 4 + fl
                            ins = e.matmul(bV2.t[0:Pn, fl * 128:(fl + 1) * 128], xc.t[:, fo, cs], bd.t[:, 1, fo, :], start=True, stop=True)
                        return ins
                    P.op("pe", mmk, [xc[hh], bd.r], [bV2.r])
                    P.op("act", lambda e: e.activation(out=kp_tok.t[0:Pn, :], in_=bV2.t[0:Pn, :], func=AF.Copy, scale=ES(hh)), [bV2.r, es.r], [kp_tok.r])

                    def mms(e):
                        for dc in range(4):
                            ins = e.matmul(bSc.t[0:Pn, 0:Pn], kT.t[:, hh * 4 + dc, cs], qT.t[:, hh * 4 + dc, cs], start=(dc == 0), stop=(dc == 3))
                        return ins
                    P.op("pe", mms, [kT.r, qT.r], [bSc.r])
                    P.op("dve", lambda e: e.scalar_tensor_tensor(out=stt.t[0:Pn, 0:Pn], in0=bSc.t[0:Pn, 0:Pn], scalar=ES(hh), in1=mask, op0=ALU.mult, op1=ALU.mult),
                         [bSc.r, es.r, consts.r], [stt.r])
                    P.op("dve", lambda e: e.tensor_tensor(out=nbf.t[:, hh * 4:(hh + 1) * 4, :], in0=nst.t[:, hh * 4:(hh + 1) * 4, :],
                                                          in1=c0bc.t[:, c, :, hh].unsqueeze(1).to_broadcast([128, 4, nseq]), op=ALU.mult), [nst[hh], c0bc.r], [nbf[hh]])
                    if not samp:
                        cb = Cbf[hh % 2]
                        P.op("act", lambda e: e.activation(out=cb.t[:], in_=Cst.t[:, hh * 4:(hh + 1) * 4, :], func=AF.Copy, scale=c0bc.t[:, c, 0, hh:hh + 1]),
                             [Cst[hh], c0bc.r], [cb.r])

                def NQ(hh, cs=cs):
                    stt = ST[hh]

                    def mmq(e):
                        e.matmul(bSc.t[0:Pn, 128 + hh:129 + hh], stt.t[0:Pn, 0:Pn], onesb[0:Pn, 0:1], start=True, stop=True)
                        for dc in range(4):
                            ins = e.matmul(bSc.t[0:Pn, 160 + hh * 16:160 + hh * 16 + nseq], qT.t[:, hh * 4 + dc, cs], nbf.t[:, hh * 4 + dc, :], start=(dc == 0), stop=(dc == 3))
                        return ins
                    P.op("pe", mmq, [stt.r, cbf.r, qT.r, nbf[hh]], [bSc.r])

                def NU_prompt(hh, c=c, cs=cs):
                    v_tok, kp_tok, stt, cb, bN = v_tk[hh % 2], kp_tk[hh % 2], ST[hh], Cbf[hh % 2], bNs[hh]

                    def mmn(e):
                        e.matmul(bN.t[0:Pn, :], stt.t[0:Pn, 0:Pn], v_tok.t[0:Pn, :], start=True, stop=False)
                        for dc in range(4):
                            ins = e.matmul(bN.t[0:Pn, :], qT.t[:, hh * 4 + dc, cs], cb.t[:, dc, :], start=False, stop=(dc == 3))
                        return ins
                    P.op("pe", mmn, [stt.r, v_tok.r, qT.r, cb.r], [bN.r])
                    NQ(hh)
                    for dc in range(4):
                        bcu = bcus[cnt[0] % 2]
                        cnt[0] += 1
                        P.op("pe", lambda e, bcu=bcu, dc=dc: e.matmul(bcu.t[:, :], kp_tok.t[0:Pn, dc * 128:(dc + 1) * 128], v_tok.t[0:Pn, :], start=True, stop=True),
                             [kp_tok.r, v_tok.r], [bcu.r])
                        P.op("dve", lambda e, bcu=bcu, dc=dc: e.scalar_tensor_tensor(out=Cst.t[:, hh * 4 + dc, :], in0=Cst.t[:, hh * 4 + dc, :], scalar=c0bc.t[:, c, 0, hh:hh + 1],
                                                                                   in1=bcu.t[:, :], op0=ALU.mult, op1=ALU.add), [bcu.r, Cst[hh], c0bc.r], [Cst[hh]])

                def NU_samp(hh, c=c, cs=cs):
                    v_tok, kp_tok, stt, bN = v_tk[hh % 2], kp_tk[hh % 2], ST[hh], bNs[hh]
                    P.op("pe", lambda e: e.matmul(bN.t[0:Pn, :], stt.t[0:Pn, 0:Pn], v_tok.t[0:Pn, :], start=True, stop=False), [stt.r, v_tok.r], [bN.r])
                    for b in range(NB):
                        idx = hh * NB + b
                        ensure_loaded(idx + len(Cslots) - 2)
                        sl = Cslots[idx % len(Cslots)]
                        cb = Cbf[b % 2]
                        P.op("act", lambda e, cb=cb, sl=sl, b=b: e.activation(out=cb.t[:], in_=sl.t[:], func=AF.Copy, scale=c0bc.t[:, c, b, hh:hh + 1]), [sl.r, c0bc.r], [cb.r])
                        qmt = qm[b % 2]
                        P.op("dve", lambda e, qmt=qmt, b=b: e.tensor_tensor(out=qmt.t[:], in0=qT.t[:, hh * 4:(hh + 1) * 4, 0:TS],
                                                                            in1=colmask.t[:, b, :].unsqueeze(1).to_broadcast([128, 4, TS]), op=ALU.mult), [qT.r, colmask.r], [qmt.r])

                        def mmc(e, qmt=qmt, cb=cb, b=b):
                            for dc in range(4):
                                ins = e.matmul(bN.t[0:Pn, :], qmt.t[:, dc, :], cb.t[:, dc, :], start=False, stop=(b == NB - 1 and dc == 3))
                            return ins
                        P.op("pe", mmc, [qmt.r, cb.r], [bN.r])
                        kpt = kpm[b % 2]
                        P.op("dve", lambda e, kpt=kpt, b=b: e.tensor_scalar(out=kpt.t[0:Pn, :], in0=kp_tok.t[0:Pn, :], scalar1=consts.t[0:Pn, CL["seqS"] + b:CL["seqS"] + b + 1],
                                                                            scalar2=None, op0=ALU.mult), [kp_tok.r, consts.r], [kpt.r])
                        for dc in range(4):
                            bcu = bcus[cnt[0] % 2]
                            cnt[0] += 1
                            P.op("pe", lambda e, bcu=bcu, kpt=kpt, dc=dc: e.matmul(bcu.t[:, :], kpt.t[0:Pn, dc * 128:(dc + 1) * 128], v_tok.t[0:Pn, :], start=True, stop=True),
                                 [kpt.r, v_tok.r], [bcu.r])
                            P.op("dve", lambda e, bcu=bcu, dc=dc, b=b, sl=sl: e.scalar_tensor_tensor(out=sl.t[:, dc, :], in0=sl.t[:, dc, :], scalar=c0bc.t[:, c, b, hh:hh + 1],
                                                                                                   in1=bcu.t[:, :], op0=ALU.mult, op1=ALU.add), [bcu.r, sl.r, c0bc.r], [sl.r])
                        P.dma("sp", Cs_o[j, b, hh].rearrange("(dc p) e -> p dc e", p=128), sl.t[:], reads=[sl.r])
                    NQ(hh)

                def NUPD(hh, c=c):
                    kp_tok = kp_tk[hh % 2]

                    def mmnn(e):
                        for dc in range(4):
                            ins = e.matmul(bSc.t[:, 256 + hh * 64 + dc * 16:256 + hh * 64 + dc * 16 + nseq], kp_tok.t[0:Pn, dc * 128:(dc + 1) * 128], seqmb, start=True, stop=True)
                        return ins
                    P.op("pe", mmnn, [kp_tok.r, cbf.r], [bSc.r])
                    P.op("dve", lambda e: e.tensor_tensor(out=tmpn2.t[:], in0=nst.t[:, hh * 4:(hh + 1) * 4, :],
                                                          in1=c0bc.t[:, c, :, hh].unsqueeze(1).to_broadcast([128, 4, nseq]), op=ALU.mult), [nst[hh], c0bc.r], [tmpn2.r])
                    P.op("dve", lambda e: e.tensor_tensor(out=nst.t[:, hh * 4:(hh + 1) * 4, :], in0=tmpn2.t[:],
                                                          in1=bSc.t[:, 256 + hh * 64:256 + hh * 64 + 64].rearrange("p (d s) -> p d s", d=4)[:, :, 0:nseq], op=ALU.add),
                         [tmpn2.r, bSc.r], [nst[hh]])

                ld_issued = [0]

                def ensure_loaded(upto):
                    while ld_issued[0] <= min(upto, 4 * NB - 1):
                        k = ld_issued[0]
                        P.dma("sp", Cslots[k % len(Cslots)].t[:], sC_d[j, k % NB, k // NB].rearrange("(dc p) e -> p dc e", p=128), writes=[Cslots[k % len(Cslots)].r])
                        ld_issued[0] += 1

                NU = NU_samp if samp else NU_prompt
                F(0)
                F(1)
                NU(0)
                NUPD(0)
                F(2)
                NU(1)
                NUPD(1)
                F(3)
                NU(2)
                NUPD(2)
                NU(3)
                NUPD(3)
                def _bs(hh):
                    P.op("dve", lambda e: e.bn_stats(out=st6[hh].t[0:Pn, :], in_=bNs[hh].t[0:Pn, :]), [bNs[hh].r], [st6[hh].r])

                def _ba(hh):
                    P.op("dve", lambda e: e.bn_aggr(out=mv.t[0:Pn, hh, :], in_=st6[hh].t[0:Pn, :]), [st6[hh].r], [mv.r])
                _bs(0)
                P.op("dve", lambda e: e.tensor_tensor(out=tmpn.t[0:Pn], in0=bSc.t[0:Pn, 160:224].rearrange("p (h s) -> p h s", h=4)[:, :, 0:nseq],
                                                      in1=seqm.unsqueeze(1).to_broadcast([Pn, 4, nseq]), op=ALU.mult), [bSc.r, consts.r], [tmpn.r])
                _bs(1)
                P.op("dve", lambda e: e.tensor_reduce(out=nqb.t[0:Pn, :], in_=tmpn.t[0:Pn], axis=AX.X, op=ALU.add), [tmpn.r], [nqb.r])
                _bs(2)
                P.op("dve", lambda e: e.tensor_tensor(out=nq.t[0:Pn, 0, :], in0=nqb.t[0:Pn, :], in1=bSc.t[0:Pn, 128:132], op=ALU.add), [nqb.r, bSc.r], [nq.r])
                _bs(3)
                P.op("dve", lambda e: e.tensor_scalar(out=nq.t[0:Pn, 1, :], in0=nq.t[0:Pn, 0, :], scalar1=-1.0, scalar2=None, op0=ALU.mult), [nq.r], [nq.r])
                _ba(0)
                P.op("dve", lambda e: e.tensor_tensor(out=nq.t[0:Pn, 2, :], in0=nq.t[0:Pn, 0, :], in1=nq.t[0:Pn, 1, :], op=ALU.max), [nq.r], [nq.r])
                _ba(1)
                P.op("dve", lambda e, c=c: e.tensor_tensor(out=den.t[0:Pn, :], in0=nq.t[0:Pn, 2, :], in1=dfl.t[0:Pn, c, :], op=ALU.max), [nq.r, dfl.r], [den.r])
                _ba(2)
                P.op("dve", lambda e: e.tensor_tensor(out=t1.t[0:Pn, :], in0=den.t[0:Pn, :], in1=den.t[0:Pn, :], op=ALU.mult), [den.r], [t1.r])
                _ba(3)
                P.op("dve", lambda e: e.scalar_tensor_tensor(out=t1.t[0:Pn, :], in0=t1.t[0:Pn, :], scalar=EPS, in1=mv.t[0:Pn, :, 1], op0=ALU.mult, op1=ALU.add), [t1.r, mv.r], [t1.r])
                P.op("act", lambda e: e.activation(out=rs.t[0:Pn, :], in_=t1.t[0:Pn, :], func=AF.Sqrt), [t1.r], [rs.r])
                P.op("dve", lambda e: e.reciprocal(out=rs.t[0:Pn, :], in_=rs.t[0:Pn, :]), [rs.r], [rs.r])
                P.op("dve", lambda e: e.scalar_tensor_tensor(out=nb.t[0:Pn, :], in0=mv.t[0:Pn, :, 0], scalar=-1.0, in1=rs.t[0:Pn, :], op0=ALU.mult, op1=ALU.mult), [mv.r, rs.r], [nb.r])
                for hh in range(4):
                    P.op("act", lambda e, hh=hh, c=c: e.activation(out=hn_tok.t[0:Pn, c, hh * 512:(hh + 1) * 512], in_=bNs[hh].t[0:Pn, :], func=AF.Identity,
                                                                   scale=rs.t[0:Pn, hh:hh + 1], bias=nb.t[0:Pn, hh:hh + 1]), [bNs[hh].r, rs.r, nb.r], [hn_tok.r])

            if kstop == 3:
                return
            pend = []
            for fo in range(16):
                if fo % 2 == 0:
                    wt = next_wA()
                    P.dma("pool", wt.t[:], wup_d[j][8 + fo // 2], writes=[wt.r])
                fc = fo % 2
                bx = banks[fo % 2]
                bT = banks[2 + 3 * (fo % 2)]
                bTb = bT.t[:].bitcast(BF16)

                def mm(e, wt=wt, bx=bx, fc=fc):
                    for k in range(8):
                        ins = e.matmul(bx.t[:, 0:T], wt.t[:, k, fc * 128:(fc + 1) * 128], xn.t[:, k, 0:T], start=(k == 0), stop=(k == 7))
                    return ins
                P.op("pe", mm, [wt.r, xn.r], [bx.r])
                szt = sz[fo % 2]
                eat = ea[fo % 2]
                P.op("act", lambda e, bx=bx, szt=szt: e.activation(out=szt.t[:, 0:T], in_=bx.t[:, 0:T], func=AF.Silu), [bx.r], [szt.r])

                def mmt(e, fo=fo, bTb=bTb):
                    for c in range(nch):
                        ins = e.transpose(out=bTb[:, c * Pn:(c + 1) * Pn], in_=hn_tok.t[0:Pn, c, fo * 128:(fo + 1) * 128], identity=identb[0:Pn, 0:Pn])
                    return ins
                P.op("pe", mmt, [hn_tok.r, cbf.r], [bT.r])
                P.op("act", lambda e, eat=eat, fo=fo, bTb=bTb: e.activation(out=eat.t[:, 0:T], in_=bTb[:, 0:T], func=AF.Copy, scale=V(f"ml_hn_g_{j}", fo)), [bT.r, vecs.r], [eat.r])
                P.op("dve", lambda e, eat=eat, fo=fo: e.scalar_tensor_tensor(out=eat.t[:, 0:T], in0=xc.t[:, fo, 0:T], scalar=V(f"ml_skip_{j}", fo), in1=eat.t[:, 0:T],
                                                                           op0=ALU.mult, op1=ALU.add), [xc[fo // 4], eat.r, vecs.r], [eat.r])
                pend.append((eat, szt, fo))
                if len(pend) == 2:
                    eat_, szt_, fo_ = pend.pop(0)
                    P.op("dve", lambda e, eat_=eat_, szt_=szt_, fo_=fo_: e.tensor_tensor(out=oT.t[:, fo_, 0:T], in0=eat_.t[:, 0:T], in1=szt_.t[:, 0:T], op=ALU.mult), [eat_.r, szt_.r], [oT.r])
            eat_, szt_, fo_ = pend.pop(0)
            P.op("dve", lambda e, eat_=eat_, szt_=szt_, fo_=fo_: e.tensor_tensor(out=oT.t[:, fo_, 0:T], in0=eat_.t[:, 0:T], in1=szt_.t[:, 0:T], op=ALU.mult), [eat_.r, szt_.r], [oT.r])
            xr = xres(t0, T)
            for d in range(8):
                wdt = next_wB()
                wv = wdt.t[:, :].rearrange("p (f c) -> p f c", f=16)
                P.dma("pool", wv, wdn_d[j][d], writes=[wdt.r])
                bk = banks[3 + 3 * (d % 2)]

                def mm(e, wv=wv, bk=bk):
                    for f in range(16):
                        ins = e.matmul(bk.t[:, 0:T], wv[:, f, :], oT.t[:, f, 0:T], start=(f == 0), stop=(f == 15))
                    return ins
                P.op("pe", mm, [wdt.r, oT.r], [bk.r])
                P.op("dve", lambda e, bk=bk, d=d: e.tensor_tensor(out=x.t[:, d, t0:t0 + T], in0=bk.t[:, 0:T], in1=x.t[:, d, t0:t0 + T], op=ALU.add), [bk.r] + xr, xr)
            if not samp:
                P.op("pool", lambda e: e.tensor_copy(out=xmh.t[:, :, 0, 0:3], in_=xmh.t[:, :, 0, T:T + 3]), [xmh.r] + [xmh[g] for g in range(4)], [xmh.r] + [xmh[g] for g in range(4)])
            if last_prompt:
                P.dma("sp", Cp_o[j].rearrange("h (dc p) e -> p (h dc) e", p=128), Cst.t[:], reads=[Cst[h_] for h_ in range(4)])
                P.dma("sp", npT_o[j], nst.t[:], reads=[nst[h_] for h_ in range(4)])
                P.dma("sp", mp_o[j], mcur.t[:, nch, :], reads=[mcur.r])
                P.dma("sp", convp_o[j], tailp.t[:], reads=[tailp.r])
            if samp:
                P.dma("sp", nsT_o[j], nst.t[:], reads=[nst[h_] for h_ in range(4)])
                P.dma("sp", ms_o[j], mcur.t[0:TS, nch, :], reads=[mcur.r])
                P.dma("sp", convs_o[j], tails.t[:], reads=[tails.r])

    def mlstm_v1_sample(i, j):
        P.phase()
        gname = f"norm_mix_{i}"
        T = 128
        NSL = 5
        bd = P.atile([128, 3, 16, 128], BF16)
        colmask = P.atile([128, NB, TS], BF16)
        P.dma("pool", colmask.t[:], colmask_d, writes=[colmask.r])
        wgate = P.atile([128, 48, 8], BF16)
        bgate = P.atile([128, 8], F32)
        xn = P.atile([128, 8, 256], BF16)
        xmh = P.atile([128, 16, 3 + T], BF16)
        tailp = P.atile([128, 16, 3], F32)
        xc = P.atile([128, 16, T], BF16)
        acc = [P.atile([128, T], F32) for _ in range(2)]
        qT = P.atile([128, 16, T], BF16)
        kT = P.atile([128, 16, T], BF16)
        sqb = Tile(kT.t[:, 0:8, :], kT.base_lw)
        sqb._res = kT._res
        vTt = [P.atile([128, T], BF16) for _ in range(2)]
        v_tk = [P.atile([128, 512], BF16) for _ in range(2)]
        kp_tk = [P.atile([128, 512], BF16) for _ in range(2)]
        Cst = P.atile([128, 4 * NSL, 512], F32)
        Cbf = [P.atile([128, 4, 512], BF16) for _ in range(2)]
        nst = P.atile([128, 16, NB], F32)
        nbf = P.atile([128, 16, NB], BF16)
        ST = [P.atile([128, 128], BF16) for _ in range(2)]
        hn_tok = P.atile([128, 2, 2048], BF16)
        tails = Tile(hn_tok.t[:, 1, 512:2048].bitcast(F32).rearrange("p (f s w) -> p f s w", f=16, s=NB), hn_tok.base_lw)
        tails._res = hn_tok._res
        sz = acc
        ea = [P.atile([128, T], F32)]
        rst = ea[0]
        gT_sb = P.atile([8, T], F32)
        sm = {nm: P.atile([128, 8], F32) for nm in ["g", "e1", "lf", "btbl", "a", "R", "d1", "es", "d2", "c0", "d3", "dfl", "m0a", "m0b",
                                                    "nq", "den", "mv", "t1", "rs", "nb", "nqb"]}
        st6 = P.atile([128, 6], F32)
        amaxT = P.atile([4, NB], F32)
        amax_exp = P.atile([4, 128], F32)
        rhsx = P.atile([128, NB, 4], F32)
        c0bc = P.atile([128, NB, 4], F32)
        tmpn = P.atile([128, NB], F32)
        tmpn2 = P.atile([128, 4, NB], F32)
        qm = [P.atile([128, 4, TS], BF16) for _ in range(2)]
        kpm = [P.atile([64, 512], BF16) for _ in range(2)]
        oT = qT
        xmh_s = Tile(xmh.t[:, :, 0:NB * 7].rearrange("p f (s l) -> p f s l", s=NB), xmh.base_lw)
        xmh_s._res = xmh._res

        P.dma("pool", bd.t[:], bd_d[j], writes=[bd.r])
        P.dma("pool", wgate.t[:], wgate_d[j], writes=[wgate.r])
        P.dma("sp", bgate.t[:], bgate_d[j], writes=[bgate.r])
        P.op("pool", lambda e: e.memset(Cst.t[:], 0.0), [], [Cst[s_] for s_ in range(NSL)])
        P.op("pool", lambda e: e.memset(nst.t[:], 0.0), [], [nst.r])
        P.op("pool", lambda e: e.memset(sm["m0a"].t[:], 0.0), [], [sm["m0a"].r])
        P.op("pool", lambda e: e.memset(xmh.t[:, :, 0:3], 0.0), [], [xmh.r])

        def xmc_ap(fo, cs=slice(0, TS)):
            base = 64 + (fo % 2) * 64
            return xn.t[:, fo // 2, base + cs.start:base + cs.stop]
        m0 = [sm["m0a"], sm["m0b"]]
        m0_i = [0]
        lnscale = math.log(DH ** -0.5)

        tiles = [(TP, TS, NB, 4, TS, 1)]
        ld_issued = [0]
        cnt = [0]
        for tidx, (t0, Tn, nseq, L, Pn, nch) in enumerate(tiles):
            samp = nseq > 1
            last_prompt = (t0 + Tn == TP)
            if samp:
                P.dma("sp", nst.t[:], snT_d[j], writes=[nst.r])
                P.dma("sp", m0[m0_i[0]].t[0:TS, 0:4], sm_d[j], writes=[m0[m0_i[0]].r])
                P.dma("sp", tails.t[:], sconv_d[j], writes=[tails.r])
                P.op("dve", lambda e: e.tensor_copy(out=xmh_s.t[:, :, :, 0:3], in_=tails.t[:]), [tails.r], [xmh.r])
            rmsnorm(t0, Tn, gname, xn, xn.r, sqb, rst, banks[6])
            for blk in range(8):
                wt = next_wA()
                P.dma("pool", wt.t[:], wup_d[j][blk], writes=[wt.r])
                for fc in range(2):
                    fo = blk * 2 + fc
                    bx = banks[cnt[0] % 2]
                    cnt[0] += 1

                    def mm(e, wt=wt, bx=bx, fc=fc):
                        for k in range(8):
                            ins = e.matmul(bx.t[:, 0:Tn], wt.t[:, k, fc * 128:(fc + 1) * 128], xn.t[:, k, 0:Tn], start=(k == 0), stop=(k == 7))
                        return ins
                    P.op("pe", mm, [wt.r, xn.r], [bx.r])
                    if not samp:
                        P.op("act", lambda e, bx=bx, fo=fo: e.activation(out=xmh.t[:, fo, 3:3 + Tn], in_=bx.t[:, 0:Tn], func=AF.Copy), [bx.r], [xmh.r])
                        if last_prompt:
                            P.op("dve", lambda e, bx=bx, fo=fo: e.tensor_copy(out=tailp.t[:, fo, :], in_=bx.t[:, Tn - 3:Tn]), [bx.r], [tailp.r])
                        xm_op = xmh.t[:, fo, 3:3 + Tn]
                        wins = [xmh.t[:, fo, w:w + Tn] for w in range(4)]
                        accv = [a_.t[:, 0:Tn] for a_ in acc]
                    else:
                        bx3 = bx.t[:, 0:Tn].rearrange("p (s l) -> p s l", s=NB)
                        P.op("act", lambda e, bx3=bx3, fo=fo: e.activation(out=xmh_s.t[:, fo, :, 3:7], in_=bx3, func=AF.Copy), [bx.r], [xmh.r])
                        P.op("act", lambda e, bx=bx, fo=fo: e.activation(out=xmc_ap(fo), in_=bx.t[:, 0:Tn], func=AF.Copy), [bx.r], [xn.r])
                        P.op("dve", lambda e, bx3=bx3, fo=fo: e.tensor_copy(out=tails.t[:, fo, :, :], in_=bx3[:, :, 1:4]), [bx.r], [tails.r])
                        xm_op = xmc_ap(fo)
                        wins = [xmh_s.t[:, fo, :, w:w + 4] for w in range(4)]
                        accv = [a_.t[:, 0:Tn].rearrange("p (s l) -> p s l", s=NB) for a_ in acc]
                    at = acc[fo % 2]
                    av = accv[fo % 2]
                    P.op("dve", lambda e, av=av, wins=wins, fo=fo: e.tensor_scalar(out=av, in0=wins[0], scalar1=V(f"ml_wconv_{j}_0", fo), scalar2=V(f"ml_bconv_{j}", fo),
                                                                                   op0=ALU.mult, op1=ALU.add), [xmh.r, vecs.r], [at.r])
                    for w in range(1, 4):
                        P.op("dve", lambda e, av=av, wins=wins, fo=fo, w=w: e.scalar_tensor_tensor(out=av, in0=wins[w], scalar=V(f"ml_wconv_{j}_{w}", fo), in1=av,
                                                                                                 op0=ALU.mult, op1=ALU.add), [xmh.r, at.r, vecs.r], [at.r])
                    P.op("act", lambda e, at=at, fo=fo: e.activation(out=xc.t[:, fo, 0:Tn], in_=at.t[:, 0:Tn], func=AF.Silu), [at.r], [xc.r])
                    bq, bk_, bv = banks[2], banks[3], banks[4]
                    P.op("pe", lambda e, fo=fo: e.matmul(bq.t[:, 0:Tn], bd.t[:, 0, fo, :], xc.t[:, fo, 0:Tn], start=True, stop=True), [bd.r, xc.r], [bq.r])
                    P.op("pe", lambda e, fo=fo: e.matmul(bk_.t[:, 0:Tn], bd.t[:, 1, fo, :], xc.t[:, fo, 0:Tn], start=True, stop=True), [bd.r, xc.r], [bk_.r])
                    P.op("pe", lambda e, fo=fo, xm_op=xm_op: e.matmul(bv.t[:, 0:Tn], bd.t[:, 2, fo, :], xm_op, start=True, stop=True), [bd.r, xmh.r, xn.r], [bv.r])
                    vt = vTt[fo % 2]
                    P.op("act", lambda e, fo=fo: e.activation(out=qT.t[:, fo, 0:Tn], in_=bq.t[:, 0:Tn], func=AF.Copy), [bq.r], [qT.r])
                    P.op("dve", lambda e, fo=fo: e.tensor_copy(out=kT.t[:, fo, 0:Tn], in_=bk_.t[:, 0:Tn]), [bk_.r], [kT.r])
                    P.op("act", lambda e, vt=vt: e.activation(out=vt.t[:, 0:Tn], in_=bv.t[:, 0:Tn], func=AF.Copy), [bv.r], [vt.r])
                    bg = banks[5]
                    for which, (src, sres) in enumerate([(qT.t[:, fo, 0:Tn], qT.r), (kT.t[:, fo, 0:Tn], kT.r), (vt.t[:, 0:Tn], vt.r)]):
                        first = (fo == 0 and which == 0)
                        lastm = (fo == 15 and which == 2)
                        P.op("pe", lambda e, src=src, which=which, fo=fo, first=first, lastm=lastm: e.matmul(bg.t[0:8, 0:Tn], wgate.t[:, which * 16 + fo, :], src, start=first, stop=lastm),
                             [wgate.r, sres], [bg.r])
            P.op("act", lambda e: e.activation(out=gT_sb.t[:, 0:Tn], in_=banks[5].t[0:8, 0:Tn], func=AF.Copy), [banks[5].r], [gT_sb.r])
            if dbg == "ml0" and tidx == int(os.environ.get("KDBG_T", "0")) and j == 0:
                do_ = dout("dbg_x", [128, 8, T])
                P.op("dve", lambda e: e.tensor_copy(out=dbg_stage.t[:, 0:8, :], in_=x.t[:, :, t0:t0 + T]), xres(t0, T), [dbg_stage.r])
                P.dma("sp", do_, dbg_stage.t[:], reads=[dbg_stage.r])
                for nm_, src_, n3 in [("dbg_xn", xn, 8), ("dbg_xm", None, 16), ("dbg_xc", xc, 16), ("dbg_q", qT, 16)]:
                    do_ = dout(nm_, [128, n3, T])
                    stg = dbg_stage
                    for h0 in range(0, n3, 8):
                        if src_ is None:
                            P.op("dve", lambda e, stg=stg, h0=h0: e.tensor_copy(out=stg.t[:, 0:8, :], in_=xmh.t[:, h0:h0 + 8, 3:3 + T]), [xmh.r], [stg.r])
                        else:
                            P.op("dve", lambda e, stg=stg, src_=src_, h0=h0: e.tensor_copy(out=stg.t[:, 0:8, :], in_=src_.t[:, h0:h0 + 8, 0:T]), [src_.r], [stg.r])
                        P.dma("sp", do_[:, h0:h0 + 8, :], stg.t[:, 0:8, :], reads=[stg.r])

            for c in range(nch):
                cs = slice(c * Pn, (c + 1) * Pn)
                bS = banks[7]
                mask = C("maskS" if samp else "maskP", Pn, Pn)
                onesblk = C("onesS" if samp else "onesP", Pn, Pn)
                seqm = C("seqS" if samp else "seqP", Pn, nseq)
                firstm = C("firstS" if samp else "firstP", Pn, nseq)
                seqmb = cbf.t[0:Pn, 256:256 + nseq] if samp else cbf.t[0:Pn, 272:273]
                m0c = m0[m0_i[0]]
                m0n = m0[1 - m0_i[0]]
                m0_i[0] = 1 - m0_i[0]
                s = sm

                def S(nm, a=0, b=4):
                    return s[nm].t[0:Pn, a:b]
                P.op("pe", lambda e, cs=cs: e.transpose(out=bS.t[0:Pn, 0:8], in_=gT_sb.t[0:8, cs], identity=C("ident", 8, 8)), [gT_sb.r, consts.r], [bS.r])
                P.op("dve", lambda e: e.tensor_tensor(out=S("g", 0, 8), in0=bS.t[0:Pn, 0:8], in1=bgate.t[0:Pn, 0:8], op=ALU.add), [bS.r, bgate.r], [s["g"].r])
                P.op("act", lambda e: e.activation(out=S("e1"), in_=S("g", 4, 8), func=AF.Exp, scale=-1.0), [s["g"].r], [s["e1"].r])
                P.op("act", lambda e: e.activation(out=S("lf"), in_=S("e1"), func=AF.Ln, bias=1.0), [s["e1"].r], [s["lf"].r])
                P.op("dve", lambda e: e.tensor_scalar(out=S("lf"), in0=S("lf"), scalar1=-1.0, scalar2=None, op0=ALU.mult), [s["lf"].r], [s["lf"].r])

                def mm(e, mask=mask, onesblk=onesblk):
                    e.matmul(bS.t[0:Pn, 8:12], mask, S("lf"), start=True, stop=True)
                    return e.matmul(bS.t[0:Pn, 12:16], onesblk, S("lf"), start=True, stop=True)
                P.op("pe", mm, [consts.r, s["lf"].r], [bS.r])
                P.op("dve", lambda e: e.tensor_copy(out=S("btbl", 0, 8), in_=bS.t[0:Pn, 8:16]), [bS.r], [s["btbl"].r])
                P.op("dve", lambda e: e.tensor_tensor(out=S("a"), in0=S("g", 0, 4), in1=S("btbl", 0, 4), op=ALU.subtract), [s["g"].r, s["btbl"].r], [s["a"].r])
                P.op("pe", lambda e: e.transpose(out=bS.t[0:4, 16:16 + Pn], in_=S("a"), identity=C("ident", Pn, Pn)), [s["a"].r, consts.r], [bS.r])
                P.op("dve", lambda e: e.tensor_reduce(out=amaxT.t[0:4, 0:nseq], in_=bS.t[0:4, 16:16 + Pn].rearrange("p (s l) -> p s l", s=nseq), axis=AX.X, op=ALU.max),
                     [bS.r], [amaxT.r])
                P.op("dve", lambda e: e.tensor_copy(out=amax_exp.t[0:4, 0:Pn].rearrange("p (s l) -> p s l", s=nseq),
                                                     in_=amaxT.t[0:4, 0:nseq].unsqueeze(2).to_broadcast([4, nseq, L])), [amaxT.r], [amax_exp.r])
                P.op("pe", lambda e: e.matmul(bS.t[0:Pn, 160:164], amax_exp.t[0:4, 0:Pn], C("ident", 4, 4), start=True, stop=True), [amax_exp.r, consts.r], [bS.r])
                P.op("dve", lambda e, m0c=m0c: e.tensor_tensor(out=S("R"), in0=bS.t[0:Pn, 160:164], in1=m0c.t[0:Pn, 0:4], op=ALU.max), [bS.r, m0c.r], [s["R"].r])
                P.op("dve", lambda e: e.tensor_tensor(out=S("d1"), in0=S("a"), in1=S("R"), op=ALU.subtract), [s["a"].r, s["R"].r], [s["d1"].r])
                P.op("act", lambda e: e.activation(out=S("es"), in_=S("d1"), func=AF.Exp, bias=V("c_lnscale", 0, Pn)), [s["d1"].r, vecs.r], [s["es"].r])
                P.op("dve", lambda e, m0c=m0c: e.tensor_tensor(out=S("d2"), in0=m0c.t[0:Pn, 0:4], in1=S("R"), op=ALU.subtract), [m0c.r, s["R"].r], [s["d2"].r])
                P.op("act", lambda e: e.activation(out=S("c0"), in_=S("d2"), func=AF.Exp), [s["d2"].r], [s["c0"].r])
                P.op("dve", lambda e: e.tensor_tensor(out=S("d3"), in0=S("btbl", 0, 4), in1=S("R"), op=ALU.add), [s["btbl"].r, s["R"].r], [s["d3"].r])
                P.op("act", lambda e: e.activation(out=S("dfl"), in_=S("d3"), func=AF.Exp, scale=-1.0), [s["d3"].r], [s["dfl"].r])
                P.op("dve", lambda e, m0n=m0n: e.tensor_tensor(out=m0n.t[0:Pn, 0:4], in0=S("btbl", 4, 8), in1=S("R"), op=ALU.add), [s["btbl"].r, s["R"].r], [m0n.r])
                P.op("dve", lambda e, firstm=firstm: e.tensor_tensor(out=rhsx.t[0:Pn, 0:nseq, :], in0=S("c0").unsqueeze(1).to_broadcast([Pn, nseq, 4]),
                                                                        in1=firstm.unsqueeze(2).to_broadcast([Pn, nseq, 4]), op=ALU.mult), [s["c0"].r, consts.r], [rhsx.r])
                P.op("pe", lambda e: e.matmul(bS.t[:, 192:192 + nseq * 4], C("onesP", Pn, 128), rhsx.t[0:Pn, 0:nseq, :].rearrange("p s h -> p (s h)"), start=True, stop=True),
                     [rhsx.r, consts.r], [bS.r])
                P.op("dve", lambda e: e.tensor_copy(out=c0bc.t[:, 0:nseq, :].rearrange("p s h -> p (s h)"), in_=bS.t[:, 192:192 + nseq * 4]), [bS.r], [c0bc.r])

                for hh in range(4):
                    bSc, bN, bQ2 = banks[2], banks[3], banks[5]
                    stt = ST[hh % 2]
                    bV2 = banks[4]
                    v_tok = v_tk[hh % 2]
                    kp_tok = kp_tk[hh % 2]

                    def mmv(e, hh=hh):
                        for fl in range(4):
                            fo = hh * 4 + fl
                            lhs = (xmc_ap(fo, cs) if samp else xmh.t[:, fo, 3 + c * Pn:3 + (c + 1) * Pn])
                            ins = e.matmul(bV2.t[0:Pn, fl * 128:(fl + 1) * 128], lhs, bd.t[:, 2, fo, :], start=True, stop=True)
                        return ins
                    P.op("pe", mmv, [xmh.r, xn.r, bd.r], [bV2.r])
                    P.op("act", lambda e, v_tok=v_tok: e.activation(out=v_tok.t[0:Pn, :], in_=bV2.t[0:Pn, :], func=AF.Copy), [bV2.r], [v_tok.r])

                    def mmk(e, hh=hh):
                        for fl in range(4):
                            fo = hh * 4 + fl
                            ins = e.matmul(bV2.t[0:Pn, fl * 128:(fl + 1) * 128], xc.t[:, fo, cs], bd.t[:, 1, fo, :], start=True, stop=True)
                        return ins
                    P.op("pe", mmk, [xc.r, bd.r], [bV2.r])
                    P.op("act", lambda e, hh=hh, kp_tok=kp_tok: e.activation(out=kp_tok.t[0:Pn, :], in_=bV2.t[0:Pn, :], func=AF.Copy, scale=S("es", hh, hh + 1)),
                         [bV2.r, s["es"].r], [kp_tok.r])

                    def mms(e, hh=hh):
                        for dc in range(4):
                            ins = e.matmul(bSc.t[0:Pn, 0:Pn], kT.t[:, hh * 4 + dc, cs], qT.t[:, hh * 4 + dc, cs], start=(dc == 0), stop=(dc == 3))
                        return ins
                    P.op("pe", mms, [kT.r, qT.r], [bSc.r])
                    P.op("dve", lambda e, hh=hh, stt=stt, mask=mask: e.scalar_tensor_tensor(out=stt.t[0:Pn, 0:Pn], in0=bSc.t[0:Pn, 0:Pn], scalar=S("es", hh, hh + 1), in1=mask,
                                                                                          op0=ALU.mult, op1=ALU.mult), [bSc.r, s["es"].r, consts.r], [stt.r])
                    P.op("dve", lambda e, hh=hh: e.tensor_tensor(out=nbf.t[:, hh * 4:(hh + 1) * 4, 0:nseq], in0=nst.t[:, hh * 4:(hh + 1) * 4, 0:nseq],
                                                                 in1=c0bc.t[:, 0:nseq, hh].unsqueeze(1).to_broadcast([128, 4, nseq]), op=ALU.mult), [nst.r, c0bc.r], [nbf.r])
                    if not samp:
                        cb = Cbf[0]
                        P.op("act", lambda e, hh=hh, cb=cb: e.activation(out=cb.t[:], in_=Cst.t[:, hh * 4:(hh + 1) * 4, :], func=AF.Copy, scale=c0bc.t[:, 0, hh:hh + 1]),
                             [Cst[hh], c0bc.r], [cb.r])

                        def mmn(e, hh=hh, stt=stt, cb=cb, v_tok=v_tok):
                            e.matmul(bN.t[0:Pn, :], stt.t[0:Pn, 0:Pn], v_tok.t[0:Pn, :], start=True, stop=False)
                            for dc in range(4):
                                ins = e.matmul(bN.t[0:Pn, :], qT.t[:, hh * 4 + dc, cs], cb.t[:, dc, :], start=False, stop=(dc == 3))
                            return ins
                        P.op("pe", mmn, [stt.r, v_tok.r, qT.r, cb.r], [bN.r])
                    else:
                        P.op("pe", lambda e, hh=hh, stt=stt, v_tok=v_tok: e.matmul(bN.t[0:Pn, :], stt.t[0:Pn, 0:Pn], v_tok.t[0:Pn, :], start=True, stop=False),
                             [stt.r, v_tok.r], [bN.r])
                        for b in range(NB):
                            slot = (hh * NB + b) % NSL
                            cslot = Cst.t[:, slot * 4:(slot + 1) * 4, :]
                            cres = Cst[slot]
                            while ld_issued[0] <= min(hh * NB + b + NSL - 2, 4 * NB - 1):
                                k_ = ld_issued[0]
                                P.dma("sp", Cst.t[:, (k_ % NSL) * 4:(k_ % NSL + 1) * 4, :], sC_d[j, k_ % NB, k_ // NB].rearrange("(dc p) e -> p dc e", p=128), writes=[Cst[k_ % NSL]])
                                ld_issued[0] += 1
                            cb = Cbf[b % 2]
                            P.op("act", lambda e, cb=cb, cslot=cslot, b=b, hh=hh: e.activation(out=cb.t[:], in_=cslot, func=AF.Copy, scale=c0bc.t[:, b, hh:hh + 1]),
                                 [cres, c0bc.r], [cb.r])
                            qmt = qm[b % 2]
                            P.op("dve", lambda e, qmt=qmt, b=b, hh=hh: e.tensor_tensor(out=qmt.t[:], in0=qT.t[:, hh * 4:(hh + 1) * 4, 0:TS],
                                                                                       in1=colmask.t[:, b, :].unsqueeze(1).to_broadcast([128, 4, TS]), op=ALU.mult),
                                 [qT.r, colmask.r], [qmt.r])

                            def mmc(e, qmt=qmt, cb=cb, b=b):
                                for dc in range(4):
                                    ins = e.matmul(bN.t[0:Pn, :], qmt.t[:, dc, :], cb.t[:, dc, :], start=False, stop=(b == NB - 1 and dc == 3))
                                return ins
                            P.op("pe", mmc, [qmt.r, cb.r], [bN.r])
                            kpt = kpm[b % 2]
                            P.op("dve", lambda e, kpt=kpt, b=b, hh=hh, kp_tok=kp_tok: e.tensor_scalar(out=kpt.t[0:Pn, :], in0=kp_tok.t[0:Pn, :],
                                                                                       scalar1=consts.t[0:Pn, CL["seqS"] + b:CL["seqS"] + b + 1], scalar2=None, op0=ALU.mult),
                                 [kp_tok.r, consts.r], [kpt.r])
                            for dc in range(4):
                                bcu = banks[cnt[0] % 2]
                                cnt[0] += 1
                                P.op("pe", lambda e, bcu=bcu, kpt=kpt, dc=dc, hh=hh, v_tok=v_tok: e.matmul(bcu.t[:, :], kpt.t[0:Pn, dc * 128:(dc + 1) * 128], v_tok.t[0:Pn, :],
                                                                                              start=True, stop=True), [kpt.r, v_tok.r], [bcu.r])
                                P.op("dve", lambda e, bcu=bcu, dc=dc, b=b, hh=hh, slot=slot: e.scalar_tensor_tensor(out=Cst.t[:, slot * 4 + dc, :], in0=Cst.t[:, slot * 4 + dc, :],
                                                                                                                   scalar=c0bc.t[:, b, hh:hh + 1], in1=bcu.t[:, :], op0=ALU.mult, op1=ALU.add),
                                     [bcu.r, cres, c0bc.r], [cres])
                            P.dma("sp", Cs_o[j, b, hh].rearrange("(dc p) e -> p dc e", p=128), cslot, reads=[cres])

                    def mmq(e, hh=hh, stt=stt, seqmb=seqmb):
                        e.matmul(bQ2.t[0:Pn, 0:1], stt.t[0:Pn, 0:Pn], onesb[0:Pn, 0:1], start=True, stop=True)
                        for dc in range(4):
                            ins = e.matmul(bQ2.t[0:Pn, 8:8 + nseq], qT.t[:, hh * 4 + dc, cs], nbf.t[:, hh * 4 + dc, 0:nseq], start=(dc == 0), stop=(dc == 3))
                        return ins
                    P.op("pe", mmq, [stt.r, cbf.r, qT.r, nbf.r], [bQ2.r])
                    P.op("dve", lambda e, seqm=seqm: e.tensor_tensor(out=tmpn.t[0:Pn, 0:nseq], in0=bQ2.t[0:Pn, 8:8 + nseq], in1=seqm, op=ALU.mult), [bQ2.r, consts.r], [tmpn.r])
                    P.op("dve", lambda e: e.tensor_reduce(out=S("nqb", 0, 1), in_=tmpn.t[0:Pn, 0:nseq], axis=AX.X, op=ALU.add), [tmpn.r], [s["nqb"].r])
                    P.op("dve", lambda e: e.tensor_tensor(out=S("nq", 0, 1), in0=S("nqb", 0, 1), in1=bQ2.t[0:Pn, 0:1], op=ALU.add), [s["nqb"].r, bQ2.r], [s["nq"].r])
                    P.op("dve", lambda e: e.tensor_scalar(out=S("nq", 1, 2), in0=S("nq", 0, 1), scalar1=-1.0, scalar2=None, op0=ALU.mult), [s["nq"].r], [s["nq"].r])
                    P.op("dve", lambda e: e.tensor_tensor(out=S("nq", 2, 3), in0=S("nq", 0, 1), in1=S("nq", 1, 2), op=ALU.max), [s["nq"].r], [s["nq"].r])
                    P.op("dve", lambda e, hh=hh: e.tensor_tensor(out=S("den", 0, 1), in0=S("nq", 2, 3), in1=S("dfl", hh, hh + 1), op=ALU.max),
                         [s["nq"].r, s["dfl"].r], [s["den"].r])
                    P.op("dve", lambda e: e.bn_stats(out=st6.t[0:Pn, :], in_=bN.t[0:Pn, :]), [bN.r], [st6.r])
                    P.op("dve", lambda e: e.bn_aggr(out=S("mv", 0, 2), in_=st6.t[0:Pn, :]), [st6.r], [s["mv"].r])
                    P.op("dve", lambda e: e.tensor_tensor(out=S("t1", 0, 1), in0=S("den", 0, 1), in1=S("den", 0, 1), op=ALU.mult), [s["den"].r], [s["t1"].r])
                    P.op("dve", lambda e: e.scalar_tensor_tensor(out=S("t1", 0, 1), in0=S("t1", 0, 1), scalar=EPS, in1=S("mv", 1, 2), op0=ALU.mult, op1=ALU.add),
                         [s["t1"].r, s["mv"].r], [s["t1"].r])
                    P.op("act", lambda e: e.activation(out=S("rs", 0, 1), in_=S("t1", 0, 1), func=AF.Sqrt), [s["t1"].r], [s["rs"].r])
                    P.op("dve", lambda e: e.reciprocal(out=S("rs", 0, 1), in_=S("rs", 0, 1)), [s["rs"].r], [s["rs"].r])
                    P.op("dve", lambda e: e.scalar_tensor_tensor(out=S("nb", 0, 1), in0=S("mv", 0, 1), scalar=-1.0, in1=S("rs", 0, 1), op0=ALU.mult, op1=ALU.mult),
                         [s["mv"].r, s["rs"].r], [s["nb"].r])
                    P.op("act", lambda e, hh=hh: e.activation(out=hn_tok.t[0:Pn, c, hh * 512:(hh + 1) * 512], in_=bN.t[0:Pn, :], func=AF.Identity,
                                                              scale=S("rs", 0, 1), bias=S("nb", 0, 1)), [bN.r, s["rs"].r, s["nb"].r], [hn_tok.r])
                    if not samp:
                        for dc in range(4):
                            bcu = banks[cnt[0] % 2]
                            cnt[0] += 1
                            P.op("pe", lambda e, bcu=bcu, dc=dc, hh=hh, kp_tok=kp_tok, v_tok=v_tok: e.matmul(bcu.t[:, :], kp_tok.t[0:Pn, dc * 128:(dc + 1) * 128],
                                                                               v_tok.t[0:Pn, :], start=True, stop=True), [kp_tok.r, v_tok.r], [bcu.r])
                            P.op("dve", lambda e, bcu=bcu, dc=dc, hh=hh: e.scalar_tensor_tensor(out=Cst.t[:, hh * 4 + dc, :], in0=Cst.t[:, hh * 4 + dc, :], scalar=c0bc.t[:, 0, hh:hh + 1],
                                                                                              in1=bcu.t[:, :], op0=ALU.mult, op1=ALU.add), [bcu.r, Cst[hh], c0bc.r], [Cst[hh]])

                    def mmnn(e, hh=hh, seqmb=seqmb, kp_tok=kp_tok):
                        for dc in range(4):
                            ins = e.matmul(bQ2.t[:, 64 + dc * NB:64 + dc * NB + nseq], kp_tok.t[0:Pn, dc * 128:(dc + 1) * 128], seqmb, start=True, stop=True)
                        return ins
                    P.op("pe", mmnn, [kp_tok.r, cbf.r], [bQ2.r])
                    P.op("dve", lambda e, hh=hh: e.tensor_tensor(out=tmpn2.t[:, :, 0:nseq], in0=nst.t[:, hh * 4:(hh + 1) * 4, 0:nseq],
                                                                 in1=c0bc.t[:, 0:nseq, hh].unsqueeze(1).to_broadcast([128, 4, nseq]), op=ALU.mult), [nst.r, c0bc.r], [tmpn2.r])
                    P.op("dve", lambda e, hh=hh: e.tensor_tensor(out=nst.t[:, hh * 4:(hh + 1) * 4, 0:nseq], in0=tmpn2.t[:, :, 0:nseq],
                                                                 in1=bQ2.t[:, 64:64 + 4 * NB].rearrange("p (d s) -> p d s", d=4)[:, :, 0:nseq], op=ALU.add), [tmpn2.r, bQ2.r], [nst.r])

            bT = banks[2]
            bTb = bT.t[:].bitcast(BF16)
            for blk in range(8, 16):
                wt = next_wA()
                P.dma("pool", wt.t[:], wup_d[j][blk], writes=[wt.r])
                for fc in range(2):
                    fo = (blk - 8) * 2 + fc
                    bx = banks[cnt[0] % 2]
                    cnt[0] += 1

                    def mm(e, wt=wt, bx=bx, fc=fc):
                        for k in range(8):
                            ins = e.matmul(bx.t[:, 0:Tn], wt.t[:, k, fc * 128:(fc + 1) * 128], xn.t[:, k, 0:Tn], start=(k == 0), stop=(k == 7))
                        return ins
                    P.op("pe", mm, [wt.r, xn.r], [bx.r])
                    szt = sz[fo % 2]
                    eat = ea[0]
                    P.op("act", lambda e, bx=bx, szt=szt: e.activation(out=szt.t[:, 0:Tn], in_=bx.t[:, 0:Tn], func=AF.Silu), [bx.r], [szt.r])

                    def mmt(e, fo=fo):
                        for c in range(nch):
                            ins = e.transpose(out=bTb[:, c * Pn:(c + 1) * Pn], in_=hn_tok.t[0:Pn, c, fo * 128:(fo + 1) * 128], identity=identb[0:Pn, 0:Pn])
                        return ins
                    P.op("pe", mmt, [hn_tok.r, cbf.r], [bT.r])
                    P.op("act", lambda e, eat=eat, fo=fo: e.activation(out=eat.t[:, 0:Tn], in_=bTb[:, 0:Tn], func=AF.Copy, scale=V(f"ml_hn_g_{j}", fo)), [bT.r, vecs.r], [eat.r])
                    P.op("dve", lambda e, eat=eat, fo=fo: e.scalar_tensor_tensor(out=eat.t[:, 0:Tn], in0=xc.t[:, fo, 0:Tn], scalar=V(f"ml_skip_{j}", fo), in1=eat.t[:, 0:Tn],
                                                                               op0=ALU.mult, op1=ALU.add), [xc.r, eat.r, vecs.r], [eat.r])
                    P.op("dve", lambda e, eat=eat, szt=szt, fo=fo: e.tensor_tensor(out=oT.t[:, fo, 0:Tn], in0=eat.t[:, 0:Tn], in1=szt.t[:, 0:Tn], op=ALU.mult), [eat.r, szt.r], [oT.r])
            xr = xres(t0, Tn)
            for d in range(8):
                wdt = next_wB()
                wv = wdt.t[:, :].rearrange("p (f c) -> p f c", f=16)
                P.dma("pool", wv, wdn_d[j][d], writes=[wdt.r])
                bk = banks[cnt[0] % 2]
                cnt[0] += 1

                def mm(e, wv=wv, bk=bk):
                    for f in range(16):
                        ins = e.matmul(bk.t[:, 0:Tn], wv[:, f, :], oT.t[:, f, 0:Tn], start=(f == 0), stop=(f == 15))
                    return ins
                P.op("pe", mm, [wdt.r, oT.r], [bk.r])
                P.op("dve", lambda e, bk=bk, d=d: e.tensor_tensor(out=x.t[:, d, t0:t0 + Tn], in0=bk.t[:, 0:Tn], in1=x.t[:, d, t0:t0 + Tn], op=ALU.add), [bk.r] + xr, xr)
            if not samp:
                P.op("pool", lambda e: e.tensor_copy(out=xmh.t[:, :, 0:3], in_=xmh.t[:, :, Tn:Tn + 3]), [xmh.r], [xmh.r])
            if last_prompt:
                P.dma("sp", Cp_o[j].rearrange("h (dc p) e -> p (h dc) e", p=128), Cst.t[:], reads=[Cst[h_] for h_ in range(4)])
                P.dma("sp", npT_o[j], nst.t[:, :, 0:1], reads=[nst.r])
                P.dma("sp", mp_o[j], m0[m0_i[0]].t[:, 0:4], reads=[m0[m0_i[0]].r])
                P.dma("sp", convp_o[j], tailp.t[:], reads=[tailp.r])
            if samp:
                P.dma("sp", nsT_o[j], nst.t[:], reads=[nst.r])
                P.dma("sp", ms_o[j], m0[m0_i[0]].t[0:TS, 0:4], reads=[m0[m0_i[0]].r])
                P.dma("sp", convs_o[j], tails.t[:], reads=[tails.r])

    def chunk_mlp(i, j):
        P.phase()
        gname = f"norm_mix_{i}"
        T = 512
        win = P.atile([128, 8, 8, 256], BF16)
        wsP = P.atile([128, 4, 128], F32)
        wsS = P.atile([64, 4, 64], F32)
        wsPb = P.atile([128, 4, 128], BF16)
        wsSb = P.atile([64, 4, 64], BF16)
        bsP = P.atile([128, 4, 128], F32)
        bsS = P.atile([128, 4, 64], F32)
        xn = P.atile([128, 8, T], BF16)
        sqb = P.atile([128, 8, T], BF16)
        uT = P.atile([128, 8, T], BF16)
        vT = P.atile([128, 8, T], F32)
        vsq = sqb
        mu = P.atile([128, T], F32)
        rst = mu
        var = P.atile([128, T], F32)
        tmp = [P.atile([128, T], F32) for _ in range(2)]
        vn = P.atile([128, 8, T], BF16)
        vnf = P.atile([128, 8, TS], F32)
        vn_tok = P.atile([128, 1024], BF16)
        yT = xn
        for blk in range(8):
            P.dma("pool", win.t[:, blk], win_d[j][blk], writes=[win.r])
        P.dma("sp", wsP.t[:], wsP_d[j], writes=[wsP.r])
        P.dma("sp", wsS.t[:], wsS_d[j], writes=[wsS.r])
        P.dma("sp", bsP.t[:], bsP_d[j], writes=[bsP.r])
        P.dma("sp", bsS.t[:], bsS_d[j], writes=[bsS.r])
        P.op("dve", lambda e: e.tensor_tensor(out=wsPb.t[:], in0=wsP.t[:], in1=C("maskP", 128, 128).unsqueeze(1).to_broadcast([128, 4, 128]), op=ALU.mult),
             [wsP.r, consts.r], [wsPb.r])
        P.op("dve", lambda e: e.tensor_tensor(out=wsSb.t[:], in0=wsS.t[:], in1=C("maskS", 64, 64).unsqueeze(1).to_broadcast([64, 4, 64]), op=ALU.mult),
             [wsS.r, consts.r], [wsSb.r])
        cnt = 0
        for ti, (t0, Tn) in enumerate(FFN_TILES):
            samp = ti == 4
            Pn = TS if samp else 128
            nch = Tn // Pn
            rmsnorm(t0, Tn, gname, xn, xn.r, sqb, rst, banks[6])
            for fo in range(16):
                bx = banks[cnt % 2]
                cnt += 1

                def mm(e, bx=bx, fo=fo):
                    for k in range(8):
                        ins = e.matmul(bx.t[:, 0:Tn], win.t[:, fo // 2, k, (fo % 2) * 128:(fo % 2 + 1) * 128], xn.t[:, k, 0:Tn], start=(k == 0), stop=(k == 7))
                    return ins
                P.op("pe", mm, [win.r, xn.r], [bx.r])
                if fo < 8:
                    P.op("act", lambda e, bx=bx, fo=fo: e.activation(out=uT.t[:, fo, 0:Tn], in_=bx.t[:, 0:Tn], func=AF.Gelu, bias=V(f"cm_b_in_{j}", fo)), [bx.r, vecs.r], [uT.r])
                else:
                    P.op("act", lambda e, bx=bx, fo=fo: e.activation(out=vT.t[:, fo - 8, 0:Tn], in_=bx.t[:, 0:Tn], func=AF.Gelu, bias=V(f"cm_b_in_{j}", fo)), [bx.r, vecs.r], [vT.r])
            P.op("act", lambda e: e.activation(out=vsq.t[:, :, 0:Tn], in_=vT.t[:, :, 0:Tn], func=AF.Square), [vT.r], [vsq.r])
            bM1, bM2 = banks[2], banks[3]

            def mm1(e):
                for k in range(8):
                    ins = e.matmul(bM1.t[:, 0:Tn], C("onesP", 128, 128), vT.t[:, k, 0:Tn], start=(k == 0), stop=(k == 7))
                return ins
            P.op("pe", mm1, [vT.r, consts.r], [bM1.r])

            def mm2(e):
                for k in range(8):
                    ins = e.matmul(bM2.t[:, 0:Tn], onesb, vsq.t[:, k, 0:Tn], start=(k == 0), stop=(k == 7))
                return ins
            P.op("pe", mm2, [vsq.r, cbf.r], [bM2.r])
            P.op("dve", lambda e: e.tensor_scalar(out=mu.t[:, 0:Tn], in0=bM1.t[:, 0:Tn], scalar1=1.0 / D, scalar2=None, op0=ALU.mult), [bM1.r], [mu.r])
            P.op("dve", lambda e: e.tensor_tensor(out=var.t[:, 0:Tn], in0=mu.t[:, 0:Tn], in1=mu.t[:, 0:Tn], op=ALU.mult), [mu.r], [var.r])
            P.op("dve", lambda e: e.scalar_tensor_tensor(out=var.t[:, 0:Tn], in0=bM2.t[:, 0:Tn], scalar=1.0 / D, in1=var.t[:, 0:Tn], op0=ALU.mult, op1=ALU.subtract),
                 [bM2.r, var.r], [var.r])
            P.op("act", lambda e: e.activation(out=var.t[:, 0:Tn], in_=var.t[:, 0:Tn], func=AF.Sqrt, bias=V("c_eps")), [var.r, vecs.r], [var.r])
            P.op("dve", lambda e: e.reciprocal(out=var.t[:, 0:Tn], in_=var.t[:, 0:Tn]), [var.r], [var.r])
            for k in range(8):
                tt = tmp[k % 2]
                P.op("dve", lambda e, k=k, tt=tt: e.tensor_tensor(out=tt.t[:, 0:Tn], in0=vT.t[:, k, 0:Tn], in1=mu.t[:, 0:Tn], op=ALU.subtract), [vT.r, mu.r], [tt.r])
                P.op("dve", lambda e, k=k, tt=tt: e.scalar_tensor_tensor(out=vn.t[:, k, 0:Tn], in0=tt.t[:, 0:Tn], scalar=V(f"cm_ln_g_{j}", k), in1=var.t[:, 0:Tn],
                                                                       op0=ALU.mult, op1=ALU.mult), [tt.r, var.r, vecs.r], [vn.r])
                if samp:
                    P.op("dve", lambda e, k=k, tt=tt: e.scalar_tensor_tensor(out=vnf.t[:, k, 0:Tn], in0=tt.t[:, 0:Tn], scalar=V(f"cm_ln_g_{j}", k), in1=var.t[:, 0:Tn],
                                                                           op0=ALU.mult, op1=ALU.mult), [tt.r, var.r, vecs.r], [vnf.r])
            if samp:
                P.dma("sp", vsT_o[j], vnf.t[:], reads=[vnf.r])
            bT = banks[4]
            bTb = bT.t[:].bitcast(BF16)
            wsb = wsSb if samp else wsPb
            bsb = bsS if samp else bsP
            for c in range(nch):
                cs = slice(c * Pn, (c + 1) * Pn)

                def mmt(e, cs=cs):
                    for k in range(8):
                        ins = e.transpose(out=bTb[0:Pn, k * 128:(k + 1) * 128], in_=vn.t[:, k, cs], identity=identb)
                    return ins
                P.op("pe", mmt, [vn.r, cbf.r], [bT.r])
                P.op("act", lambda e: e.activation(out=vn_tok.t[0:Pn, :], in_=bTb[0:Pn, :], func=AF.Copy), [bT.r], [vn_tok.r])
                for k in range(8):
                    g = k // 2
                    bmx = banks[cnt % 2]
                    cnt += 1
                    tt = tmp[k % 2]
                    P.op("pe", lambda e, bmx=bmx, k=k, g=g: e.matmul(bmx.t[:, 0:Pn], vn_tok.t[0:Pn, k * 128:(k + 1) * 128], wsb.t[0:Pn, g, 0:Pn], start=True, stop=True),
                         [vn_tok.r, wsb.r], [bmx.r])
                    P.op("dve", lambda e, bmx=bmx, tt=tt, g=g: e.tensor_tensor(out=tt.t[:, 0:Pn], in0=bmx.t[:, 0:Pn], in1=bsb.t[:, g, 0:Pn], op=ALU.add), [bmx.r, bsb.r], [tt.r])
                    P.op("dve", lambda e, tt=tt, k=k, cs=cs: e.tensor_tensor(out=yT.t[:, k, cs], in0=tt.t[:, 0:Pn], in1=uT.t[:, k, cs], op=ALU.mult), [tt.r, uT.r], [yT.r])
            for d in range(8):
                bk = banks[cnt % 2]
                cnt += 1
                wdt = next_wB()
                wv = wdt.t[:, 0:1024].rearrange("p (f c) -> p f c", f=8)
                P.dma("pool", wv, wout_d[j][d], writes=[wdt.r])

                def mm(e, bk=bk, wv=wv):
                    for f in range(8):
                        ins = e.matmul(bk.t[:, 0:Tn], wv[:, f, :], yT.t[:, f, 0:Tn], start=(f == 0), stop=(f == 7))
                    return ins
                P.op("pe", mm, [wdt.r, yT.r], [bk.r])
                P.op("dve", lambda e, bk=bk, d=d: e.tensor_tensor(out=x.t[:, d, t0:t0 + Tn], in0=bk.t[:, 0:Tn], in1=x.t[:, d, t0:t0 + Tn], op=ALU.add), [bk.r, x[ti]], [x[ti]])

    if dbg == "ffn":
        ffn(0, 1)
    for i in range(n_layers):
        ffn(i, 1)
        if i % 2 == 0:
            if dbg != "onlysamp":
                mlstm(i, i // 2, False)
            if dbg == "newsamp":
                mlstm(i, i // 2, True)
            elif dbg != "nosamp":
                mlstm_v1_sample(i, i // 2)
        else:
            chunk_mlp(i, i // 2)
        ffn(i, 2)

    P.phase()
    sqb = P.atile([128, 8, 512], BF16)
    rst = P.atile([128, 512], F32)
    yst = [P.atile([128, 8, 512], F32) for _ in range(2)]
    yv = yT_o.rearrange("(k p) t -> p k t", p=128)
    for ti, (t0, n) in enumerate(FFN_TILES):
        ys = yst[ti % 2]
        rmsnorm(t0, n, "norm_final", ys, ys.r, sqb, rst, banks[6])
        P.dma("sp", yv[:, :, t0:t0 + n], ys.t[:, :, 0:n], reads=[ys.r])
    P.emit()
    return nc, P


def _cols(v):
    v = np.asarray(v, np.float32)
    return np.ascontiguousarray(v.reshape(-1, 128).T)


def _blk_k(W, ncol):
    K, F = W.shape
    return np.ascontiguousarray(W.reshape(K // 128, 128, F // ncol, ncol).transpose(2, 1, 0, 3))


def _blk_f(W, ncol):
    Fin, Dn = W.shape
    return np.ascontiguousarray(W.reshape(Fin // 128, 128, Dn // ncol, ncol).transpose(2, 1, 0, 3))


def prep_inputs(inp):
    f32 = np.float32
    g = {k: np.asarray(v) for k, v in inp.items()}
    shared = {}
    vecs = np.zeros((128, VL["_n"]), f32)

    def put(name, v):
        c = _cols(v)
        vecs[:, VL[name]:VL[name] + c.shape[1]] = c
    for i in range(DEPTH):
        put(f"norm_ff1_{i}", g["norm_ff1"][i])
        put(f"norm_mix_{i}", g["norm_mix"][i])
        put(f"norm_ff2_{i}", g["norm_ff2"][i])
    put("norm_final", g["norm_final"])
    for j in range(2):
        for w in range(4):
            put(f"ml_wconv_{j}_{w}", g["ml_w_conv"][j, w])
        put(f"ml_bconv_{j}", g["ml_b_conv"][j])
        put(f"ml_hn_g_{j}", g["ml_hn_g"][j])
        put(f"ml_skip_{j}", g["ml_skip"][j])
        put(f"cm_b_in_{j}", g["cm_b_in"][j])
        put(f"cm_ln_g_{j}", g["cm_ln_g"][j])
    vecs[:, VL["c_eps"]] = EPS
    vecs[:, VL["c_lnscale"]] = math.log(DH ** -0.5)
    shared["vecs"] = vecs
    cst = np.zeros((128, CL["_n"]), f32)
    cst[:, CL["ident"]:CL["ident"] + 128] = np.eye(128, dtype=f32)
    cst[:, CL["maskP"]:CL["maskP"] + 128] = np.triu(np.ones((128, 128), f32))
    cst[:, CL["onesP"]:CL["onesP"] + 128] = 1.0
    cst[:64, CL["maskS"]:CL["maskS"] + 64] = np.kron(np.eye(NB, dtype=f32), np.triu(np.ones((4, 4), f32)))
    cst[:64, CL["onesS"]:CL["onesS"] + 64] = np.kron(np.eye(NB, dtype=f32), np.ones((4, 4), f32))
    cst[:64, CL["seqS"]:CL["seqS"] + 16] = np.kron(np.eye(NB, dtype=f32), np.ones((4, 1), f32))
    fs = np.zeros((64, 16), f32)
    fs[np.arange(16) * 4, np.arange(16)] = 1.0
    cst[:64, CL["firstS"]:CL["firstS"] + 16] = fs
    cst[:, CL["seqP"]] = 1.0
    cst[0, CL["firstP"]] = 1.0
    shared["consts"] = cst
    cm = np.zeros((128, NB, TS), f32)
    for b in range(NB):
        cm[:, b, 4 * b:4 * b + 4] = 1.0
    shared["colmask"] = cm
    for i in range(DEPTH):
        for a in (1, 2):
            shared[f"wg{i}{a}"] = _blk_k(g[f"ffn{a}_w_gate"][i], 256)
            shared[f"wu{i}{a}"] = _blk_k(g[f"ffn{a}_w_up"][i], 256)
            shared[f"wd{i}{a}"] = _blk_f(g[f"ffn{a}_w_down"][i], 256)
    for j in range(2):
        shared[f"wup{j}"] = _blk_k(g["ml_w_up"][j], 256)
        shared[f"wdn{j}"] = _blk_f(g["ml_w_down"][j], 128)
        bd = np.zeros((128, 3, 16, 128), f32)
        for wi, nm in enumerate(["ml_w_q", "ml_w_k", "ml_w_v"]):
            w = g[nm][j].reshape(16, 32, 4, 4)
            for n in range(32):
                bd[4 * n:4 * n + 4, wi, :, 4 * n:4 * n + 4] = w[:, n].transpose(1, 0, 2)
        shared[f"bd{j}"] = bd
        wcat = np.concatenate([g["ml_w_ig"][j], g["ml_w_fg"][j]], axis=1)
        shared[f"wgate{j}"] = np.ascontiguousarray(wcat.reshape(48, 128, 8).transpose(1, 0, 2))
        shared[f"bgate{j}"] = np.ascontiguousarray(np.broadcast_to(np.concatenate([g["ml_b_ig"][j], g["ml_b_fg"][j]])[None, :], (128, 8))).astype(f32)
        shared[f"win{j}"] = _blk_k(g["cm_w_in"][j], 256)
        shared[f"wout{j}"] = _blk_f(g["cm_w_out"][j], 128)
        ws = g["cm_w_s"][j]
        shared[f"wsP{j}"] = np.ascontiguousarray(ws.transpose(2, 0, 1))
        wss = np.zeros((64, 4, 64), f32)
        for b in range(NB):
            wss[4 * b:4 * b + 4, :, 4 * b:4 * b + 4] = ws[:, :4, :4].transpose(2, 0, 1)
        shared[f"wsS{j}"] = wss
        bs = g["cm_b_s"][j]
        shared[f"bsP{j}"] = np.ascontiguousarray(np.broadcast_to(bs[None, :, :], (128, 4, 128))).astype(f32)
        shared[f"bsS{j}"] = np.ascontiguousarray(np.broadcast_to(np.tile(bs[:, :4], (1, NB))[None, :, :], (128, 4, 64))).astype(f32)
    in_maps = []
    for c in range(N_CORES):
        m = dict(shared)
        sl = slice(c * NB, (c + 1) * NB)
        xp = g["x_prompt"][c]
        xs = g["x_sample"][sl].reshape(TS, D)
        m["xT"] = np.ascontiguousarray(np.concatenate([xp, xs], axis=0).T)
        m["sC"] = np.ascontiguousarray(g["state_C"][:, sl])
        sn = g["state_n"][:, sl]
        m["snT"] = np.ascontiguousarray(sn.reshape(2, NB, 16, 128).transpose(0, 3, 2, 1))
        m["sm"] = np.ascontiguousarray(np.repeat(g["state_m"][:, sl], 4, axis=1))
        sc = g["state_conv"][:, sl]
        m["sconvT"] = np.ascontiguousarray(sc.reshape(2, NB, 3, 16, 128).transpose(0, 4, 3, 1, 2))
        in_maps.append(m)
    return in_maps


def assemble(results, n_cores=N_CORES):
    f32 = np.float32
    y_p = np.zeros((8, TP, D), f32)
    y_s = np.zeros((128, 4, D), f32)
    C_p = np.zeros((2, 8, H, DH, DH), f32)
    n_p = np.zeros((2, 8, H, DH), f32)
    m_p = np.zeros((2, 8, H), f32)
    cv_p = np.zeros((2, 8, 3, INNER), f32)
    C_s = np.zeros((2, 128, H, DH, DH), f32)
    n_s = np.zeros((2, 128, H, DH), f32)
    m_s = np.zeros((2, 128, H), f32)
    cv_s = np.zeros((2, 128, 3, INNER), f32)
    v_s = np.zeros((2, 128, 4, D), f32)
    for c in range(n_cores):
        r = results[c]
        sl = slice(c * NB, (c + 1) * NB)
        yT = r["yT"]
        y_p[c] = yT[:, :TP].T
        y_s[sl] = yT[:, TP:].T.reshape(NB, 4, D)
        C_p[:, c] = r["Cp"]
        n_p[:, c] = r["npT"][:, :, :, 0].transpose(0, 2, 1).reshape(2, H, DH)
        m_p[:, c] = r["mp"][:, 0, :]
        cv_p[:, c] = r["convpT"].transpose(0, 3, 2, 1).reshape(2, 3, INNER)
        C_s[:, sl] = r["Cs"]
        n_s[:, sl] = r["nsT"].transpose(0, 3, 2, 1).reshape(2, NB, H, DH)
        m_s[:, sl] = r["ms"][:, ::4, :]
        cv_s[:, sl] = r["convsT"].transpose(0, 3, 4, 2, 1).reshape(2, NB, 3, INNER)
        v_s[:, sl] = r["vsT"].transpose(0, 3, 2, 1).reshape(2, NB, 4, D)
    return (y_p, y_s, C_p, n_p, m_p, cv_p, C_s, n_s, m_s, cv_s, v_s)


_NC_CACHE = {}


def kernel(**inputs):
    in_maps = prep_inputs(inputs)
    if "nc" not in _NC_CACHE:
        _NC_CACHE["nc"] = build_program()[0]
    nc = _NC_CACHE["nc"]
    res = run_bass_kernel_spmd(nc, in_maps, core_ids=list(range(N_CORES)))
    return assemble(res.results)
```

```python
from contextlib import ExitStack
import math
import types
import numpy as np
import concourse.bass as bass
import concourse.mybir as mybir
from concourse.bass_utils import run_bass_kernel_spmd

F32 = mybir.dt.float32
BF16 = mybir.dt.bfloat16
AF = mybir.ActivationFunctionType
ALU = mybir.AluOpType
AX = mybir.AxisListType

DEPTH = 4
D = 1024
TP = 2048
TS = 64
TT = TP + TS
FF = 2816
NFC = 22
INNER = 2048
H = 4
DH = 512
EPS = 1e-6
NB = 16
N_CORES = 8

ENGS = ("pe", "act", "dve", "pool", "sp")
N_LANES = 8


def _freeze(fn, depth=0):
    if not isinstance(fn, types.FunctionType) or fn.__closure__ is None or depth > 4:
        return fn
    cells = []
    for c in fn.__closure__:
        try:
            v = c.cell_contents
        except ValueError:
            cells.append(c)
            continue
        if isinstance(v, types.FunctionType):
            v = _freeze(v, depth + 1)
        cells.append(types.CellType(v))
    g = types.FunctionType(fn.__code__, fn.__globals__, fn.__name__, fn.__defaults__, tuple(cells))
    g.__kwdefaults__ = fn.__kwdefaults__
    return g


class Res:
    __slots__ = ("lw", "rd")

    def __init__(self, lw=None):
        self.lw = lw
        self.rd = []


class Tile:
    def __init__(self, t, base_lw=None):
        self.t = t
        self._res = {}
        self.base_lw = base_lw

    def __getitem__(self, key):
        r = self._res.get(key)
        if r is None:
            r = self._res[key] = Res(self.base_lw)
        return r

    @property
    def r(self):
        return self[None]


class Op:
    __slots__ = ("eng", "fn", "deps", "dma", "lane", "val", "signal", "idx")


class Prog:
    def __init__(self, nc, arena_words):
        self.nc = nc
        self.ops = []
        self.stack = ExitStack()
        self.n_t = 0
        self.arena = self.stack.enter_context(nc.sbuf_tensor("arena", [128, arena_words], F32))
        self.arena_words = arena_words
        self.arena_ptr = 0
        self.arena_tiles = []
        self.cur_barrier = None
        self.dummy = self.sbuf([128, 8], F32, "dummy_bar")

    def sbuf(self, shape, dtype, name=None):
        self.n_t += 1
        t = self.stack.enter_context(self.nc.sbuf_tensor("sb_" + (name or f"t{self.n_t}"), list(shape), dtype))
        tl = Tile(t)
        self.persist = getattr(self, "persist", [])
        self.persist.append(tl)
        return tl

    def psum(self, shape, dtype, name=None):
        self.n_t += 1
        t = self.stack.enter_context(self.nc.psum_tensor("ps_" + (name or f"t{self.n_t}"), list(shape), dtype))
        tl = Tile(t)
        self.persist = getattr(self, "persist", [])
        self.persist.append(tl)
        return tl

    def phase(self):
        ress = []
        for tl in self.arena_tiles:
            ress.extend(tl._res.values())
        if ress:
            d = self.dummy
            o = self.op("pool", lambda e: e.memset(d.t[:, 0:1], 0.0), [], ress + [d.r])
            self.cur_barrier = o.idx
        self.arena_tiles = []
        self.arena_ptr = 0

    def barrier_all(self):
        ress = []
        for tl in self.arena_tiles + getattr(self, "persist", []):
            ress.extend(tl._res.values())
        d = self.dummy
        if d.r not in ress:
            ress.append(d.r)
        self.op("pool", lambda e: e.memset(d.t[:, 0:1], 0.0), [], ress)

    def atile(self, shape, dtype):
        n = 1
        for s in shape[1:]:
            n *= s
        words = (n * (2 if dtype == BF16 else 4) + 3) // 4
        words = (words + 7) // 8 * 8
        a0 = self.arena_ptr
        self.arena_ptr += words
        assert self.arena_ptr <= self.arena_words, f"arena overflow {self.arena_ptr} > {self.arena_words}"
        ap = self.arena.__getitem__((slice(0, shape[0]), slice(a0, a0 + words)))
        if dtype == BF16:
            ap = ap.bitcast(BF16)
        ap = ap[:, 0:n]
        if len(shape) == 3:
            ap = ap.rearrange("p (a b) -> p a b", a=shape[1])
        elif len(shape) == 4:
            ap = ap.rearrange("p (a b c) -> p a b c", a=shape[1], b=shape[2])
        tl = Tile(ap, self.cur_barrier)
        self.arena_tiles.append(tl)
        return tl

    def op(self, eng, fn, reads=(), writes=(), dma=False):
        o = Op()
        o.eng, o.fn, o.dma = eng, _freeze(fn), dma
        o.idx = len(self.ops)
        o.lane = None
        o.val = None
        o.signal = False
        deps = set()
        for r in reads:
            if r.lw is not None:
                deps.add(r.lw)
        for w in writes:
            if w.lw is not None:
                deps.add(w.lw)
            for x in w.rd:
                deps.add(x)
        deps.discard(o.idx)
        o.deps = deps
        for r in reads:
            r.rd.append(o.idx)
        for w in writes:
            w.lw = o.idx
            w.rd = []
        self.ops.append(o)
        return o

    def dma(self, eng, out_ap, in_ap, reads=(), writes=()):
        return self.op(eng, lambda e: e.dma_start(out=out_ap, in_=in_ap, allow_slow_non_contiguous=True), reads, writes, dma=True)

    def emit(self):
        nc = self.nc
        ops = self.ops
        fin = Op()
        fin.eng, fin.fn, fin.dma = "sp", None, False
        fin.idx = len(ops)
        fin.deps = {o.idx for o in ops if o.dma}
        fin.lane = None
        fin.val = None
        fin.signal = False
        ops.append(fin)
        for o in ops:
            nd = set()
            for d in o.deps:
                p = ops[d]
                if (not p.dma) and (not o.dma) and p.eng == o.eng and o.eng == "pe":
                    continue
                nd.add(d)
            o.deps = nd
        for o in ops:
            for d in o.deps:
                ops[d].signal = True
        st = self.stack
        sems = {e: st.enter_context(nc.semaphore(f"s_{e}")) for e in ENGS}
        lanes = {e: [st.enter_context(nc.semaphore(f"l_{e}{i}")) for i in range(N_LANES)] for e in ("sp", "pool")}
        cnt = {e: 0 for e in ENGS}
        lane_cnt = {e: [0] * N_LANES for e in lanes}
        lane_prev = {e: [None] * N_LANES for e in lanes}
        dma_seq = {e: 0 for e in lanes}
        for o in ops:
            if o.dma:
                i = dma_seq[o.eng] % N_LANES
                dma_seq[o.eng] += 1
                o.lane = (o.eng, i)
                prev = lane_prev[o.eng][i]
                if prev is not None:
                    o.deps.add(prev)
                lane_cnt[o.eng][i] += 16
                o.val = lane_cnt[o.eng][i]
                lane_prev[o.eng][i] = o.idx
            elif o.signal:
                cnt[o.eng] += 1
                o.val = cnt[o.eng]

        def sem_of(p):
            if p.dma:
                return lanes[p.lane[0]][p.lane[1]], ("L",) + p.lane
            return sems[p.eng], ("E", p.eng)

        by_eng = {e: [o for o in ops if o.eng == e] for e in ENGS}
        self.stats = {e: len(by_eng[e]) for e in ENGS}
        blk = st.enter_context(nc.Block())

        def run(eng_name, eng):
            known = {}
            for o in by_eng[eng_name]:
                need = {}
                for d in o.deps:
                    p = ops[d]
                    s, key = sem_of(p)
                    if known.get(key, 0) >= p.val:
                        continue
                    if key not in need or need[key][1] < p.val:
                        need[key] = (s, p.val)
                for key, (s, v) in need.items():
                    eng.wait_ge(s, v)
                    known[key] = v
                if o.fn is None:
                    continue
                ins = o.fn(eng)
                if o.dma:
                    ins.then_inc(lanes[o.lane[0]][o.lane[1]], 16)
                elif o.signal:
                    ins.then_inc(sems[o.eng], 1)

        @blk.tensor
        def _(e):
            run("pe", e)

        @blk.scalar
        def _(e):
            run("act", e)

        @blk.vector
        def _(e):
            run("dve", e)

        @blk.gpsimd
        def _(e):
            run("pool", e)

        @blk.sync
        def _(e):
            run("sp", e)

        st.close()


def vec_layout():
    lay = {}
    c = 0

    def add(name, n):
        nonlocal c
        lay[name] = c
        c += n

    for i in range(DEPTH):
        add(f"norm_ff1_{i}", 8)
        add(f"norm_mix_{i}", 8)
        add(f"norm_ff2_{i}", 8)
    add("norm_final", 8)
    for j in range(2):
        for w in range(4):
            add(f"ml_wconv_{j}_{w}", 16)
        add(f"ml_bconv_{j}", 16)
        add(f"ml_hn_g_{j}", 16)
        add(f"ml_skip_{j}", 16)
        add(f"cm_b_in_{j}", 16)
        add(f"cm_ln_g_{j}", 8)
    add("c_eps", 1)
    add("c_lnscale", 1)
    lay["_n"] = c
    return lay


def const_layout():
    lay = {}
    c = 0
    for name, n in [("ident", 128), ("maskP", 128), ("onesP", 128), ("maskS", 64), ("onesS", 64),
                    ("seqS", 16), ("firstS", 16), ("seqP", 1), ("firstP", 1)]:
        lay[name] = c
        c += n
    lay["_n"] = c
    return lay


VL = vec_layout()
CL = const_layout()

FFN_TILES = [(0, 512), (512, 512), (1024, 512), (1536, 512), (2048, 64)]
FFN_PARTS = [(0, 4), (4, 8), (8, 11)]
ML_T = 256


def build_program(n_layers=DEPTH):
    nc = bass.Bass("TRN2", target_bir_lowering=False)
    import os
    ARENA_WORDS = 28712
    P = Prog(nc, ARENA_WORDS)

    def din(name, shape):
        return nc.dram_tensor(name, list(shape), F32, kind="ExternalInput").ap()

    def dout(name, shape):
        return nc.dram_tensor(name, list(shape), F32, kind="ExternalOutput").ap()

    xT_d = din("xT", [D, TT])
    vecs_d = din("vecs", [128, VL["_n"]])
    consts_d = din("consts", [128, CL["_n"]])
    colmask_d = din("colmask", [128, NB, TS])
    wg_d, wu_d, wd_d = {}, {}, {}
    for i in range(DEPTH):
        for a in (1, 2):
            wg_d[i, a] = din(f"wg{i}{a}", [11, 128, 8, 256])
            wu_d[i, a] = din(f"wu{i}{a}", [11, 128, 8, 256])
            wd_d[i, a] = din(f"wd{i}{a}", [4, 128, NFC, 256])
    wup_d = [din(f"wup{j}", [16, 128, 8, 256]) for j in range(2)]
    wdn_d = [din(f"wdn{j}", [8, 128, 16, 128]) for j in range(2)]
    bd_d = [din(f"bd{j}", [128, 3, 16, 128]) for j in range(2)]
    wgate_d = [din(f"wgate{j}", [128, 48, 8]) for j in range(2)]
    bgate_d = [din(f"bgate{j}", [128, 8]) for j in range(2)]
    win_d = [din(f"win{j}", [8, 128, 8, 256]) for j in range(2)]
    wout_d = [din(f"wout{j}", [8, 128, 8, 128]) for j in range(2)]
    wsP_d = [din(f"wsP{j}", [128, 4, 128]) for j in range(2)]
    wsS_d = [din(f"wsS{j}", [64, 4, 64]) for j in range(2)]
    bsP_d = [din(f"bsP{j}", [128, 4, 128]) for j in range(2)]
    bsS_d = [din(f"bsS{j}", [128, 4, 64]) for j in range(2)]
    sC_d = din("sC", [2, NB, H, DH, DH])
    snT_d = din("snT", [2, 128, 16, NB])
    sm_d = din("sm", [2, TS, 4])
    sconv_d = din("sconvT", [2, 128, 16, NB, 3])

    yT_o = dout("yT", [D, TT])
    Cp_o = dout("Cp", [2, H, DH, DH])
    npT_o = dout("npT", [2, 128, 16, 1])
    mp_o = dout("mp", [2, 128, 4])
    convp_o = dout("convpT", [2, 128, 16, 3])
    Cs_o = dout("Cs", [2, NB, H, DH, DH])
    nsT_o = dout("nsT", [2, 128, 16, NB])
    ms_o = dout("ms", [2, TS, 4])
    convs_o = dout("convsT", [2, 128, 16, NB, 3])
    vsT_o = dout("vsT", [2, 128, 8, TS])

    x = P.sbuf([128, 8, TT], F32, "x")
    vecs = P.sbuf([128, VL["_n"]], F32, "vecs")
    consts = P.sbuf([128, CL["_n"]], F32, "consts")
    cbf = P.sbuf([128, 512], BF16, "cbf")
    wA = [P.sbuf([128, 8, 256], BF16, f"wA{i}") for i in range(4)]
    wB = [P.sbuf([128, 2048], BF16, f"wB{i}") for i in range(2)]
    banks = [P.psum([128, 512], F32, f"bank{i}") for i in range(8)]

    def V(name, k=0, p=128):
        c = VL[name] + k
        return vecs.t[0:p, c:c + 1]

    def C(name, p, n):
        c = CL[name]
        return consts.t[0:p, c:c + n]

    P.dma("sp", vecs.t[:], vecs_d, writes=[vecs.r])
    P.dma("sp", consts.t[:], consts_d, writes=[consts.r])
    xv = xT_d.rearrange("(k p) t -> p k t", p=128)
    for ti, (t0, n) in enumerate(FFN_TILES):
        P.dma("sp", x.t[:, :, t0:t0 + n], xv[:, :, t0:t0 + n], writes=[x[ti]])
    P.op("dve", lambda e: e.tensor_copy(out=cbf.t[:, 0:128], in_=consts.t[:, CL["ident"]:CL["ident"] + 128]), [consts.r], [cbf.r])
    P.op("dve", lambda e: e.tensor_copy(out=cbf.t[:, 128:256], in_=consts.t[:, CL["onesP"]:CL["onesP"] + 128]), [consts.r], [cbf.r])
    P.op("dve", lambda e: e.tensor_copy(out=cbf.t[:, 256:272], in_=consts.t[:, CL["seqS"]:CL["seqS"] + 16]), [consts.r], [cbf.r])
    P.op("dve", lambda e: e.tensor_copy(out=cbf.t[:, 272:273], in_=consts.t[:, CL["seqP"]:CL["seqP"] + 1]), [consts.r], [cbf.r])
    identb = cbf.t[:, 0:128]
    onesb = cbf.t[:, 128:256]

    def xres(t0, n):
        out = []
        for ti, (a, m) in enumerate(FFN_TILES):
            if a < t0 + n and t0 < a + m:
                out.append(x[ti])
        return out

    import os
    dbg = os.environ.get("KDBG", "")
    dbg_stage = None
    wA_i = [0]
    wB_i = [0]

    def next_wA():
        t = wA[wA_i[0] % len(wA)]
        wA_i[0] += 1
        return t

    def next_wB():
        t = wB[wB_i[0] % len(wB)]
        wB_i[0] += 1
        return t

    def rmsnorm(t0, n, gname, dst, dst_res, sqb, rst, bank, f32_out=False):
        xr = xres(t0, n)
        P.op("act", lambda e: e.activation(out=sqb.t[:, :, 0:n], in_=x.t[:, :, t0:t0 + n], func=AF.Square), xr, [sqb.r])

        def mm(e):
            for k in range(8):
                ins = e.matmul(bank.t[:, 0:n], onesb, sqb.t[:, k, 0:n], start=(k == 0), stop=(k == 7))
            return ins
        P.op("pe", mm, [sqb.r, cbf.r], [bank.r])
        P.op("act", lambda e: e.activation(out=rst.t[:, 0:n], in_=bank.t[:, 0:n], func=AF.Sqrt, scale=1.0 / D, bias=V("c_eps")), [bank.r, vecs.r], [rst.r])
        P.op("dve", lambda e: e.reciprocal(out=rst.t[:, 0:n], in_=rst.t[:, 0:n]), [rst.r], [rst.r])
        for k in range(8):
            P.op("dve", lambda e, k=k: e.scalar_tensor_tensor(out=dst.t[:, k, 0:n], in0=x.t[:, k, t0:t0 + n], scalar=V(gname, k),
                                                                in1=rst.t[:, 0:n], op0=ALU.mult, op1=ALU.mult),
                 xr + [rst.r, vecs.r], [dst_res])

    def ffn(i, a):
        P.phase()
        gname = f"norm_ff{a}_{i}"
        xn = P.atile([128, 8, TT], BF16)
        hbuf = P.atile([128, 8, TT], BF16)
        sqb = P.atile([128, 8, 512], BF16)
        rst = [P.atile([128, 512], F32) for _ in range(2)]
        sg = [P.atile([128, 512], F32) for _ in range(2)]
        normed = set()

        def norm_tile(ti):
            if ti in normed:
                return
            normed.add(ti)
            t0_, n_ = FFN_TILES[ti]
            xn_v = Tile(xn.t[:, :, t0_:t0_ + n_])
            rmsnorm(t0_, n_, gname, xn_v, xn[ti], sqb, rst[ti % 2], banks[6])
        norm_tile(0)
        cnt = 0
        for (b0, b1) in FFN_PARTS:
            for blk in range(b0, b1):
                wgt, wut = next_wA(), next_wA()
                P.dma("pool", wgt.t[:], wg_d[i, a][blk], writes=[wgt.r])
                P.dma("pool", wut.t[:], wu_d[i, a][blk], writes=[wut.r])
                for fc in range(2):
                    fl = (blk - b0) * 2 + fc
                    for ti, (t0, n) in enumerate(FFN_TILES):
                        if ti + 1 < len(FFN_TILES):
                            norm_tile(ti + 1)
                        bg, bu = banks[cnt % 2], banks[2 + cnt % 2]
                        sgt = sg[cnt % 2]
                        cnt += 1

                        def mm(e, wt=wgt, bk=bg, fc=fc, t0=t0, n=n):
                            for k in range(8):
                                ins = e.matmul(bk.t[:, 0:n], wt.t[:, k, fc * 128:(fc + 1) * 128], xn.t[:, k, t0:t0 + n], start=(k == 0), stop=(k == 7))
                            return ins
                        P.op("pe", mm, [wgt.r, xn[ti]], [bg.r])

                        def mm2(e, wt=wut, bk=bu, fc=fc, t0=t0, n=n):
                            for k in range(8):
                                ins = e.matmul(bk.t[:, 0:n], wt.t[:, k, fc * 128:(fc + 1) * 128], xn.t[:, k, t0:t0 + n], start=(k == 0), stop=(k == 7))
                            return ins
                        P.op("pe", mm2, [wut.r, xn[ti]], [bu.r])
                        P.op("act", lambda e, bk=bg, s=sgt, n=n: e.activation(out=s.t[:, 0:n], in_=bk.t[:, 0:n], func=AF.Silu), [bg.r], [sgt.r])
                        P.op("dve", lambda e, bk=bu, s=sgt, fl=fl, t0=t0, n=n: e.tensor_tensor(out=hbuf.t[:, fl, t0:t0 + n], in0=s.t[:, 0:n], in1=bk.t[:, 0:n], op=ALU.mult),
                             [bu.r, sgt.r], [hbuf[ti]])
            nfc = (b1 - b0) * 2
            for dp in range(4):
                wdt = next_wB()
                wv = wdt.t[:, 0:nfc * 256].rearrange("p (f c) -> p f c", f=nfc)
                P.dma("pool", wv, wd_d[i, a][dp, :, b0 * 2:b1 * 2, :], writes=[wdt.r])
                for dc in range(2):
                    d = dp * 2 + dc
                    for ti, (t0, n) in enumerate(FFN_TILES):
                        bk = banks[4 + cnt % 2]
                        cnt += 1

                        def mm(e, wv=wv, bk=bk, dc=dc, t0=t0, n=n, nfc=nfc):
                            for f in range(nfc):
                                ins = e.matmul(bk.t[:, 0:n], wv[:, f, dc * 128:(dc + 1) * 128], hbuf.t[:, f, t0:t0 + n], start=(f == 0), stop=(f == nfc - 1))
                            return ins
                        P.op("pe", mm, [wdt.r, hbuf[ti]], [bk.r])
                        P.op("dve", lambda e, bk=bk, d=d, t0=t0, n=n: e.scalar_tensor_tensor(out=x.t[:, d, t0:t0 + n], in0=bk.t[:, 0:n], scalar=0.5,
                                                                                         in1=x.t[:, d, t0:t0 + n], op0=ALU.mult, op1=ALU.add),
                             [bk.r, x[ti]], [x[ti]])

    def mlstm(i, j, samp):
        P.phase()
        gname = f"norm_mix_{i}"
        T = TS if samp else ML_T
        nseq, L, Pn, nch = (NB, 4, TS, 1) if samp else (1, 128, 128, 2)
        csn, csL = (NB, 4) if samp else (1, T)
        t0s = [TP] if samp else list(range(0, TP, T))
        NS4 = nch * nseq * 4
        bd = P.atile([128, 3, 16, 128], BF16)
        wgate = P.atile([128, 48, 8], BF16)
        bgate = P.atile([128, 8], F32)
        xn = P.atile([128, 8, T], BF16)
        xmh = P.atile([128, 16, csn, 3 + csL], BF16)
        xc = P.atile([128, 16, T], BF16)
        acc = [P.atile([128, T], F32) for _ in range(4)]
        qT = P.atile([128, 16, T], BF16)
        kT = P.atile([128, 16, T], BF16)
        sqb = Tile(kT.t[:, 0:8, :], kT.base_lw)
        sqb._res = kT._res
        vTt = [P.atile([128, T], BF16) for _ in range(2)]
        v_tk = [P.atile([128, 512], BF16) for _ in range(2)]
        kp_tk = [P.atile([128, 512], BF16) for _ in range(2)]
        Cbf = [P.atile([128, 4, 512], BF16) for _ in range(2)]
        nst = P.atile([128, 16, nseq], F32)
        nbf = P.atile([128, 16, nseq], BF16)
        ST = [P.atile([128, 128], BF16) for _ in range(4)]
        hn_tok = P.atile([128, nch, 2048], BF16)
        ea = [P.atile([128, T], F32) for _ in range(2)]
        rst = ea[0]
        sz = acc
        gT_sb = Tile(ea[1].t[0:8, :], ea[1].base_lw)
        gT_sb._res = ea[1]._res
        gg = P.atile([128, nch, 8], F32)
        e1 = P.atile([128, nch, 4], F32)
        lf = P.atile([128, nch, 4], F32)
        bt = P.atile([128, nch, 4], F32)
        bl = P.atile([128, nch, 4], F32)
        aa = P.atile([128, nch, 16], F32)
        RR = P.atile([128, nch, 4], F32)
        mstk = [P.atile([128, nch + 1, 4], F32) for _ in range(2)]
        d1 = P.atile([128, nch, 4], F32)
        es = P.atile([128, nch, 4], F32)
        d2 = P.atile([128, nch, 4], F32)
        c0 = P.atile([128, nch, 4], F32)
        d3 = P.atile([128, nch, 4], F32)
        dfl = P.atile([128, nch, 4], F32)
        amaxT = P.atile([4, nch * nseq], F32)
        amax_exp = P.atile([4, nch * Pn], F32)
        rhsx = P.atile([128, NS4], F32)
        c0bc = P.atile([128, nch, nseq, 4], F32)
        tmpn = P.atile([128, 4, nseq], F32)
        tmpn2 = P.atile([128, 4, nseq], F32)
        nqb = P.atile([128, 4], F32)
        nq = P.atile([128, 3, 4], F32)
        den = P.atile([128, 4], F32)
        t1 = P.atile([128, 4], F32)
        rs = P.atile([128, 4], F32)
        nb = P.atile([128, 4], F32)
        mv = P.atile([128, 4, 2], F32)
        st6 = [P.atile([128, 6], F32) for _ in range(4)]
        oT = qT
        if samp:
            xmc = P.atile([128, 16, TS], BF16)
            tails = P.atile([128, 16, NB, 3], F32)
            colmask = P.atile([128, NB, TS], BF16)
            qm = [P.atile([128, 4, TS], BF16) for _ in range(2)]
            kpm = [P.atile([64, 512], BF16) for _ in range(2)]
            nslot = min(12, (P.arena_words - P.arena_ptr) // 2048)
            assert nslot >= 4, nslot
            Cslots = [P.atile([128, 4, 512], F32) for _ in range(nslot)]
            P.dma("pool", colmask.t[:], colmask_d, writes=[colmask.r])
        else:
            tailp = P.atile([128, 16, 3], F32)
            Cst = P.atile([128, 16, 512], F32)
            P.op("pool", lambda e: e.memset(Cst.t[:], 0.0), [], [Cst[h_] for h_ in range(4)])

        P.dma("pool", bd.t[:], bd_d[j], writes=[bd.r])
        P.dma("pool", wgate.t[:], wgate_d[j], writes=[wgate.r])
        P.dma("sp", bgate.t[:], bgate_d[j], writes=[bgate.r])
        if samp:
            P.dma("sp", nst.t[:], snT_d[j], writes=[nst[h_] for h_ in range(4)])
            P.dma("sp", mstk[1].t[0:TS, nch, :], sm_d[j], writes=[mstk[1].r])
            P.dma("sp", tails.t[:], sconv_d[j], writes=[tails.r])
            P.op("dve", lambda e: e.tensor_copy(out=xmh.t[:, :, :, 0:3], in_=tails.t[:]), [tails.r], [xmh.r] + [xmh[g_] for g_ in range(4)])
        else:
            P.op("pool", lambda e: e.memset(nst.t[:], 0.0), [], [nst[h_] for h_ in range(4)])
            P.op("pool", lambda e: e.memset(mstk[1].t[:], 0.0), [], [mstk[1].r])
            P.op("pool", lambda e: e.memset(xmh.t[:, :, :, 0:3], 0.0), [], [xmh.r] + [xmh[g_] for g_ in range(4)])
        mask = C("maskS" if samp else "maskP", Pn, Pn)
        onesblk = C("onesS" if samp else "onesP", Pn, Pn)
        seqm = C("seqS" if samp else "seqP", Pn, nseq)
        firstm = C("firstS" if samp else "firstP", Pn, nseq)
        seqmb = cbf.t[0:Pn, 256:256 + nseq] if samp else cbf.t[0:Pn, 272:273]
        cnt = [0]
        slot_i = [0]

        def xm_op(fo, a=0, b=None):
            b = T if b is None else b
            if samp:
                return xmc.t[:, fo, a:b]
            return xmh.t[:, fo, 0, 3 + a:3 + b]

        for tidx, t0 in enumerate(t0s):
            last_prompt = (not samp) and (t0 + T == TP)
            mcur, mprev = mstk[tidx % 2], mstk[1 - tidx % 2]
            rmsnorm(t0, T, gname, xn, xn.r, sqb, rst, banks[7])
            P.op("dve", lambda e: e.tensor_copy(out=mcur.t[0:Pn, 0, :], in_=mprev.t[0:Pn, nch, :]), [mprev.r], [mcur.r])

            wts = {}

            def B1(g):
                for fo in range(4 * g, 4 * g + 4):
                    if fo % 2 == 0:
                        wt = next_wA()
                        P.dma("pool", wt.t[:], wup_d[j][fo // 2], writes=[wt.r])
                        wts[fo // 2] = wt
                    wt = wts[fo // 2]
                    fc = fo % 2
                    bx = banks[fo % 2]

                    def mm(e, wt=wt, bx=bx, fc=fc):
                        for k in range(8):
                            ins = e.matmul(bx.t[:, 0:T], wt.t[:, k, fc * 128:(fc + 1) * 128], xn.t[:, k, 0:T], start=(k == 0), stop=(k == 7))
                        return ins
                    P.op("pe", mm, [wt.r, xn.r], [bx.r])
                    bx3 = bx.t[:, 0:T].rearrange("p (s l) -> p s l", s=csn)
                    P.op("act", lambda e, bx3=bx3, fo=fo: e.activation(out=xmh.t[:, fo, :, 3:3 + csL], in_=bx3, func=AF.Copy), [bx.r], [xmh[fo // 4]])
                    if samp:
                        P.op("act", lambda e, bx=bx, fo=fo: e.activation(out=xmc.t[:, fo, :], in_=bx.t[:, 0:T], func=AF.Copy), [bx.r], [xmc.r])
                        P.op("dve", lambda e, bx3=bx3, fo=fo: e.tensor_copy(out=tails.t[:, fo, :, :], in_=bx3[:, :, 1:4]), [bx.r], [tails.r])
                    elif last_prompt:
                        P.op("dve", lambda e, bx=bx, fo=fo: e.tensor_copy(out=tailp.t[:, fo, :], in_=bx.t[:, T - 3:T]), [bx.r], [tailp.r])

            def B2(g):
                fos = range(4 * g, 4 * g + 4)
                for w in range(4):
                    for fo in fos:
                        at = acc[fo % 4]
                        av = at.t[:, 0:T].rearrange("p (s l) -> p s l", s=csn)
                        win = xmh.t[:, fo, :, w:w + csL]
                        if w == 0:
                            P.op("dve", lambda e, av=av, win=win, fo=fo: e.tensor_scalar(out=av, in0=win, scalar1=V(f"ml_wconv_{j}_0", fo), scalar2=V(f"ml_bconv_{j}", fo),
                                                                                         op0=ALU.mult, op1=ALU.add), [xmh[fo // 4], xmh.r, vecs.r], [at.r])
                        else:
                            P.op("dve", lambda e, av=av, win=win, fo=fo, w=w: e.scalar_tensor_tensor(out=av, in0=win, scalar=V(f"ml_wconv_{j}_{w}", fo), in1=av,
                                                                                                   op0=ALU.mult, op1=ALU.add), [xmh[fo // 4], xmh.r, at.r, vecs.r], [at.r])
                for fo in fos:
                    at = acc[fo % 4]
                    P.op("act", lambda e, at=at, fo=fo: e.activation(out=xc.t[:, fo, 0:T], in_=at.t[:, 0:T], func=AF.Silu), [at.r], [xc[fo // 4]])

            def B3(g):
                for fo in range(4 * g, 4 * g + 4):
                    bA, bK, bB = (banks[2], banks[3], banks[4]) if fo % 2 == 0 else (banks[5], banks[7], banks[1])
                    P.op("pe", lambda e, fo=fo, bA=bA: e.matmul(bA.t[:, 0:T], bd.t[:, 0, fo, :], xc.t[:, fo, 0:T], start=True, stop=True), [bd.r, xc[fo // 4]], [bA.r])
                    P.op("pe", lambda e, fo=fo, bK=bK: e.matmul(bK.t[:, 0:T], bd.t[:, 1, fo, :], xc.t[:, fo, 0:T], start=True, stop=True), [bd.r, xc[fo // 4]], [bK.r])
                    P.op("pe", lambda e, fo=fo, bB=bB: e.matmul(bB.t[:, 0:T], bd.t[:, 2, fo, :], xm_op(fo), start=True, stop=True),
                         [bd.r, xmh[fo // 4]] + ([xmc.r] if samp else []), [bB.r])
                    vt = vTt[fo % 2]
                    P.op("act", lambda e, fo=fo, bA=bA: e.activation(out=qT.t[:, fo, 0:T], in_=bA.t[:, 0:T], func=AF.Copy), [bA.r], [qT.r])
                    P.op("dve", lambda e, fo=fo, bK=bK: e.tensor_copy(out=kT.t[:, fo, 0:T], in_=bK.t[:, 0:T]), [bK.r], [kT.r])
                    P.op("act", lambda e, vt=vt, bB=bB: e.activation(out=vt.t[:, 0:T], in_=bB.t[:, 0:T], func=AF.Copy), [bB.r], [vt.r])
                    bg = banks[6]
                    for which, (src, sres) in enumerate([(qT.t[:, fo, 0:T], qT.r), (kT.t[:, fo, 0:T], kT.r), (vt.t[:, 0:T], vt.r)]):
                        first = (fo == 0 and which == 0)
                        lastm = (fo == 15 and which == 2)
                        P.op("pe", lambda e, src=src, which=which, fo=fo, first=first, lastm=lastm: e.matmul(bg.t[0:8, 0:T], wgate.t[:, which * 16 + fo, :], src, start=first, stop=lastm),
                             [wgate.r, sres], [bg.r])

            kstop = int(os.environ.get("KSTOP", "0"))
            if kstop == 11:
                return
            if samp:
                P.barrier_all()
            B1(0)
            B1(1)
            if kstop == 12:
                return
            B2(0)
            if kstop == 13:
                return
            B3(0)
            if kstop == 14:
                return
            B1(2)
            B2(1)
            B3(1)
            B1(3)
            B2(2)
            B3(2)
            B2(3)
            B3(3)
            P.op("act", lambda e: e.activation(out=gT_sb.t[:, 0:T], in_=banks[6].t[0:8, 0:T], func=AF.Copy), [banks[6].r], [gT_sb.r])
            kstop = int(os.environ.get("KSTOP", "0"))
            if kstop == 1:
                return
            if samp:
                P.barrier_all()

            bS = banks[4]

            def A2(tl, a=0, b=4):
                return tl.t[0:Pn, :, a:b]

            def trg(e):
                for c in range(nch):
                    ins = e.transpose(out=bS.t[0:Pn, c * 16:c * 16 + 8], in_=gT_sb.t[0:8, c * Pn:(c + 1) * Pn], identity=C("ident", 8, 8))
                return ins
            P.op("pe", trg, [gT_sb.r, consts.r], [bS.r])
            P.op("dve", lambda e: e.tensor_tensor(out=A2(gg, 0, 8), in0=bS.t[0:Pn, 0:nch * 16].rearrange("p (c g) -> p c g", c=nch)[:, :, 0:8],
                                                   in1=bgate.t[0:Pn, 0:8].unsqueeze(1).to_broadcast([Pn, nch, 8]), op=ALU.add), [bS.r, bgate.r], [gg.r])
            P.op("act", lambda e: e.activation(out=A2(e1), in_=A2(gg, 4, 8), func=AF.Exp, scale=-1.0), [gg.r], [e1.r])
            P.op("act", lambda e: e.activation(out=A2(lf), in_=A2(e1), func=AF.Ln, bias=1.0), [e1.r], [lf.r])
            P.op("dve", lambda e: e.tensor_scalar(out=A2(lf), in0=A2(lf), scalar1=-1.0, scalar2=None, op0=ALU.mult), [lf.r], [lf.r])

            def mmcs(e):
                lfv = lf.t[0:Pn, :, :].rearrange("p c h -> p (c h)")
                e.matmul(bS.t[0:Pn, 32:32 + nch * 4], mask, lfv, start=True, stop=True)
                return e.matmul(bS.t[0:Pn, 48:48 + nch * 4], onesblk, lfv, start=True, stop=True)
            P.op("pe", mmcs, [consts.r, lf.r], [bS.r])
            P.op("dve", lambda e: e.tensor_copy(out=A2(bt), in_=bS.t[0:Pn, 32:32 + nch * 4].rearrange("p (c h) -> p c h", c=nch)), [bS.r], [bt.r])
            P.op("dve", lambda e: e.tensor_copy(out=A2(bl), in_=bS.t[0:Pn, 48:48 + nch * 4].rearrange("p (c h) -> p c h", c=nch)), [bS.r], [bl.r])
            P.op("dve", lambda e: e.tensor_tensor(out=A2(aa), in0=A2(gg, 0, 4), in1=A2(bt), op=ALU.subtract), [gg.r, bt.r], [aa.r])

            def tra(e):
                for c in range(nch):
                    ins = e.transpose(out=bS.t[0:4, 64 + c * Pn:64 + (c + 1) * Pn], in_=aa.t[0:Pn, c, 0:4], identity=C("ident", Pn, Pn))
                return ins
            P.op("pe", tra, [aa.r, consts.r], [bS.r])
            P.op("dve", lambda e: e.tensor_reduce(out=amaxT.t[0:4, :], in_=bS.t[0:4, 64:64 + nch * Pn].rearrange("p (s l) -> p s l", l=L), axis=AX.X, op=ALU.max),
                 [bS.r], [amaxT.r])
            P.op("dve", lambda e: e.tensor_copy(out=amax_exp.t[0:4, :].rearrange("p (s l) -> p s l", l=L),
                                                 in_=amaxT.t[0:4, :].unsqueeze(2).to_broadcast([4, nch * nseq, L])), [amaxT.r], [amax_exp.r])

            def mmbc(e):
                for c in range(nch):
                    ins = e.matmul(bS.t[0:Pn, 384 + c * 16:388 + c * 16], amax_exp.t[0:4, c * Pn:(c + 1) * Pn], C("ident", 4, 4), start=True, stop=True)
                return ins
            P.op("pe", mmbc, [amax_exp.r, consts.r], [bS.r])
            for c in range(nch):
                P.op("dve", lambda e, c=c: e.tensor_tensor(out=RR.t[0:Pn, c, :], in0=bS.t[0:Pn, 384 + c * 16:388 + c * 16], in1=mcur.t[0:Pn, c, :], op=ALU.max), [bS.r, mcur.r], [RR.r])
                P.op("dve", lambda e, c=c: e.tensor_tensor(out=mcur.t[0:Pn, c + 1, :], in0=bl.t[0:Pn, c, :], in1=RR.t[0:Pn, c, :], op=ALU.add), [bl.r, RR.r], [mcur.r])
            P.op("dve", lambda e: e.tensor_tensor(out=A2(d1), in0=A2(aa), in1=A2(RR), op=ALU.subtract), [aa.r, RR.r], [d1.r])
            P.op("act", lambda e: e.activation(out=A2(es), in_=A2(d1), func=AF.Exp, bias=V("c_lnscale", 0, Pn)), [d1.r, vecs.r], [es.r])
            P.op("dve", lambda e: e.tensor_tensor(out=A2(d2), in0=mcur.t[0:Pn, 0:nch, :], in1=A2(RR), op=ALU.subtract), [mcur.r, RR.r], [d2.r])
            P.op("act", lambda e: e.activation(out=A2(c0), in_=A2(d2), func=AF.Exp), [d2.r], [c0.r])
            P.op("dve", lambda e: e.tensor_tensor(out=A2(d3), in0=A2(bt), in1=A2(RR), op=ALU.add), [bt.r, RR.r], [d3.r])
            P.op("act", lambda e: e.activation(out=A2(dfl), in_=A2(d3), func=AF.Exp, scale=-1.0), [d3.r], [dfl.r])
            for c in range(nch):
                P.op("dve", lambda e, c=c: e.tensor_tensor(out=rhsx.t[0:Pn, c * nseq * 4:(c + 1) * nseq * 4].rearrange("p (s h) -> p s h", s=nseq),
                                                           in0=c0.t[0:Pn, c, :].unsqueeze(1).to_broadcast([Pn, nseq, 4]),
                                                           in1=firstm.unsqueeze(2).to_broadcast([Pn, nseq, 4]), op=ALU.mult), [c0.r, consts.r], [rhsx.r])
            P.op("pe", lambda e: e.matmul(bS.t[:, 448:448 + NS4], C("onesP", Pn, 128), rhsx.t[0:Pn, 0:NS4], start=True, stop=True), [rhsx.r, consts.r], [bS.r])
            P.op("dve", lambda e: e.tensor_copy(out=c0bc.t[:].rearrange("p c s h -> p (c s h)"), in_=bS.t[:, 448:448 + NS4]), [bS.r], [c0bc.r])

            if kstop == 2:
                return
            if samp:
                P.barrier_all()
            bV2 = banks[4]
            bSc = banks[2]
            bNs = [banks[3], banks[6], banks[5], banks[1]]
            bcus = [banks[0], banks[7]]
            for c in range(nch):
                cs = slice(c * Pn, (c + 1) * Pn)

                def ES(hh, c=c):
                    return es.t[0:Pn, c, hh:hh + 1]

                def F(hh, c=c, cs=cs):
                    v_tok, kp_tok, stt = v_tk[hh % 2], kp_tk[hh % 2], ST[hh]

                    def mmv(e):
                        for fl in range(4):
                            fo = hh * 4 + fl
                            ins = e.matmul(bV2.t[0:Pn, fl * 128:(fl + 1) * 128], xm_op(fo, cs.start, cs.stop), bd.t[:, 2, fo, :], start=True, stop=True)
                        return ins
                    P.op("pe", mmv, [xmh[hh], bd.r] + ([xmc.r] if samp else []), [bV2.r])
                    P.op("act", lambda e: e.activation(out=v_tok.t[0:Pn, :], in_=bV2.t[0:Pn, :], func=AF.Copy), [bV2.r], [v_tok.r])

                    def mmk(e):
                        for fl in range(4):
                            fo = hh * 4 + fl
                            ins = e.matmul(bV2.t[0:Pn, fl * 128:(fl + 1) * 128], xc.t[:, fo, cs], bd.t[:, 1, fo, :], start=True, stop=True)
                        return ins
                    P.op("pe", mmk, [xc[hh], bd.r], [bV2.r])
                    P.op("act", lambda e: e.activation(out=kp_tok.t[0:Pn, :], in_=bV2.t[0:Pn, :], func=AF.Copy, scale=ES(hh)), [bV2.r, es.r], [kp_tok.r])

                    def mms(e):
                        for dc in range(4):
                            ins = e.matmul(bSc.t[0:Pn, 0:Pn], kT.t[:, hh * 4 + dc, cs], qT.t[:, hh * 4 + dc, cs], start=(dc == 0), stop=(dc == 3))
                        return ins
                    P.op("pe", mms, [kT.r, qT.r], [bSc.r])
                    P.op("dve", lambda e: e.scalar_tensor_tensor(out=stt.t[0:Pn, 0:Pn], in0=bSc.t[0:Pn, 0:Pn], scalar=ES(hh), in1=mask, op0=ALU.mult, op1=ALU.mult),
                         [bSc.r, es.r, consts.r], [stt.r])
                    P.op("dve", lambda e: e.tensor_tensor(out=nbf.t[:, hh * 4:(hh + 1) * 4, :], in0=nst.t[:, hh * 4:(hh + 1) * 4, :],
                                                          in1=c0bc.t[:, c, :, hh].unsqueeze(1).to_broadcast([128, 4, nseq]), op=ALU.mult), [nst[hh], c0bc.r], [nbf[hh]])
                    if not samp:
                        cb = Cbf[hh % 2]
                        P.op("act", lambda e: e.activation(out=cb.t[:], in_=Cst.t[:, hh * 4:(hh + 1) * 4, :], func=AF.Copy, scale=c0bc.t[:, c, 0, hh:hh + 1]),
                             [Cst[hh], c0bc.r], [cb.r])

                def NQ(hh, cs=cs):
                    stt = ST[hh]

                    def mmq(e):
                        e.matmul(bSc.t[0:Pn, 128 + hh:129 + hh], stt.t[0:Pn, 0:Pn], onesb[0:Pn, 0:1], start=True, stop=True)
                        for dc in range(4):
                            ins = e.matmul(bSc.t[0:Pn, 160 + hh * 16:160 + hh * 16 + nseq], qT.t[:, hh * 4 + dc, cs], nbf.t[:, hh * 4 + dc, :], start=(dc == 0), stop=(dc == 3))
                        return ins
                    P.op("pe", mmq, [stt.r, cbf.r, qT.r, nbf[hh]], [bSc.r])

                def NU_prompt(hh, c=c, cs=cs):
                    v_tok, kp_tok, stt, cb, bN = v_tk[hh % 2], kp_tk[hh % 2], ST[hh], Cbf[hh % 2], bNs[hh]

                    def mmn(e):
                        e.matmul(bN.t[0:Pn, :], stt.t[0:Pn, 0:Pn], v_tok.t[0:Pn, :], start=True, stop=False)
                        for dc in range(4):
                            ins = e.matmul(bN.t[0:Pn, :], qT.t[:, hh * 4 + dc, cs], cb.t[:, dc, :], start=False, stop=(dc == 3))
                        return ins
                    P.op("pe", mmn, [stt.r, v_tok.r, qT.r, cb.r], [bN.r])
                    NQ(hh)
                    for dc in range(4):
                        bcu = bcus[cnt[0] % 2]
                        cnt[0] += 1
                        P.op("pe", lambda e, bcu=bcu, dc=dc: e.matmul(bcu.t[:, :], kp_tok.t[0:Pn, dc * 128:(dc + 1) * 128], v_tok.t[0:Pn, :], start=True, stop=True),
                             [kp_tok.r, v_tok.r], [bcu.r])
                        P.op("dve", lambda e, bcu=bcu, dc=dc: e.scalar_tensor_tensor(out=Cst.t[:, hh * 4 + dc, :], in0=Cst.t[:, hh * 4 + dc, :], scalar=c0bc.t[:, c, 0, hh:hh + 1],
                                                                                   in1=bcu.t[:, :], op0=ALU.mult, op1=ALU.add), [bcu.r, Cst[hh], c0bc.r], [Cst[hh]])

                def NU_samp(hh, c=c, cs=cs):
                    v_tok, kp_tok, stt, bN = v_tk[hh % 2], kp_tk[hh % 2], ST[hh], bNs[hh]
                    P.op("pe", lambda e: e.matmul(bN.t[0:Pn, :], stt.t[0:Pn, 0:Pn], v_tok.t[0:Pn, :], start=True, stop=False), [stt.r, v_tok.r], [bN.r])
                    for b in range(NB):
                        idx = hh * NB + b
                        ensure_loaded(idx + len(Cslots) - 2)
                        sl = Cslots[idx % len(Cslots)]
                        cb = Cbf[b % 2]
                        P.op("act", lambda e, cb=cb, sl=sl, b=b: e.activation(out=cb.t[:], in_=sl.t[:], func=AF.Copy, scale=c0bc.t[:, c, b, hh:hh + 1]), [sl.r, c0bc.r], [cb.r])
                        qmt = qm[b % 2]
                        P.op("dve", lambda e, qmt=qmt, b=b: e.tensor_tensor(out=qmt.t[:], in0=qT.t[:, hh * 4:(hh + 1) * 4, 0:TS],
                                                                            in1=colmask.t[:, b, :].unsqueeze(1).to_broadcast([128, 4, TS]), op=ALU.mult), [qT.r, colmask.r], [qmt.r])

                        def mmc(e, qmt=qmt, cb=cb, b=b):
                            for dc in range(4):
                                ins = e.matmul(bN.t[0:Pn, :], qmt.t[:, dc, :], cb.t[:, dc, :], start=False, stop=(b == NB - 1 and dc == 3))
                            return ins
                        P.op("pe", mmc, [qmt.r, cb.r], [bN.r])
                        kpt = kpm[b % 2]
                        P.op("dve", lambda e, kpt=kpt, b=b: e.tensor_scalar(out=kpt.t[0:Pn, :], in0=kp_tok.t[0:Pn, :], scalar1=consts.t[0:Pn, CL["seqS"] + b:CL["seqS"] + b + 1],
                                                                            scalar2=None, op0=ALU.mult), [kp_tok.r, consts.r], [kpt.r])
                        for dc in range(4):
                            bcu = bcus[cnt[0] % 2]
                            cnt[0] += 1
                            P.op("pe", lambda e, bcu=bcu, kpt=kpt, dc=dc: e.matmul(bcu.t[:, :], kpt.t[0:Pn, dc * 128:(dc + 1) * 128], v_tok.t[0:Pn, :], start=True, stop=True),
                                 [kpt.r, v_tok.r], [bcu.r])
                            P.op("dve", lambda e, bcu=bcu, dc=dc, b=b, sl=sl: e.scalar_tensor_tensor(out=sl.t[:, dc, :], in0=sl.t[:, dc, :], scalar=c0bc.t[:, c, b, hh:hh + 1],
                                                                                                   in1=bcu.t[:, :], op0=ALU.mult, op1=ALU.add), [bcu.r, sl.r, c0bc.r], [sl.r])
                        P.dma("sp", Cs_o[j, b, hh].rearrange("(dc p) e -> p dc e", p=128), sl.t[:], reads=[sl.r])
                    NQ(hh)

                def NUPD(hh, c=c):
                    kp_tok = kp_tk[hh % 2]

                    def mmnn(e):
                        for dc in range(4):
                            ins = e.matmul(bSc.t[:, 256 + hh * 64 + dc * 16:256 + hh * 64 + dc * 16 + nseq], kp_tok.t[0:Pn, dc * 128:(dc + 1) * 128], seqmb, start=True, stop=True)
                        return ins
                    P.op("pe", mmnn, [kp_tok.r, cbf.r], [bSc.r])
                    P.op("dve", lambda e: e.tensor_tensor(out=tmpn2.t[:], in0=nst.t[:, hh * 4:(hh + 1) * 4, :],
                                                          in1=c0bc.t[:, c, :, hh].unsqueeze(1).to_broadcast([128, 4, nseq]), op=ALU.mult), [nst[hh], c0bc.r], [tmpn2.r])
                    P.op("dve", lambda e: e.tensor_tensor(out=nst.t[:, hh * 4:(hh + 1) * 4, :], in0=tmpn2.t[:],
                                                          in1=bSc.t[:, 256 + hh * 64:256 + hh * 64 + 64].rearrange("p (d s) -> p d s", d=4)[:, :, 0:nseq], op=ALU.add),
                         [tmpn2.r, bSc.r], [nst[hh]])

                ld_issued = [0]

                def ensure_loaded(upto):
                    while ld_issued[0] <= min(upto, 4 * NB - 1):
                        k = ld_issued[0]
                        P.dma("sp", Cslots[k % len(Cslots)].t[:], sC_d[j, k % NB, k // NB].rearrange("(dc p) e -> p dc e", p=128), writes=[Cslots[k % len(Cslots)].r])
                        ld_issued[0] += 1

                NU = NU_samp if samp else NU_prompt
                F(0)
                F(1)
                NU(0)
                NUPD(0)
                F(2)
                NU(1)
                NUPD(1)
                F(3)
                NU(2)
                NUPD(2)
                NU(3)
                NUPD(3)
                P.op("dve", lambda e: e.tensor_tensor(out=tmpn.t[0:Pn], in0=bSc.t[0:Pn, 160:224].rearrange("p (h s) -> p h s", h=4)[:, :, 0:nseq],
                                                      in1=seqm.unsqueeze(1).to_broadcast([Pn, 4, nseq]), op=ALU.mult), [bSc.r, consts.r], [tmpn.r])
                P.op("dve", lambda e: e.tensor_reduce(out=nqb.t[0:Pn, :], in_=tmpn.t[0:Pn], axis=AX.X, op=ALU.add), [tmpn.r], [nqb.r])
                P.op("dve", lambda e: e.tensor_tensor(out=nq.t[0:Pn, 0, :], in0=nqb.t[0:Pn, :], in1=bSc.t[0:Pn, 128:132], op=ALU.add), [nqb.r, bSc.r], [nq.r])
                P.op("dve", lambda e: e.tensor_scalar(out=nq.t[0:Pn, 1, :], in0=nq.t[0:Pn, 0, :], scalar1=-1.0, scalar2=None, op0=ALU.mult), [nq.r], [nq.r])
                P.op("dve", lambda e: e.tensor_tensor(out=nq.t[0:Pn, 2, :], in0=nq.t[0:Pn, 0, :], in1=nq.t[0:Pn, 1, :], op=ALU.max), [nq.r], [nq.r])
                P.op("dve", lambda e, c=c: e.tensor_tensor(out=den.t[0:Pn, :], in0=nq.t[0:Pn, 2, :], in1=dfl.t[0:Pn, c, :], op=ALU.max), [nq.r, dfl.r], [den.r])
                for hh in range(4):
                    P.op("dve", lambda e, hh=hh: e.bn_stats(out=st6[hh].t[0:Pn, :], in_=bNs[hh].t[0:Pn, :]), [bNs[hh].r], [st6[hh].r])
                for hh in range(4):
                    P.op("dve", lambda e, hh=hh: e.bn_aggr(out=mv.t[0:Pn, hh, :], in_=st6[hh].t[0:Pn, :]), [st6[hh].r], [mv.r])
                P.op("dve", lambda e: e.tensor_tensor(out=t1.t[0:Pn, :], in0=den.t[0:Pn, :], in1=den.t[0:Pn, :], op=ALU.mult), [den.r], [t1.r])
                P.op("dve", lambda e: e.scalar_tensor_tensor(out=t1.t[0:Pn, :], in0=t1.t[0:Pn, :], scalar=EPS, in1=mv.t[0:Pn, :, 1], op0=ALU.mult, op1=ALU.add), [t1.r, mv.r], [t1.r])
                P.op("act", lambda e: e.activation(out=rs.t[0:Pn, :], in_=t1.t[0:Pn, :], func=AF.Sqrt), [t1.r], [rs.r])
                P.op("dve", lambda e: e.reciprocal(out=rs.t[0:Pn, :], in_=rs.t[0:Pn, :]), [rs.r], [rs.r])
                P.op("dve", lambda e: e.scalar_tensor_tensor(out=nb.t[0:Pn, :], in0=mv.t[0:Pn, :, 0], scalar=-1.0, in1=rs.t[0:Pn, :], op0=ALU.mult, op1=ALU.mult), [mv.r, rs.r], [nb.r])
                for hh in range(4):
                    P.op("act", lambda e, hh=hh, c=c: e.activation(out=hn_tok.t[0:Pn, c, hh * 512:(hh + 1) * 512], in_=bNs[hh].t[0:Pn, :], func=AF.Identity,
                                                                   scale=rs.t[0:Pn, hh:hh + 1], bias=nb.t[0:Pn, hh:hh + 1]), [bNs[hh].r, rs.r, nb.r], [hn_tok.r])

            if kstop == 3:
                return
            pend = []
            for fo in range(16):
                if fo % 2 == 0:
                    wt = next_wA()
                    P.dma("pool", wt.t[:], wup_d[j][8 + fo // 2], writes=[wt.r])
                fc = fo % 2
                bx = banks[fo % 2]
                bT = banks[2 + 3 * (fo % 2)]
                bTb = bT.t[:].bitcast(BF16)

                def mm(e, wt=wt, bx=bx, fc=fc):
                    for k in range(8):
                        ins = e.matmul(bx.t[:, 0:T], wt.t[:, k, fc * 128:(fc + 1) * 128], xn.t[:, k, 0:T], start=(k == 0), stop=(k == 7))
                    return ins
                P.op("pe", mm, [wt.r, xn.r], [bx.r])
                szt = sz[fo % 2]
                eat = ea[fo % 2]
                P.op("act", lambda e, bx=bx, szt=szt: e.activation(out=szt.t[:, 0:T], in_=bx.t[:, 0:T], func=AF.Silu), [bx.r], [szt.r])

                def mmt(e, fo=fo, bTb=bTb):
                    for c in range(nch):
                        ins = e.transpose(out=bTb[:, c * Pn:(c + 1) * Pn], in_=hn_tok.t[0:Pn, c, fo * 128:(fo + 1) * 128], identity=identb[0:Pn, 0:Pn])
                    return ins
                P.op("pe", mmt, [hn_tok.r, cbf.r], [bT.r])
                P.op("act", lambda e, eat=eat, fo=fo, bTb=bTb: e.activation(out=eat.t[:, 0:T], in_=bTb[:, 0:T], func=AF.Copy, scale=V(f"ml_hn_g_{j}", fo)), [bT.r, vecs.r], [eat.r])
                P.op("dve", lambda e, eat=eat, fo=fo: e.scalar_tensor_tensor(out=eat.t[:, 0:T], in0=xc.t[:, fo, 0:T], scalar=V(f"ml_skip_{j}", fo), in1=eat.t[:, 0:T],
                                                                           op0=ALU.mult, op1=ALU.add), [xc[fo // 4], eat.r, vecs.r], [eat.r])
                pend.append((eat, szt, fo))
                if len(pend) == 2:
                    eat_, szt_, fo_ = pend.pop(0)
                    P.op("dve", lambda e, eat_=eat_, szt_=szt_, fo_=fo_: e.tensor_tensor(out=oT.t[:, fo_, 0:T], in0=eat_.t[:, 0:T], in1=szt_.t[:, 0:T], op=ALU.mult), [eat_.r, szt_.r], [oT.r])
            eat_, szt_, fo_ = pend.pop(0)
            P.op("dve", lambda e, eat_=eat_, szt_=szt_, fo_=fo_: e.tensor_tensor(out=oT.t[:, fo_, 0:T], in0=eat_.t[:, 0:T], in1=szt_.t[:, 0:T], op=ALU.mult), [eat_.r, szt_.r], [oT.r])
            xr = xres(t0, T)
            for d in range(8):
                wdt = next_wB()
                wv = wdt.t[:, :].rearrange("p (f c) -> p f c", f=16)
                P.dma("pool", wv, wdn_d[j][d], writes=[wdt.r])
                bk = banks[3 + 3 * (d % 2)]

                def mm(e, wv=wv, bk=bk):
                    for f in range(16):
                        ins = e.matmul(bk.t[:, 0:T], wv[:, f, :], oT.t[:, f, 0:T], start=(f == 0), stop=(f == 15))
                    return ins
                P.op("pe", mm, [wdt.r, oT.r], [bk.r])
                P.op("dve", lambda e, bk=bk, d=d: e.tensor_tensor(out=x.t[:, d, t0:t0 + T], in0=bk.t[:, 0:T], in1=x.t[:, d, t0:t0 + T], op=ALU.add), [bk.r] + xr, xr)
            if not samp:
                P.op("pool", lambda e: e.tensor_copy(out=xmh.t[:, :, 0, 0:3], in_=xmh.t[:, :, 0, T:T + 3]), [xmh.r] + [xmh[g] for g in range(4)], [xmh.r] + [xmh[g] for g in range(4)])
            if last_prompt:
                P.dma("sp", Cp_o[j].rearrange("h (dc p) e -> p (h dc) e", p=128), Cst.t[:], reads=[Cst[h_] for h_ in range(4)])
                P.dma("sp", npT_o[j], nst.t[:], reads=[nst[h_] for h_ in range(4)])
                P.dma("sp", mp_o[j], mcur.t[:, nch, :], reads=[mcur.r])
                P.dma("sp", convp_o[j], tailp.t[:], reads=[tailp.r])
            if samp:
                P.dma("sp", nsT_o[j], nst.t[:], reads=[nst[h_] for h_ in range(4)])
                P.dma("sp", ms_o[j], mcur.t[0:TS, nch, :], reads=[mcur.r])
                P.dma("sp", convs_o[j], tails.t[:], reads=[tails.r])

    def mlstm_v1_sample(i, j):
        P.phase()
        gname = f"norm_mix_{i}"
        T = 128
        NSL = 5
        bd = P.atile([128, 3, 16, 128], BF16)
        colmask = P.atile([128, NB, TS], BF16)
        P.dma("pool", colmask.t[:], colmask_d, writes=[colmask.r])
        wgate = P.atile([128, 48, 8], BF16)
        bgate = P.atile([128, 8], F32)
        xn = P.atile([128, 8, 256], BF16)
        xmh = P.atile([128, 16, 3 + T], BF16)
        tailp = P.atile([128, 16, 3], F32)
        xc = P.atile([128, 16, T], BF16)
        acc = [P.atile([128, T], F32) for _ in range(2)]
        qT = P.atile([128, 16, T], BF16)
        kT = P.atile([128, 16, T], BF16)
        sqb = Tile(kT.t[:, 0:8, :], kT.base_lw)
        sqb._res = kT._res
        vTt = [P.atile([128, T], BF16) for _ in range(2)]
        v_tk = [P.atile([128, 512], BF16) for _ in range(2)]
        kp_tk = [P.atile([128, 512], BF16) for _ in range(2)]
        Cst = P.atile([128, 4 * NSL, 512], F32)
        Cbf = [P.atile([128, 4, 512], BF16) for _ in range(2)]
        nst = P.atile([128, 16, NB], F32)
        nbf = P.atile([128, 16, NB], BF16)
        ST = [P.atile([128, 128], BF16) for _ in range(2)]
        hn_tok = P.atile([128, 2, 2048], BF16)
        tails = Tile(hn_tok.t[:, 1, 512:2048].bitcast(F32).rearrange("p (f s w) -> p f s w", f=16, s=NB), hn_tok.base_lw)
        tails._res = hn_tok._res
        sz = acc
        ea = [P.atile([128, T], F32)]
        rst = ea[0]
        gT_sb = P.atile([8, T], F32)
        sm = {nm: P.atile([128, 8], F32) for nm in ["g", "e1", "lf", "btbl", "a", "R", "d1", "es", "d2", "c0", "d3", "dfl", "m0a", "m0b",
                                                    "nq", "den", "mv", "t1", "rs", "nb", "nqb"]}
        st6 = P.atile([128, 6], F32)
        amaxT = P.atile([4, NB], F32)
        amax_exp = P.atile([4, 128], F32)
        rhsx = P.atile([128, NB, 4], F32)
        c0bc = P.atile([128, NB, 4], F32)
        tmpn = P.atile([128, NB], F32)
        tmpn2 = P.atile([128, 4, NB], F32)
        qm = [P.atile([128, 4, TS], BF16) for _ in range(2)]
        kpm = [P.atile([64, 512], BF16) for _ in range(2)]
        oT = qT
        xmh_s = Tile(xmh.t[:, :, 0:NB * 7].rearrange("p f (s l) -> p f s l", s=NB), xmh.base_lw)
        xmh_s._res = xmh._res

        P.dma("pool", bd.t[:], bd_d[j], writes=[bd.r])
        P.dma("pool", wgate.t[:], wgate_d[j], writes=[wgate.r])
        P.dma("sp", bgate.t[:], bgate_d[j], writes=[bgate.r])
        P.op("pool", lambda e: e.memset(Cst.t[:], 0.0), [], [Cst[s_] for s_ in range(NSL)])
        P.op("pool", lambda e: e.memset(nst.t[:], 0.0), [], [nst.r])
        P.op("pool", lambda e: e.memset(sm["m0a"].t[:], 0.0), [], [sm["m0a"].r])
        P.op("pool", lambda e: e.memset(xmh.t[:, :, 0:3], 0.0), [], [xmh.r])

        def xmc_ap(fo, cs=slice(0, TS)):
            base = 64 + (fo % 2) * 64
            return xn.t[:, fo // 2, base + cs.start:base + cs.stop]
        m0 = [sm["m0a"], sm["m0b"]]
        m0_i = [0]
        lnscale = math.log(DH ** -0.5)

        tiles = [(TP, TS, NB, 4, TS, 1)]
        ld_issued = [0]
        cnt = [0]
        for tidx, (t0, Tn, nseq, L, Pn, nch) in enumerate(tiles):
            samp = nseq > 1
            last_prompt = (t0 + Tn == TP)
            if samp:
                P.dma("sp", nst.t[:], snT_d[j], writes=[nst.r])
                P.dma("sp", m0[m0_i[0]].t[0:TS, 0:4], sm_d[j], writes=[m0[m0_i[0]].r])
                P.dma("sp", tails.t[:], sconv_d[j], writes=[tails.r])
                P.op("dve", lambda e: e.tensor_copy(out=xmh_s.t[:, :, :, 0:3], in_=tails.t[:]), [tails.r], [xmh.r])
            rmsnorm(t0, Tn, gname, xn, xn.r, sqb, rst, banks[6])
            for blk in range(8):
                wt = next_wA()
                P.dma("pool", wt.t[:], wup_d[j][blk], writes=[wt.r])
                for fc in range(2):
                    fo = blk * 2 + fc
                    bx = banks[cnt[0] % 2]
                    cnt[0] += 1

                    def mm(e, wt=wt, bx=bx, fc=fc):
                        for k in range(8):
                            ins = e.matmul(bx.t[:, 0:Tn], wt.t[:, k, fc * 128:(fc + 1) * 128], xn.t[:, k, 0:Tn], start=(k == 0), stop=(k == 7))
                        return ins
                    P.op("pe", mm, [wt.r, xn.r], [bx.r])
                    if not samp:
                        P.op("act", lambda e, bx=bx, fo=fo: e.activation(out=xmh.t[:, fo, 3:3 + Tn], in_=bx.t[:, 0:Tn], func=AF.Copy), [bx.r], [xmh.r])
                        if last_prompt:
                            P.op("dve", lambda e, bx=bx, fo=fo: e.tensor_copy(out=tailp.t[:, fo, :], in_=bx.t[:, Tn - 3:Tn]), [bx.r], [tailp.r])
                        xm_op = xmh.t[:, fo, 3:3 + Tn]
                        wins = [xmh.t[:, fo, w:w + Tn] for w in range(4)]
                        accv = [a_.t[:, 0:Tn] for a_ in acc]
                    else:
                        bx3 = bx.t[:, 0:Tn].rearrange("p (s l) -> p s l", s=NB)
                        P.op("act", lambda e, bx3=bx3, fo=fo: e.activation(out=xmh_s.t[:, fo, :, 3:7], in_=bx3, func=AF.Copy), [bx.r], [xmh.r])
                        P.op("act", lambda e, bx=bx, fo=fo: e.activation(out=xmc_ap(fo), in_=bx.t[:, 0:Tn], func=AF.Copy), [bx.r], [xn.r])
                        P.op("dve", lambda e, bx3=bx3, fo=fo: e.tensor_copy(out=tails.t[:, fo, :, :], in_=bx3[:, :, 1:4]), [bx.r], [tails.r])
                        xm_op = xmc_ap(fo)
                        wins = [xmh_s.t[:, fo, :, w:w + 4] for w in range(4)]
                        accv = [a_.t[:, 0:Tn].rearrange("p (s l) -> p s l", s=NB) for a_ in acc]
                    at = acc[fo % 2]
                    av = accv[fo % 2]
                    P.op("dve", lambda e, av=av, wins=wins, fo=fo: e.tensor_scalar(out=av, in0=wins[0], scalar1=V(f"ml_wconv_{j}_0", fo), scalar2=V(f"ml_bconv_{j}", fo),
                                                                                   op0=ALU.mult, op1=ALU.add), [xmh.r, vecs.r], [at.r])
                    for w in range(1, 4):
                        P.op("dve", lambda e, av=av, wins=wins, fo=fo, w=w: e.scalar_tensor_tensor(out=av, in0=wins[w], scalar=V(f"ml_wconv_{j}_{w}", fo), in1=av,
                                                                                                 op0=ALU.mult, op1=ALU.add), [xmh.r, at.r, vecs.r], [at.r])
                    P.op("act", lambda e, at=at, fo=fo: e.activation(out=xc.t[:, fo, 0:Tn], in_=at.t[:, 0:Tn], func=AF.Silu), [at.r], [xc.r])
                    bq, bk_, bv = banks[2], banks[3], banks[4]
                    P.op("pe", lambda e, fo=fo: e.matmul(bq.t[:, 0:Tn], bd.t[:, 0, fo, :], xc.t[:, fo, 0:Tn], start=True, stop=True), [bd.r, xc.r], [bq.r])
                    P.op("pe", lambda e, fo=fo: e.matmul(bk_.t[:, 0:Tn], bd.t[:, 1, fo, :], xc.t[:, fo, 0:Tn], start=True, stop=True), [bd.r, xc.r], [bk_.r])
                    P.op("pe", lambda e, fo=fo, xm_op=xm_op: e.matmul(bv.t[:, 0:Tn], bd.t[:, 2, fo, :], xm_op, start=True, stop=True), [bd.r, xmh.r, xn.r], [bv.r])
                    vt = vTt[fo % 2]
                    P.op("act", lambda e, fo=fo: e.activation(out=qT.t[:, fo, 0:Tn], in_=bq.t[:, 0:Tn], func=AF.Copy), [bq.r], [qT.r])
                    P.op("dve", lambda e, fo=fo: e.tensor_copy(out=kT.t[:, fo, 0:Tn], in_=bk_.t[:, 0:Tn]), [bk_.r], [kT.r])
                    P.op("act", lambda e, vt=vt: e.activation(out=vt.t[:, 0:Tn], in_=bv.t[:, 0:Tn], func=AF.Copy), [bv.r], [vt.r])
                    bg = banks[5]
                    for which, (src, sres) in enumerate([(qT.t[:, fo, 0:Tn], qT.r), (kT.t[:, fo, 0:Tn], kT.r), (vt.t[:, 0:Tn], vt.r)]):
                        first = (fo == 0 and which == 0)
                        lastm = (fo == 15 and which == 2)
                        P.op("pe", lambda e, src=src, which=which, fo=fo, first=first, lastm=lastm: e.matmul(bg.t[0:8, 0:Tn], wgate.t[:, which * 16 + fo, :], src, start=first, stop=lastm),
                             [wgate.r, sres], [bg.r])
            P.op("act", lambda e: e.activation(out=gT_sb.t[:, 0:Tn], in_=banks[5].t[0:8, 0:Tn], func=AF.Copy), [banks[5].r], [gT_sb.r])
            if dbg == "ml0" and tidx == int(os.environ.get("KDBG_T", "0")) and j == 0:
                do_ = dout("dbg_x", [128, 8, T])
                P.op("dve", lambda e: e.tensor_copy(out=dbg_stage.t[:, 0:8, :], in_=x.t[:, :, t0:t0 + T]), xres(t0, T), [dbg_stage.r])
                P.dma("sp", do_, dbg_stage.t[:], reads=[dbg_stage.r])
                for nm_, src_, n3 in [("dbg_xn", xn, 8), ("dbg_xm", None, 16), ("dbg_xc", xc, 16), ("dbg_q", qT, 16)]:
                    do_ = dout(nm_, [128, n3, T])
                    stg = dbg_stage
                    for h0 in range(0, n3, 8):
                        if src_ is None:
                            P.op("dve", lambda e, stg=stg, h0=h0: e.tensor_copy(out=stg.t[:, 0:8, :], in_=xmh.t[:, h0:h0 + 8, 3:3 + T]), [xmh.r], [stg.r])
                        else:
                            P.op("dve", lambda e, stg=stg, src_=src_, h0=h0: e.tensor_copy(out=stg.t[:, 0:8, :], in_=src_.t[:, h0:h0 + 8, 0:T]), [src_.r], [stg.r])
                        P.dma("sp", do_[:, h0:h0 + 8, :], stg.t[:, 0:8, :], reads=[stg.r])

            for c in range(nch):
                cs = slice(c * Pn, (c + 1) * Pn)
                bS = banks[7]
                mask = C("maskS" if samp else "maskP", Pn, Pn)
                onesblk = C("onesS" if samp else "onesP", Pn, Pn)
                seqm = C("seqS" if samp else "seqP", Pn, nseq)
                firstm = C("firstS" if samp else "firstP", Pn, nseq)
                seqmb = cbf.t[0:Pn, 256:256 + nseq] if samp else cbf.t[0:Pn, 272:273]
                m0c = m0[m0_i[0]]
                m0n = m0[1 - m0_i[0]]
                m0_i[0] = 1 - m0_i[0]
                s = sm

                def S(nm, a=0, b=4):
                    return s[nm].t[0:Pn, a:b]
                P.op("pe", lambda e, cs=cs: e.transpose(out=bS.t[0:Pn, 0:8], in_=gT_sb.t[0:8, cs], identity=C("ident", 8, 8)), [gT_sb.r, consts.r], [bS.r])
                P.op("dve", lambda e: e.tensor_tensor(out=S("g", 0, 8), in0=bS.t[0:Pn, 0:8], in1=bgate.t[0:Pn, 0:8], op=ALU.add), [bS.r, bgate.r], [s["g"].r])
                P.op("act", lambda e: e.activation(out=S("e1"), in_=S("g", 4, 8), func=AF.Exp, scale=-1.0), [s["g"].r], [s["e1"].r])
                P.op("act", lambda e: e.activation(out=S("lf"), in_=S("e1"), func=AF.Ln, bias=1.0), [s["e1"].r], [s["lf"].r])
                P.op("dve", lambda e: e.tensor_scalar(out=S("lf"), in0=S("lf"), scalar1=-1.0, scalar2=None, op0=ALU.mult), [s["lf"].r], [s["lf"].r])

                def mm(e, mask=mask, onesblk=onesblk):
                    e.matmul(bS.t[0:Pn, 8:12], mask, S("lf"), start=True, stop=True)
                    return e.matmul(bS.t[0:Pn, 12:16], onesblk, S("lf"), start=True, stop=True)
                P.op("pe", mm, [consts.r, s["lf"].r], [bS.r])
                P.op("dve", lambda e: e.tensor_copy(out=S("btbl", 0, 8), in_=bS.t[0:Pn, 8:16]), [bS.r], [s["btbl"].r])
                P.op("dve", lambda e: e.tensor_tensor(out=S("a"), in0=S("g", 0, 4), in1=S("btbl", 0, 4), op=ALU.subtract), [s["g"].r, s["btbl"].r], [s["a"].r])
                P.op("pe", lambda e: e.transpose(out=bS.t[0:4, 16:16 + Pn], in_=S("a"), identity=C("ident", Pn, Pn)), [s["a"].r, consts.r], [bS.r])
                P.op("dve", lambda e: e.tensor_reduce(out=amaxT.t[0:4, 0:nseq], in_=bS.t[0:4, 16:16 + Pn].rearrange("p (s l) -> p s l", s=nseq), axis=AX.X, op=ALU.max),
                     [bS.r], [amaxT.r])
                P.op("dve", lambda e: e.tensor_copy(out=amax_exp.t[0:4, 0:Pn].rearrange("p (s l) -> p s l", s=nseq),
                                                     in_=amaxT.t[0:4, 0:nseq].unsqueeze(2).to_broadcast([4, nseq, L])), [amaxT.r], [amax_exp.r])
                P.op("pe", lambda e: e.matmul(bS.t[0:Pn, 160:164], amax_exp.t[0:4, 0:Pn], C("ident", 4, 4), start=True, stop=True), [amax_exp.r, consts.r], [bS.r])
                P.op("dve", lambda e, m0c=m0c: e.tensor_tensor(out=S("R"), in0=bS.t[0:Pn, 160:164], in1=m0c.t[0:Pn, 0:4], op=ALU.max), [bS.r, m0c.r], [s["R"].r])
                P.op("dve", lambda e: e.tensor_tensor(out=S("d1"), in0=S("a"), in1=S("R"), op=ALU.subtract), [s["a"].r, s["R"].r], [s["d1"].r])
                P.op("act", lambda e: e.activation(out=S("es"), in_=S("d1"), func=AF.Exp, bias=V("c_lnscale", 0, Pn)), [s["d1"].r, vecs.r], [s["es"].r])
                P.op("dve", lambda e, m0c=m0c: e.tensor_tensor(out=S("d2"), in0=m0c.t[0:Pn, 0:4], in1=S("R"), op=ALU.subtract), [m0c.r, s["R"].r], [s["d2"].r])
                P.op("act", lambda e: e.activation(out=S("c0"), in_=S("d2"), func=AF.Exp), [s["d2"].r], [s["c0"].r])
                P.op("dve", lambda e: e.tensor_tensor(out=S("d3"), in0=S("btbl", 0, 4), in1=S("R"), op=ALU.add), [s["btbl"].r, s["R"].r], [s["d3"].r])
                P.op("act", lambda e: e.activation(out=S("dfl"), in_=S("d3"), func=AF.Exp, scale=-1.0), [s["d3"].r], [s["dfl"].r])
                P.op("dve", lambda e, m0n=m0n: e.tensor_tensor(out=m0n.t[0:Pn, 0:4], in0=S("btbl", 4, 8), in1=S("R"), op=ALU.add), [s["btbl"].r, s["R"].r], [m0n.r])
                P.op("dve", lambda e, firstm=firstm: e.tensor_tensor(out=rhsx.t[0:Pn, 0:nseq, :], in0=S("c0").unsqueeze(1).to_broadcast([Pn, nseq, 4]),
                                                                        in1=firstm.unsqueeze(2).to_broadcast([Pn, nseq, 4]), op=ALU.mult), [s["c0"].r, consts.r], [rhsx.r])
                P.op("pe", lambda e: e.matmul(bS.t[:, 192:192 + nseq * 4], C("onesP", Pn, 128), rhsx.t[0:Pn, 0:nseq, :].rearrange("p s h -> p (s h)"), start=True, stop=True),
                     [rhsx.r, consts.r], [bS.r])
                P.op("dve", lambda e: e.tensor_copy(out=c0bc.t[:, 0:nseq, :].rearrange("p s h -> p (s h)"), in_=bS.t[:, 192:192 + nseq * 4]), [bS.r], [c0bc.r])

                for hh in range(4):
                    bSc, bN, bQ2 = banks[2], banks[3], banks[5]
                    stt = ST[hh % 2]
                    bV2 = banks[4]
                    v_tok = v_tk[hh % 2]
                    kp_tok = kp_tk[hh % 2]

                    def mmv(e, hh=hh):
                        for fl in range(4):
                            fo = hh * 4 + fl
                            lhs = (xmc_ap(fo, cs) if samp else xmh.t[:, fo, 3 + c * Pn:3 + (c + 1) * Pn])
                            ins = e.matmul(bV2.t[0:Pn, fl * 128:(fl + 1) * 128], lhs, bd.t[:, 2, fo, :], start=True, stop=True)
                        return ins
                    P.op("pe", mmv, [xmh.r, xn.r, bd.r], [bV2.r])
                    P.op("act", lambda e, v_tok=v_tok: e.activation(out=v_tok.t[0:Pn, :], in_=bV2.t[0:Pn, :], func=AF.Copy), [bV2.r], [v_tok.r])

                    def mmk(e, hh=hh):
                        for fl in range(4):
                            fo = hh * 4 + fl
                            ins = e.matmul(bV2.t[0:Pn, fl * 128:(fl + 1) * 128], xc.t[:, fo, cs], bd.t[:, 1, fo, :], start=True, stop=True)
                        return ins
                    P.op("pe", mmk, [xc.r, bd.r], [bV2.r])
                    P.op("act", lambda e, hh=hh, kp_tok=kp_tok: e.activation(out=kp_tok.t[0:Pn, :], in_=bV2.t[0:Pn, :], func=AF.Copy, scale=S("es", hh, hh + 1)),
                         [bV2.r, s["es"].r], [kp_tok.r])

                    def mms(e, hh=hh):
                        for dc in range(4):
                            ins = e.matmul(bSc.t[0:Pn, 0:Pn], kT.t[:, hh * 4 + dc, cs], qT.t[:, hh * 4 + dc, cs], start=(dc == 0), stop=(dc == 3))
                        return ins
                    P.op("pe", mms, [kT.r, qT.r], [bSc.r])
                    P.op("dve", lambda e, hh=hh, stt=stt, mask=mask: e.scalar_tensor_tensor(out=stt.t[0:Pn, 0:Pn], in0=bSc.t[0:Pn, 0:Pn], scalar=S("es", hh, hh + 1), in1=mask,
                                                                                          op0=ALU.mult, op1=ALU.mult), [bSc.r, s["es"].r, consts.r], [stt.r])
                    P.op("dve", lambda e, hh=hh: e.tensor_tensor(out=nbf.t[:, hh * 4:(hh + 1) * 4, 0:nseq], in0=nst.t[:, hh * 4:(hh + 1) * 4, 0:nseq],
                                                                 in1=c0bc.t[:, 0:nseq, hh].unsqueeze(1).to_broadcast([128, 4, nseq]), op=ALU.mult), [nst.r, c0bc.r], [nbf.r])
                    if not samp:
                        cb = Cbf[0]
                        P.op("act", lambda e, hh=hh, cb=cb: e.activation(out=cb.t[:], in_=Cst.t[:, hh * 4:(hh + 1) * 4, :], func=AF.Copy, scale=c0bc.t[:, 0, hh:hh + 1]),
                             [Cst[hh], c0bc.r], [cb.r])

                        def mmn(e, hh=hh, stt=stt, cb=cb, v_tok=v_tok):
                            e.matmul(bN.t[0:Pn, :], stt.t[0:Pn, 0:Pn], v_tok.t[0:Pn, :], start=True, stop=False)
                            for dc in range(4):
                                ins = e.matmul(bN.t[0:Pn, :], qT.t[:, hh * 4 + dc, cs], cb.t[:, dc, :], start=False, stop=(dc == 3))
                            return ins
                        P.op("pe", mmn, [stt.r, v_tok.r, qT.r, cb.r], [bN.r])
                    else:
                        P.op("pe", lambda e, hh=hh, stt=stt, v_tok=v_tok: e.matmul(bN.t[0:Pn, :], stt.t[0:Pn, 0:Pn], v_tok.t[0:Pn, :], start=True, stop=False),
                             [stt.r, v_tok.r], [bN.r])
                        for b in range(NB):
                            slot = (hh * NB + b) % NSL
                            cslot = Cst.t[:, slot * 4:(slot + 1) * 4, :]
                            cres = Cst[slot]
                            while ld_issued[0] <= min(hh * NB + b + NSL - 2, 4 * NB - 1):
                                k_ = ld_issued[0]
                                P.dma("sp", Cst.t[:, (k_ % NSL) * 4:(k_ % NSL + 1) * 4, :], sC_d[j, k_ % NB, k_ // NB].rearrange("(dc p) e -> p dc e", p=128), writes=[Cst[k_ % NSL]])
                                ld_issued[0] += 1
                            cb = Cbf[b % 2]
                            P.op("act", lambda e, cb=cb, cslot=cslot, b=b, hh=hh: e.activation(out=cb.t[:], in_=cslot, func=AF.Copy, scale=c0bc.t[:, b, hh:hh + 1]),
                                 [cres, c0bc.r], [cb.r])
                            qmt = qm[b % 2]
                            P.op("dve", lambda e, qmt=qmt, b=b, hh=hh: e.tensor_tensor(out=qmt.t[:], in0=qT.t[:, hh * 4:(hh + 1) * 4, 0:TS],
                                                                                       in1=colmask.t[:, b, :].unsqueeze(1).to_broadcast([128, 4, TS]), op=ALU.mult),
                                 [qT.r, colmask.r], [qmt.r])

                            def mmc(e, qmt=qmt, cb=cb, b=b):
                                for dc in range(4):
                                    ins = e.matmul(bN.t[0:Pn, :], qmt.t[:, dc, :], cb.t[:, dc, :], start=False, stop=(b == NB - 1 and dc == 3))
                                return ins
                            P.op("pe", mmc, [qmt.r, cb.r], [bN.r])
                            kpt = kpm[b % 2]
                            P.op("dve", lambda e, kpt=kpt, b=b, hh=hh, kp_tok=kp_tok: e.tensor_scalar(out=kpt.t[0:Pn, :], in0=kp_tok.t[0:Pn, :],
                                                                                       scalar1=consts.t[0:Pn, CL["seqS"] + b:CL["seqS"] + b + 1], scalar2=None, op0=ALU.mult),
                                 [kp_tok.r, consts.r], [kpt.r])
                            for dc in range(4):
                                bcu = banks[cnt[0] % 2]
                                cnt[0] += 1
                                P.op("pe", lambda e, bcu=bcu, kpt=kpt, dc=dc, hh=hh, v_tok=v_tok: e.matmul(bcu.t[:, :], kpt.t[0:Pn, dc * 128:(dc + 1) * 128], v_tok.t[0:Pn, :],
                                                                                              start=True, stop=True), [kpt.r, v_tok.r], [bcu.r])
                                P.op("dve", lambda e, bcu=bcu, dc=dc, b=b, hh=hh, slot=slot: e.scalar_tensor_tensor(out=Cst.t[:, slot * 4 + dc, :], in0=Cst.t[:, slot * 4 + dc, :],
                                                                                                                   scalar=c0bc.t[:, b, hh:hh + 1], in1=bcu.t[:, :], op0=ALU.mult, op1=ALU.add),
                                     [bcu.r, cres, c0bc.r], [cres])
                            P.dma("sp", Cs_o[j, b, hh].rearrange("(dc p) e -> p dc e", p=128), cslot, reads=[cres])

                    def mmq(e, hh=hh, stt=stt, seqmb=seqmb):
                        e.matmul(bQ2.t[0:Pn, 0:1], stt.t[0:Pn, 0:Pn], onesb[0:Pn, 0:1], start=True, stop=True)
                        for dc in range(4):
                            ins = e.matmul(bQ2.t[0:Pn, 8:8 + nseq], qT.t[:, hh * 4 + dc, cs], nbf.t[:, hh * 4 + dc, 0:nseq], start=(dc == 0), stop=(dc == 3))
                        return ins
                    P.op("pe", mmq, [stt.r, cbf.r, qT.r, nbf.r], [bQ2.r])
                    P.op("dve", lambda e, seqm=seqm: e.tensor_tensor(out=tmpn.t[0:Pn, 0:nseq], in0=bQ2.t[0:Pn, 8:8 + nseq], in1=seqm, op=ALU.mult), [bQ2.r, consts.r], [tmpn.r])
                    P.op("dve", lambda e: e.tensor_reduce(out=S("nqb", 0, 1), in_=tmpn.t[0:Pn, 0:nseq], axis=AX.X, op=ALU.add), [tmpn.r], [s["nqb"].r])
                    P.op("dve", lambda e: e.tensor_tensor(out=S("nq", 0, 1), in0=S("nqb", 0, 1), in1=bQ2.t[0:Pn, 0:1], op=ALU.add), [s["nqb"].r, bQ2.r], [s["nq"].r])
                    P.op("dve", lambda e: e.tensor_scalar(out=S("nq", 1, 2), in0=S("nq", 0, 1), scalar1=-1.0, scalar2=None, op0=ALU.mult), [s["nq"].r], [s["nq"].r])
                    P.op("dve", lambda e: e.tensor_tensor(out=S("nq", 2, 3), in0=S("nq", 0, 1), in1=S("nq", 1, 2), op=ALU.max), [s["nq"].r], [s["nq"].r])
                    P.op("dve", lambda e, hh=hh: e.tensor_tensor(out=S("den", 0, 1), in0=S("nq", 2, 3), in1=S("dfl", hh, hh + 1), op=ALU.max),
                         [s["nq"].r, s["dfl"].r], [s["den"].r])
                    P.op("dve", lambda e: e.bn_stats(out=st6.t[0:Pn, :], in_=bN.t[0:Pn, :]), [bN.r], [st6.r])
                    P.op("dve", lambda e: e.bn_aggr(out=S("mv", 0, 2), in_=st6.t[0:Pn, :]), [st6.r], [s["mv"].r])
                    P.op("dve", lambda e: e.tensor_tensor(out=S("t1", 0, 1), in0=S("den", 0, 1), in1=S("den", 0, 1), op=ALU.mult), [s["den"].r], [s["t1"].r])
                    P.op("dve", lambda e: e.scalar_tensor_tensor(out=S("t1", 0, 1), in0=S("t1", 0, 1), scalar=EPS, in1=S("mv", 1, 2), op0=ALU.mult, op1=ALU.add),
                         [s["t1"].r, s["mv"].r], [s["t1"].r])
                    P.op("act", lambda e: e.activation(out=S("rs", 0, 1), in_=S("t1", 0, 1), func=AF.Sqrt), [s["t1"].r], [s["rs"].r])
                    P.op("dve", lambda e: e.reciprocal(out=S("rs", 0, 1), in_=S("rs", 0, 1)), [s["rs"].r], [s["rs"].r])
                    P.op("dve", lambda e: e.scalar_tensor_tensor(out=S("nb", 0, 1), in0=S("mv", 0, 1), scalar=-1.0, in1=S("rs", 0, 1), op0=ALU.mult, op1=ALU.mult),
                         [s["mv"].r, s["rs"].r], [s["nb"].r])
                    P.op("act", lambda e, hh=hh: e.activation(out=hn_tok.t[0:Pn, c, hh * 512:(hh + 1) * 512], in_=bN.t[0:Pn, :], func=AF.Identity,
                                                              scale=S("rs", 0, 1), bias=S("nb", 0, 1)), [bN.r, s["rs"].r, s["nb"].r], [hn_tok.r])
                    if not samp:
                        for dc in range(4):
                            bcu = banks[cnt[0] % 2]
                            cnt[0] += 1
                            P.op("pe", lambda e, bcu=bcu, dc=dc, hh=hh, kp_tok=kp_tok, v_tok=v_tok: e.matmul(bcu.t[:, :], kp_tok.t[0:Pn, dc * 128:(dc + 1) * 128],
                                                                               v_tok.t[0:Pn, :], start=True, stop=True), [kp_tok.r, v_tok.r], [bcu.r])
                            P.op("dve", lambda e, bcu=bcu, dc=dc, hh=hh: e.scalar_tensor_tensor(out=Cst.t[:, hh * 4 + dc, :], in0=Cst.t[:, hh * 4 + dc, :], scalar=c0bc.t[:, 0, hh:hh + 1],
                                                                                              in1=bcu.t[:, :], op0=ALU.mult, op1=ALU.add), [bcu.r, Cst[hh], c0bc.r], [Cst[hh]])

                    def mmnn(e, hh=hh, seqmb=seqmb, kp_tok=kp_tok):
                        for dc in range(4):
                            ins = e.matmul(bQ2.t[:, 64 + dc * NB:64 + dc * NB + nseq], kp_tok.t[0:Pn, dc * 128:(dc + 1) * 128], seqmb, start=True, stop=True)
                        return ins
                    P.op("pe", mmnn, [kp_tok.r, cbf.r], [bQ2.r])
                    P.op("dve", lambda e, hh=hh: e.tensor_tensor(out=tmpn2.t[:, :, 0:nseq], in0=nst.t[:, hh * 4:(hh + 1) * 4, 0:nseq],
                                                                 in1=c0bc.t[:, 0:nseq, hh].unsqueeze(1).to_broadcast([128, 4, nseq]), op=ALU.mult), [nst.r, c0bc.r], [tmpn2.r])
                    P.op("dve", lambda e, hh=hh: e.tensor_tensor(out=nst.t[:, hh * 4:(hh + 1) * 4, 0:nseq], in0=tmpn2.t[:, :, 0:nseq],
                                                                 in1=bQ2.t[:, 64:64 + 4 * NB].rearrange("p (d s) -> p d s", d=4)[:, :, 0:nseq], op=ALU.add), [tmpn2.r, bQ2.r], [nst.r])

            bT = banks[2]
            bTb = bT.t[:].bitcast(BF16)
            for blk in range(8, 16):
                wt = next_wA()
                P.dma("pool", wt.t[:], wup_d[j][blk], writes=[wt.r])
                for fc in range(2):
                    fo = (blk - 8) * 2 + fc
                    bx = banks[cnt[0] % 2]
                    cnt[0] += 1

                    def mm(e, wt=wt, bx=bx, fc=fc):
                        for k in range(8):
                            ins = e.matmul(bx.t[:, 0:Tn], wt.t[:, k, fc * 128:(fc + 1) * 128], xn.t[:, k, 0:Tn], start=(k == 0), stop=(k == 7))
                        return ins
                    P.op("pe", mm, [wt.r, xn.r], [bx.r])
                    szt = sz[fo % 2]
                    eat = ea[0]
                    P.op("act", lambda e, bx=bx, szt=szt: e.activation(out=szt.t[:, 0:Tn], in_=bx.t[:, 0:Tn], func=AF.Silu), [bx.r], [szt.r])

                    def mmt(e, fo=fo):
                        for c in range(nch):
                            ins = e.transpose(out=bTb[:, c * Pn:(c + 1) * Pn], in_=hn_tok.t[0:Pn, c, fo * 128:(fo + 1) * 128], identity=identb[0:Pn, 0:Pn])
                        return ins
                    P.op("pe", mmt, [hn_tok.r, cbf.r], [bT.r])
                    P.op("act", lambda e, eat=eat, fo=fo: e.activation(out=eat.t[:, 0:Tn], in_=bTb[:, 0:Tn], func=AF.Copy, scale=V(f"ml_hn_g_{j}", fo)), [bT.r, vecs.r], [eat.r])
                    P.op("dve", lambda e, eat=eat, fo=fo: e.scalar_tensor_tensor(out=eat.t[:, 0:Tn], in0=xc.t[:, fo, 0:Tn], scalar=V(f"ml_skip_{j}", fo), in1=eat.t[:, 0:Tn],
                                                                               op0=ALU.mult, op1=ALU.add), [xc.r, eat.r, vecs.r], [eat.r])
                    P.op("dve", lambda e, eat=eat, szt=szt, fo=fo: e.tensor_tensor(out=oT.t[:, fo, 0:Tn], in0=eat.t[:, 0:Tn], in1=szt.t[:, 0:Tn], op=ALU.mult), [eat.r, szt.r], [oT.r])
            xr = xres(t0, Tn)
            for d in range(8):
                wdt = next_wB()
                wv = wdt.t[:, :].rearrange("p (f c) -> p f c", f=16)
                P.dma("pool", wv, wdn_d[j][d], writes=[wdt.r])
                bk = banks[cnt[0] % 2]
                cnt[0] += 1

                def mm(e, wv=wv, bk=bk):
                    for f in range(16):
                        ins = e.matmul(bk.t[:, 0:Tn], wv[:, f, :], oT.t[:, f, 0:Tn], start=(f == 0), stop=(f == 15))
                    return ins
                P.op("pe", mm, [wdt.r, oT.r], [bk.r])
                P.op("dve", lambda e, bk=bk, d=d: e.tensor_tensor(out=x.t[:, d, t0:t0 + Tn], in0=bk.t[:, 0:Tn], in1=x.t[:, d, t0:t0 + Tn], op=ALU.add), [bk.r] + xr, xr)
            if not samp:
                P.op("pool", lambda e: e.tensor_copy(out=xmh.t[:, :, 0:3], in_=xmh.t[:, :, Tn:Tn + 3]), [xmh.r], [xmh.r])
            if last_prompt:
                P.dma("sp", Cp_o[j].rearrange("h (dc p) e -> p (h dc) e", p=128), Cst.t[:], reads=[Cst[h_] for h_ in range(4)])
                P.dma("sp", npT_o[j], nst.t[:, :, 0:1], reads=[nst.r])
                P.dma("sp", mp_o[j], m0[m0_i[0]].t[:, 0:4], reads=[m0[m0_i[0]].r])
                P.dma("sp", convp_o[j], tailp.t[:], reads=[tailp.r])
            if samp:
                P.dma("sp", nsT_o[j], nst.t[:], reads=[nst.r])
                P.dma("sp", ms_o[j], m0[m0_i[0]].t[0:TS, 0:4], reads=[m0[m0_i[0]].r])
                P.dma("sp", convs_o[j], tails.t[:], reads=[tails.r])

    def chunk_mlp(i, j):
        P.phase()
        gname = f"norm_mix_{i}"
        T = 512
        win = P.atile([128, 8, 8, 256], BF16)
        wsP = P.atile([128, 4, 128], F32)
        wsS = P.atile([64, 4, 64], F32)
        wsPb = P.atile([128, 4, 128], BF16)
        wsSb = P.atile([64, 4, 64], BF16)
        bsP = P.atile([128, 4, 128], F32)
        bsS = P.atile([128, 4, 64], F32)
        xn = P.atile([128, 8, T], BF16)
        sqb = P.atile([128, 8, T], BF16)
        uT = P.atile([128, 8, T], BF16)
        vT = P.atile([128, 8, T], F32)
        vsq = sqb
        mu = P.atile([128, T], F32)
        rst = mu
        var = P.atile([128, T], F32)
        tmp = [P.atile([128, T], F32) for _ in range(2)]
        vn = P.atile([128, 8, T], BF16)
        vnf = P.atile([128, 8, TS], F32)
        vn_tok = P.atile([128, 1024], BF16)
        yT = xn
        for blk in range(8):
            P.dma("pool", win.t[:, blk], win_d[j][blk], writes=[win.r])
        P.dma("sp", wsP.t[:], wsP_d[j], writes=[wsP.r])
        P.dma("sp", wsS.t[:], wsS_d[j], writes=[wsS.r])
        P.dma("sp", bsP.t[:], bsP_d[j], writes=[bsP.r])
        P.dma("sp", bsS.t[:], bsS_d[j], writes=[bsS.r])
        P.op("dve", lambda e: e.tensor_tensor(out=wsPb.t[:], in0=wsP.t[:], in1=C("maskP", 128, 128).unsqueeze(1).to_broadcast([128, 4, 128]), op=ALU.mult),
             [wsP.r, consts.r], [wsPb.r])
        P.op("dve", lambda e: e.tensor_tensor(out=wsSb.t[:], in0=wsS.t[:], in1=C("maskS", 64, 64).unsqueeze(1).to_broadcast([64, 4, 64]), op=ALU.mult),
             [wsS.r, consts.r], [wsSb.r])
        cnt = 0
        for ti, (t0, Tn) in enumerate(FFN_TILES):
            samp = ti == 4
            Pn = TS if samp else 128
            nch = Tn // Pn
            rmsnorm(t0, Tn, gname, xn, xn.r, sqb, rst, banks[6])
            for fo in range(16):
                bx = banks[cnt % 2]
                cnt += 1

                def mm(e, bx=bx, fo=fo):
                    for k in range(8):
                        ins = e.matmul(bx.t[:, 0:Tn], win.t[:, fo // 2, k, (fo % 2) * 128:(fo % 2 + 1) * 128], xn.t[:, k, 0:Tn], start=(k == 0), stop=(k == 7))
                    return ins
                P.op("pe", mm, [win.r, xn.r], [bx.r])
                if fo < 8:
                    P.op("act", lambda e, bx=bx, fo=fo: e.activation(out=uT.t[:, fo, 0:Tn], in_=bx.t[:, 0:Tn], func=AF.Gelu, bias=V(f"cm_b_in_{j}", fo)), [bx.r, vecs.r], [uT.r])
                else:
                    P.op("act", lambda e, bx=bx, fo=fo: e.activation(out=vT.t[:, fo - 8, 0:Tn], in_=bx.t[:, 0:Tn], func=AF.Gelu, bias=V(f"cm_b_in_{j}", fo)), [bx.r, vecs.r], [vT.r])
            P.op("act", lambda e: e.activation(out=vsq.t[:, :, 0:Tn], in_=vT.t[:, :, 0:Tn], func=AF.Square), [vT.r], [vsq.r])
            bM1, bM2 = banks[2], banks[3]

            def mm1(e):
                for k in range(8):
                    ins = e.matmul(bM1.t[:, 0:Tn], C("onesP", 128, 128), vT.t[:, k, 0:Tn], start=(k == 0), stop=(k == 7))
                return ins
            P.op("pe", mm1, [vT.r, consts.r], [bM1.r])

            def mm2(e):
                for k in range(8):
                    ins = e.matmul(bM2.t[:, 0:Tn], onesb, vsq.t[:, k, 0:Tn], start=(k == 0), stop=(k == 7))
                return ins
            P.op("pe", mm2, [vsq.r, cbf.r], [bM2.r])
            P.op("dve", lambda e: e.tensor_scalar(out=mu.t[:, 0:Tn], in0=bM1.t[:, 0:Tn], scalar1=1.0 / D, scalar2=None, op0=ALU.mult), [bM1.r], [mu.r])
            P.op("dve", lambda e: e.tensor_tensor(out=var.t[:, 0:Tn], in0=mu.t[:, 0:Tn], in1=mu.t[:, 0:Tn], op=ALU.mult), [mu.r], [var.r])
            P.op("dve", lambda e: e.scalar_tensor_tensor(out=var.t[:, 0:Tn], in0=bM2.t[:, 0:Tn], scalar=1.0 / D, in1=var.t[:, 0:Tn], op0=ALU.mult, op1=ALU.subtract),
                 [bM2.r, var.r], [var.r])
            P.op("act", lambda e: e.activation(out=var.t[:, 0:Tn], in_=var.t[:, 0:Tn], func=AF.Sqrt, bias=V("c_eps")), [var.r, vecs.r], [var.r])
            P.op("dve", lambda e: e.reciprocal(out=var.t[:, 0:Tn], in_=var.t[:, 0:Tn]), [var.r], [var.r])
            for k in range(8):
                tt = tmp[k % 2]
                P.op("dve", lambda e, k=k, tt=tt: e.tensor_tensor(out=tt.t[:, 0:Tn], in0=vT.t[:, k, 0:Tn], in1=mu.t[:, 0:Tn], op=ALU.subtract), [vT.r, mu.r], [tt.r])
                P.op("dve", lambda e, k=k, tt=tt: e.scalar_tensor_tensor(out=vn.t[:, k, 0:Tn], in0=tt.t[:, 0:Tn], scalar=V(f"cm_ln_g_{j}", k), in1=var.t[:, 0:Tn],
                                                                       op0=ALU.mult, op1=ALU.mult), [tt.r, var.r, vecs.r], [vn.r])
                if samp:
                    P.op("dve", lambda e, k=k, tt=tt: e.scalar_tensor_tensor(out=vnf.t[:, k, 0:Tn], in0=tt.t[:, 0:Tn], scalar=V(f"cm_ln_g_{j}", k), in1=var.t[:, 0:Tn],
                                                                           op0=ALU.mult, op1=ALU.mult), [tt.r, var.r, vecs.r], [vnf.r])
            if samp:
                P.dma("sp", vsT_o[j], vnf.t[:], reads=[vnf.r])
            bT = banks[4]
            bTb = bT.t[:].bitcast(BF16)
            wsb = wsSb if samp else wsPb
            bsb = bsS if samp else bsP
            for c in range(nch):
                cs = slice(c * Pn, (c + 1) * Pn)

                def mmt(e, cs=cs):
                    for k in range(8):
                        ins = e.transpose(out=bTb[0:Pn, k * 128:(k + 1) * 128], in_=vn.t[:, k, cs], identity=identb)
                    return ins
                P.op("pe", mmt, [vn.r, cbf.r], [bT.r])
                P.op("act", lambda e: e.activation(out=vn_tok.t[0:Pn, :], in_=bTb[0:Pn, :], func=AF.Copy), [bT.r], [vn_tok.r])
                for k in range(8):
                    g = k // 2
                    bmx = banks[cnt % 2]
                    cnt += 1
                    tt = tmp[k % 2]
                    P.op("pe", lambda e, bmx=bmx, k=k, g=g: e.matmul(bmx.t[:, 0:Pn], vn_tok.t[0:Pn, k * 128:(k + 1) * 128], wsb.t[0:Pn, g, 0:Pn], start=True, stop=True),
                         [vn_tok.r, wsb.r], [bmx.r])
                    P.op("dve", lambda e, bmx=bmx, tt=tt, g=g: e.tensor_tensor(out=tt.t[:, 0:Pn], in0=bmx.t[:, 0:Pn], in1=bsb.t[:, g, 0:Pn], op=ALU.add), [bmx.r, bsb.r], [tt.r])
                    P.op("dve", lambda e, tt=tt, k=k, cs=cs: e.tensor_tensor(out=yT.t[:, k, cs], in0=tt.t[:, 0:Pn], in1=uT.t[:, k, cs], op=ALU.mult), [tt.r, uT.r], [yT.r])
            for d in range(8):
                bk = banks[cnt % 2]
                cnt += 1
                wdt = next_wB()
                wv = wdt.t[:, 0:1024].rearrange("p (f c) -> p f c", f=8)
                P.dma("pool", wv, wout_d[j][d], writes=[wdt.r])

                def mm(e, bk=bk, wv=wv):
                    for f in range(8):
                        ins = e.matmul(bk.t[:, 0:Tn], wv[:, f, :], yT.t[:, f, 0:Tn], start=(f == 0), stop=(f == 7))
                    return ins
                P.op("pe", mm, [wdt.r, yT.r], [bk.r])
                P.op("dve", lambda e, bk=bk, d=d: e.tensor_tensor(out=x.t[:, d, t0:t0 + Tn], in0=bk.t[:, 0:Tn], in1=x.t[:, d, t0:t0 + Tn], op=ALU.add), [bk.r, x[ti]], [x[ti]])

    if dbg == "ffn":
        ffn(0, 1)
    for i in range(n_layers):
        ffn(i, 1)
        if i % 2 == 0:
            if dbg != "onlysamp":
                mlstm(i, i // 2, False)
            if dbg == "newsamp":
                mlstm(i, i // 2, True)
            elif dbg != "nosamp":
                mlstm_v1_sample(i, i // 2)
        else:
            chunk_mlp(i, i // 2)
        ffn(i, 2)

    P.phase()
    sqb = P.atile([128, 8, 512], BF16)
    rst = P.atile([128, 512], F32)
    yst = [P.atile([128, 8, 512], F32) for _ in range(2)]
    yv = yT_o.rearrange("(k p) t -> p k t", p=128)
    for ti, (t0, n) in enumerate(FFN_TILES):
        ys = yst[ti % 2]
        rmsnorm(t0, n, "norm_final", ys, ys.r, sqb, rst, banks[6])
        P.dma("sp", yv[:, :, t0:t0 + n], ys.t[:, :, 0:n], reads=[ys.r])
    P.emit()
    return nc, P


def _cols(v):
    v = np.asarray(v, np.float32)
    return np.ascontiguousarray(v.reshape(-1, 128).T)


def _blk_k(W, ncol):
    K, F = W.shape
    return np.ascontiguousarray(W.reshape(K // 128, 128, F // ncol, ncol).transpose(2, 1, 0, 3))


def _blk_f(W, ncol):
    Fin, Dn = W.shape
    return np.ascontiguousarray(W.reshape(Fin // 128, 128, Dn // ncol, ncol).transpose(2, 1, 0, 3))


def prep_inputs(inp):
    f32 = np.float32
    g = {k: np.asarray(v) for k, v in inp.items()}
    shared = {}
    vecs = np.zeros((128, VL["_n"]), f32)

    def put(name, v):
        c = _cols(v)
        vecs[:, VL[name]:VL[name] + c.shape[1]] = c
    for i in range(DEPTH):
        put(f"norm_ff1_{i}", g["norm_ff1"][i])
        put(f"norm_mix_{i}", g["norm_mix"][i])
        put(f"norm_ff2_{i}", g["norm_ff2"][i])
    put("norm_final", g["norm_final"])
    for j in range(2):
        for w in range(4):
            put(f"ml_wconv_{j}_{w}", g["ml_w_conv"][j, w])
        put(f"ml_bconv_{j}", g["ml_b_conv"][j])
        put(f"ml_hn_g_{j}", g["ml_hn_g"][j])
        put(f"ml_skip_{j}", g["ml_skip"][j])
        put(f"cm_b_in_{j}", g["cm_b_in"][j])
        put(f"cm_ln_g_{j}", g["cm_ln_g"][j])
    vecs[:, VL["c_eps"]] = EPS
    vecs[:, VL["c_lnscale"]] = math.log(DH ** -0.5)
    shared["vecs"] = vecs
    cst = np.zeros((128, CL["_n"]), f32)
    cst[:, CL["ident"]:CL["ident"] + 128] = np.eye(128, dtype=f32)
    cst[:, CL["maskP"]:CL["maskP"] + 128] = np.triu(np.ones((128, 128), f32))
    cst[:, CL["onesP"]:CL["onesP"] + 128] = 1.0
    cst[:64, CL["maskS"]:CL["maskS"] + 64] = np.kron(np.eye(NB, dtype=f32), np.triu(np.ones((4, 4), f32)))
    cst[:64, CL["onesS"]:CL["onesS"] + 64] = np.kron(np.eye(NB, dtype=f32), np.ones((4, 4), f32))
    cst[:64, CL["seqS"]:CL["seqS"] + 16] = np.kron(np.eye(NB, dtype=f32), np.ones((4, 1), f32))
    fs = np.zeros((64, 16), f32)
    fs[np.arange(16) * 4, np.arange(16)] = 1.0
    cst[:64, CL["firstS"]:CL["firstS"] + 16] = fs
    cst[:, CL["seqP"]] = 1.0
    cst[0, CL["firstP"]] = 1.0
    shared["consts"] = cst
    cm = np.zeros((128, NB, TS), f32)
    for b in range(NB):
        cm[:, b, 4 * b:4 * b + 4] = 1.0
    shared["colmask"] = cm
    for i in range(DEPTH):
        for a in (1, 2):
            shared[f"wg{i}{a}"] = _blk_k(g[f"ffn{a}_w_gate"][i], 256)
            shared[f"wu{i}{a}"] = _blk_k(g[f"ffn{a}_w_up"][i], 256)
            shared[f"wd{i}{a}"] = _blk_f(g[f"ffn{a}_w_down"][i], 256)
    for j in range(2):
        shared[f"wup{j}"] = _blk_k(g["ml_w_up"][j], 256)
        shared[f"wdn{j}"] = _blk_f(g["ml_w_down"][j], 128)
        bd = np.zeros((128, 3, 16, 128), f32)
        for wi, nm in enumerate(["ml_w_q", "ml_w_k", "ml_w_v"]):
            w = g[nm][j].reshape(16, 32, 4, 4)
            for n in range(32):
                bd[4 * n:4 * n + 4, wi, :, 4 * n:4 * n + 4] = w[:, n].transpose(1, 0, 2)
        shared[f"bd{j}"] = bd
        wcat = np.concatenate([g["ml_w_ig"][j], g["ml_w_fg"][j]], axis=1)
        shared[f"wgate{j}"] = np.ascontiguousarray(wcat.reshape(48, 128, 8).transpose(1, 0, 2))
        shared[f"bgate{j}"] = np.ascontiguousarray(np.broadcast_to(np.concatenate([g["ml_b_ig"][j], g["ml_b_fg"][j]])[None, :], (128, 8))).astype(f32)
        shared[f"win{j}"] = _blk_k(g["cm_w_in"][j], 256)
        shared[f"wout{j}"] = _blk_f(g["cm_w_out"][j], 128)
        ws = g["cm_w_s"][j]
        shared[f"wsP{j}"] = np.ascontiguousarray(ws.transpose(2, 0, 1))
        wss = np.zeros((64, 4, 64), f32)
        for b in range(NB):
            wss[4 * b:4 * b + 4, :, 4 * b:4 * b + 4] = ws[:, :4, :4].transpose(2, 0, 1)
        shared[f"wsS{j}"] = wss
        bs = g["cm_b_s"][j]
        shared[f"bsP{j}"] = np.ascontiguousarray(np.broadcast_to(bs[None, :, :], (128, 4, 128))).astype(f32)
        shared[f"bsS{j}"] = np.ascontiguousarray(np.broadcast_to(np.tile(bs[:, :4], (1, NB))[None, :, :], (128, 4, 64))).astype(f32)
    in_maps = []
    for c in range(N_CORES):
        m = dict(shared)
        sl = slice(c * NB, (c + 1) * NB)
        xp = g["x_prompt"][c]
        xs = g["x_sample"][sl].reshape(TS, D)
        m["xT"] = np.ascontiguousarray(np.concatenate([xp, xs], axis=0).T)
        m["sC"] = np.ascontiguousarray(g["state_C"][:, sl])
        sn = g["state_n"][:, sl]
        m["snT"] = np.ascontiguousarray(sn.reshape(2, NB, 16, 128).transpose(0, 3, 2, 1))
        m["sm"] = np.ascontiguousarray(np.repeat(g["state_m"][:, sl], 4, axis=1))
        sc = g["state_conv"][:, sl]
        m["sconvT"] = np.ascontiguousarray(sc.reshape(2, NB, 3, 16, 128).transpose(0, 4, 3, 1, 2))
        in_maps.append(m)
    return in_maps


def assemble(results, n_cores=N_CORES):
    f32 = np.float32
    y_p = np.zeros((8, TP, D), f32)
    y_s = np.zeros((128, 4, D), f32)
    C_p = np.zeros((2, 8, H, DH, DH), f32)
    n_p = np.zeros((2, 8, H, DH), f32)
    m_p = np.zeros((2, 8, H), f32)
    cv_p = np.zeros((2, 8, 3, INNER), f32)
    C_s = np.zeros((2, 128, H, DH, DH), f32)
    n_s = np.zeros((2, 128, H, DH), f32)
    m_s = np.zeros((2, 128, H), f32)
    cv_s = np.zeros((2, 128, 3, INNER), f32)
    v_s = np.zeros((2, 128, 4, D), f32)
    for c in range(n_cores):
        r = results[c]
        sl = slice(c * NB, (c + 1) * NB)
        yT = r["yT"]
        y_p[c] = yT[:, :TP].T
        y_s[sl] = yT[:, TP:].T.reshape(NB, 4, D)
        C_p[:, c] = r["Cp"]
        n_p[:, c] = r["npT"][:, :, :, 0].transpose(0, 2, 1).reshape(2, H, DH)
        m_p[:, c] = r["mp"][:, 0, :]
        cv_p[:, c] = r["convpT"].transpose(0, 3, 2, 1).reshape(2, 3, INNER)
        C_s[:, sl] = r["Cs"]
        n_s[:, sl] = r["nsT"].transpose(0, 3, 2, 1).reshape(2, NB, H, DH)
        m_s[:, sl] = r["ms"][:, ::4, :]
        cv_s[:, sl] = r["convsT"].transpose(0, 3, 4, 2, 1).reshape(2, NB, 3, INNER)
        v_s[:, sl] = r["vsT"].transpose(0, 3, 2, 1).reshape(2, NB, 4, D)
    return (y_p, y_s, C_p, n_p, m_p, cv_p, C_s, n_s, m_s, cv_s, v_s)


_NC_CACHE = {}


def kernel(**inputs):
    in_maps = prep_inputs(inputs)
    if "nc" not in _NC_CACHE:
        _NC_CACHE["nc"] = build_program()[0]
    nc = _NC_CACHE["nc"]
    res = run_bass_kernel_spmd(nc, in_maps, core_ids=list(range(N_CORES)))
    return assemble(res.results)
```
